# Optimizing a Trainium2 kernel written in Bass

```python
import math, functools
import jax, jax.numpy as jnp
from jax import lax
import numpy as np

D_MODEL = 1024
BATCH = 2
SEQ = 16384
DEPTH = 4

GRID_W = 64
CTX_LEN = 256
N_MIXERS = 3
N_A_LAYERS = (DEPTH + 2) // 3
N_B_LAYERS = (DEPTH + 1) // 3
N_C_LAYERS = DEPTH // 3
DEEPNORM_ALPHA = (2.0 * DEPTH) ** 0.25
DEEPNORM_BETA = (8.0 * DEPTH) ** -0.25
LN_EPS = 1e-6
RMS_EPS = 1e-6
NEG_INF = -1e30
ROPE_THETA = 10000.0
ATT_BLOCK = 128
MOD_SCALE = 0.5
S5_GROUP_CH = 16
S5_GROUPS = D_MODEL // S5_GROUP_CH
S5_STATE = 64
S5_DT_MIN = 0.001
S5_DT_MAX = 0.1
SCAN_CHUNK = 128
WINDOW = 128
HALO = ((WINDOW + ATT_BLOCK - 1) // ATT_BLOCK) * ATT_BLOCK
HEAD_DIM_B = 64
HEADS_B = D_MODEL // HEAD_DIM_B
KV_HEADS_B = 2
HEAD_DIM_C = 128
HEADS_C = D_MODEL // HEAD_DIM_C
KV_HEADS_C = 2
N_EXPERTS = 16
N_EXPERT_GROUPS = 4
EXPERTS_PER_GROUP = N_EXPERTS // N_EXPERT_GROUPS
TOP_K = 2
D_EXPERT = D_MODEL // 2

kernel_name = 'hybrid_s5_swa_axialgqa_moe_dit'


def _layer_norm(x, g, b):
    xf = x.astype(jnp.float32)
    xc = xf - xf.mean(-1, keepdims=True)
    var = jnp.mean(xc * xc, -1, keepdims=True)
    return (xc * lax.rsqrt(var + LN_EPS) * g.astype(jnp.float32) + b.astype(jnp.float32)).astype(x.dtype)


def _rms_norm(x, g):
    xf = x.astype(jnp.float32)
    return (xf * lax.rsqrt(jnp.mean(xf * xf, -1, keepdims=True) + RMS_EPS) * g.astype(jnp.float32)).astype(x.dtype)


def _modulation(cond, w, b):
    return jnp.split(jax.nn.silu(cond) @ w + b, 6, axis=-1)


def _axial_rope_tables(n_rows, head_dim):
    quarter = head_dim // 4
    inv_freq = ROPE_THETA ** (-jnp.arange(quarter, dtype=jnp.float32) / quarter)
    rows = jnp.repeat(jnp.arange(n_rows, dtype=jnp.float32), GRID_W)
    cols = jnp.tile(jnp.arange(GRID_W, dtype=jnp.float32), n_rows)
    ang = jnp.stack([rows[:, None] * inv_freq, cols[:, None] * inv_freq], axis=1)
    return jnp.cos(ang), jnp.sin(ang)


def _apply_axial_rope(x, cos, sin):
    Bsz, S, H, Dh = x.shape
    xr = x.reshape(Bsz, S, H, 2, 2, Dh // 4)
    x1, x2 = xr[..., 0, :], xr[..., 1, :]
    c = cos[None, :, None].astype(x.dtype)
    s = sin[None, :, None].astype(x.dtype)
    return jnp.stack([x1 * c - x2 * s, x2 * c + x1 * s], axis=-2).reshape(x.shape)


def _split_qkv(proj, n_heads, n_kv, head_dim):
    Bsz, L, _ = proj.shape
    q, k, v = jnp.split(proj, [n_heads * head_dim, (n_heads + n_kv) * head_dim], axis=-1)
    return (q.reshape(Bsz, L, n_heads, head_dim), k.reshape(Bsz, L, n_kv, head_dim),
            v.reshape(Bsz, L, n_kv, head_dim))


def _group_heads(q, n_kv):
    Bsz, L, H, Dh = q.shape
    return q.reshape(Bsz, L, n_kv, H // n_kv, Dh)


def _to_query_blocks(q, n_kv):
    Bsz, S, H, Dh = q.shape
    qb = q.reshape(Bsz, S // ATT_BLOCK, ATT_BLOCK, n_kv, H // n_kv, Dh)
    return qb.transpose(1, 0, 2, 3, 4, 5)


def _from_query_blocks(o):
    nb, Bsz, T, KV, R, Dh = o.shape
    return o.transpose(1, 0, 2, 3, 4, 5).reshape(Bsz, nb * T, KV * R * Dh)


def _attend(q, kv_groups, sink):
    scale = q.shape[-1] ** -0.5
    scores = []
    for k, v, mask in kv_groups:
        s = jnp.einsum('btkrd,blkd->bkrtl', q, k, preferred_element_type=jnp.float32) * scale
        if mask is not None:
            s = jnp.where(mask, s, NEG_INF)
        scores.append(s)
    m = functools.reduce(jnp.maximum, [s.max(-1, keepdims=True) for s in scores])
    if sink is not None:
        sink_logit = sink.astype(jnp.float32)[None, :, :, None, None]
        m = jnp.maximum(m, sink_logit)
    probs = [jnp.exp(s - m) for s in scores]
    denom = functools.reduce(jnp.add, [p.sum(-1, keepdims=True) for p in probs])
    if sink is not None:
        denom = denom + jnp.exp(sink_logit - m)
    inv = 1.0 / denom
    outs = [jnp.einsum('bkrtl,blkd->btkrd', (p * inv).astype(grp[1].dtype), grp[1])
            for p, grp in zip(probs, kv_groups)]
    return functools.reduce(jnp.add, outs)


def _complex_linear_combine(e1, e2):
    a1r, a1i, b1r, b1i = e1
    a2r, a2i, b2r, b2i = e2
    return (a2r * a1r - a2i * a1i, a2r * a1i + a2i * a1r,
            a2r * b1r - a2i * b1i + b2r, a2r * b1i + a2i * b1r + b2i)


def _s5_discretise(a_re, a_im, log_dt, b_re, b_im):
    f32 = jnp.float32
    a_re, a_im, b_re, b_im = a_re.astype(f32), a_im.astype(f32), b_re.astype(f32), b_im.astype(f32)
    dt = jnp.exp(log_dt.astype(f32))[:, None]
    mag = jnp.exp(a_re * dt)
    lam_re, lam_im = mag * jnp.cos(a_im * dt), mag * jnp.sin(a_im * dt)
    inv_den = 1.0 / (a_re * a_re + a_im * a_im)
    n_re = lam_re - 1.0
    f_re = (n_re * a_re + lam_im * a_im) * inv_den
    f_im = (lam_im * a_re - n_re * a_im) * inv_den
    bb_re = f_re[..., None] * b_re - f_im[..., None] * b_im
    bb_im = f_re[..., None] * b_im + f_im[..., None] * b_re
    return lam_re, lam_im, bb_re, bb_im


def _s5_scan(u, h0_re, h0_im, lam_re, lam_im, bb_re, bb_im, c_re, c_im, with_output):
    Bsz, L, G, GC = u.shape
    n_chunks = L // SCAN_CHUNK
    u_chunks = u.reshape(Bsz, n_chunks, SCAN_CHUNK, G, GC).transpose(1, 2, 0, 3, 4)
    lam_t_re = jnp.broadcast_to(lam_re, (SCAN_CHUNK, 1) + lam_re.shape)
    lam_t_im = jnp.broadcast_to(lam_im, (SCAN_CHUNK, 1) + lam_im.shape)

    def chunk_step(carry, u_blk):
        h_re, h_im = carry
        x_re = jnp.einsum('tbgc,gpc->tbgp', u_blk, bb_re)
        x_im = jnp.einsum('tbgc,gpc->tbgp', u_blk, bb_im)
        p_re, p_im, s_re, s_im = lax.associative_scan(
            _complex_linear_combine, (lam_t_re, lam_t_im, x_re, x_im), axis=0)
        s_re, s_im = (s_re + p_re * h_re - p_im * h_im, s_im + p_re * h_im + p_im * h_re)
        y = None
        if with_output:
            y = jnp.einsum('tbgp,gcp->tbgc', s_re, c_re) - jnp.einsum('tbgp,gcp->tbgc', s_im, c_im)
        return (s_re[-1], s_im[-1]), y

    (h_re, h_im), y = lax.scan(chunk_step, (h0_re, h0_im), u_chunks)
    if with_output:
        y = y.transpose(2, 0, 1, 3, 4).reshape(Bsz, L, G * GC)
    return y, h_re, h_im


def _s5_mixer(h_lat, h_ctx, a_re, a_im, log_dt, b_re, b_im, c_re, c_im, d_skip, w_gate, w_val, ctx_out):
    f32 = jnp.float32
    Bsz, S, D = h_lat.shape
    n_ctx = h_ctx.shape[1]
    u_lat = h_lat.astype(f32).reshape(Bsz, S, S5_GROUPS, S5_GROUP_CH)
    u_ctx = h_ctx.astype(f32).reshape(Bsz, n_ctx, S5_GROUPS, S5_GROUP_CH)
    zero = jnp.zeros((Bsz, S5_GROUPS, S5_STATE), f32)
    y_lat = d_skip.astype(f32) * h_lat.astype(f32)
    y_ctx = d_skip.astype(f32) * h_ctx.astype(f32) if ctx_out else None
    for direction in range(2):
        flip = (lambda t: t[:, ::-1]) if direction == 1 else (lambda t: t)
        lam_re, lam_im, bb_re, bb_im = _s5_discretise(
            a_re[direction], a_im[direction], log_dt[direction], b_re[direction], b_im[direction])
        cr, ci = c_re[direction].astype(f32), c_im[direction].astype(f32)
        yc, hc_re, hc_im = _s5_scan(flip(u_ctx), zero, zero, lam_re, lam_im, bb_re, bb_im, cr, ci, ctx_out)
        yl, _, _ = _s5_scan(flip(u_lat), hc_re, hc_im, lam_re, lam_im, bb_re, bb_im, cr, ci, True)
        y_lat = y_lat + flip(yl)
        if ctx_out:
            y_ctx = y_ctx + flip(yc)

    def glu(y, dtype):
        g = jax.nn.gelu(y.astype(dtype))
        return (g @ w_val) * jax.nn.sigmoid(g @ w_gate)

    out_lat = glu(y_lat, h_lat.dtype)
    out_ctx = glu(y_ctx, h_ctx.dtype) if ctx_out else None
    return out_lat, out_ctx


def _windowed_gqa(h_lat, h_ctx, w_qkv, w_o, sink, cos, sin, ctx_out):
    Bsz, S, _ = h_lat.shape
    n_ctx = h_ctx.shape[1]
    q, k, v = _split_qkv(h_lat @ w_qkv, HEADS_B, KV_HEADS_B, HEAD_DIM_B)
    q, k = _apply_axial_rope(q, cos, sin), _apply_axial_rope(k, cos, sin)
    qc, kc, vc = _split_qkv(h_ctx @ w_qkv, HEADS_B, KV_HEADS_B, HEAD_DIM_B)
    sink_g = sink.reshape(KV_HEADS_B, HEADS_B // KV_HEADS_B)
    pad = ((0, 0), (HALO, HALO), (0, 0), (0, 0))
    kp, vp = jnp.pad(k, pad), jnp.pad(v, pad)
    band = ATT_BLOCK + 2 * HALO
    qi = jnp.arange(ATT_BLOCK)[:, None]
    kj = jnp.arange(band)[None, :]
    in_window = jnp.abs(kj - HALO - qi) <= WINDOW

    def one_block(args):
        b, q_blk = args
        start = b * ATT_BLOCK
        k_blk = lax.dynamic_slice_in_dim(kp, start, band, axis=1)
        v_blk = lax.dynamic_slice_in_dim(vp, start, band, axis=1)
        key_pos = start - HALO + kj
        mask = in_window & (key_pos >= 0) & (key_pos < S)
        return _attend(q_blk, ((k_blk, v_blk, mask), (kc, vc, None)), sink_g)

    o = lax.map(one_block, (jnp.arange(S // ATT_BLOCK), _to_query_blocks(q, KV_HEADS_B)))
    y_lat = _from_query_blocks(o) @ w_o
    y_ctx = None
    if ctx_out:
        o_c = _attend(_group_heads(qc, KV_HEADS_B), ((kc, vc, None),), sink_g)
        y_ctx = o_c.reshape(Bsz, n_ctx, HEADS_B * HEAD_DIM_B) @ w_o
    return y_lat, y_ctx


def _axial_gqa(h_lat, h_ctx, w_qkv, w_o, q_norm, k_norm, cos, sin, ctx_out):
    Bsz, S, _ = h_lat.shape
    n_ctx = h_ctx.shape[1]
    q, k, v = _split_qkv(h_lat @ w_qkv, HEADS_C, KV_HEADS_C, HEAD_DIM_C)
    q = _apply_axial_rope(_rms_norm(q, q_norm), cos, sin)
    k = _apply_axial_rope(_rms_norm(k, k_norm), cos, sin)
    qc, kc, vc = _split_qkv(h_ctx @ w_qkv, HEADS_C, KV_HEADS_C, HEAD_DIM_C)
    kc = _rms_norm(kc, k_norm)

    def one_block(q_blk):
        return _attend(q_blk, ((k, v, None), (kc, vc, None)), None)

    o = lax.map(one_block, _to_query_blocks(q, KV_HEADS_C))
    y_lat = _from_query_blocks(o) @ w_o
    y_ctx = None
    if ctx_out:
        o_c = _attend(_group_heads(_rms_norm(qc, q_norm), KV_HEADS_C), ((kc, vc, None),), None)
        y_ctx = o_c.reshape(Bsz, n_ctx, HEADS_C * HEAD_DIM_C) @ w_o
    return y_lat, y_ctx


def _moe(h, router_w, router_b, w1, w3, w2):
    n_tok = h.shape[0]
    aff = jax.nn.sigmoid((h @ router_w).astype(jnp.float32))
    sel = aff + router_b.astype(jnp.float32)
    group_score = lax.top_k(sel.reshape(n_tok, N_EXPERT_GROUPS, EXPERTS_PER_GROUP), TOP_K)[0].sum(-1)
    group = jnp.argmax(group_score, axis=-1)
    expert_group = jnp.arange(N_EXPERTS) // EXPERTS_PER_GROUP
    masked = jnp.where(expert_group[None, :] == group[:, None], sel, NEG_INF)
    _, idx = lax.top_k(masked, TOP_K)
    gate = jnp.take_along_axis(aff, idx, axis=-1)
    gate = gate / gate.sum(-1, keepdims=True)
    combine = jnp.einsum('nk,nke->ne', gate, jax.nn.one_hot(idx, N_EXPERTS, dtype=jnp.float32)).astype(h.dtype)
    y = jnp.zeros_like(h)
    for e in range(N_EXPERTS):
        out = (jax.nn.silu(h @ w1[e]) * (h @ w3[e])) @ w2[e]
        y = y + combine[:, e:e + 1] * out
    return y


def setup_inputs(seed: int = 0) -> dict:
    key = jax.random.key(seed)
    ks = iter(jax.random.split(key, 40))
    f32 = jnp.float32

    def nrm(shape, scale):
        return scale * jax.random.normal(next(ks), shape, f32)

    D = D_MODEL
    qkv_b = (HEADS_B + 2 * KV_HEADS_B) * HEAD_DIM_B
    qkv_c = (HEADS_C + 2 * KV_HEADS_C) * HEAD_DIM_C
    s5_shape = (N_A_LAYERS, 2, S5_GROUPS, S5_STATE)
    return {
        'x': nrm((BATCH, SEQ, D), 1.0),
        'c': nrm((BATCH, D), 1.0),
        'ctx': nrm((BATCH, CTX_LEN, D), 1.0),
        'c_ctx': nrm((D,), 1.0),
        'mod_w': nrm((DEPTH, D, 6 * D), MOD_SCALE * D ** -0.5),
        'mod_b': nrm((DEPTH, 6 * D), 0.02),
        'ln_g': 1.0 + nrm((DEPTH, 2, D), 0.02),
        'ln_b': nrm((DEPTH, 2, D), 0.02),
        'router_w': nrm((D, N_EXPERTS), D ** -0.5),
        'router_b': nrm((N_EXPERTS,), 0.01),
        'moe_w1': nrm((DEPTH, N_EXPERTS, D, D_EXPERT), D ** -0.5),
        'moe_w3': nrm((DEPTH, N_EXPERTS, D, D_EXPERT), D ** -0.5),
        'moe_w2': nrm((DEPTH, N_EXPERTS, D_EXPERT, D), DEEPNORM_BETA * D_EXPERT ** -0.5),
        's5_a_re': -0.5 + nrm(s5_shape, 0.01),
        's5_a_im': jnp.pi * jnp.arange(S5_STATE, dtype=f32) + nrm(s5_shape, 0.01),
        's5_log_dt': jax.random.uniform(next(ks), (N_A_LAYERS, 2, S5_GROUPS), f32,
                                        math.log(S5_DT_MIN), math.log(S5_DT_MAX)),
        's5_b_re': nrm((N_A_LAYERS, 2, S5_GROUPS, S5_STATE, S5_GROUP_CH), (2 * S5_GROUP_CH) ** -0.5),
        's5_b_im': nrm((N_A_LAYERS, 2, S5_GROUPS, S5_STATE, S5_GROUP_CH), (2 * S5_GROUP_CH) ** -0.5),
        's5_c_re': nrm((N_A_LAYERS, 2, S5_GROUPS, S5_GROUP_CH, S5_STATE), 0.5 ** 0.5),
        's5_c_im': nrm((N_A_LAYERS, 2, S5_GROUPS, S5_GROUP_CH, S5_STATE), 0.5 ** 0.5),
        's5_d': nrm((N_A_LAYERS, D), 1.0),
        's5_w_gate': nrm((N_A_LAYERS, D, D), D ** -0.5),
        's5_w_val': nrm((N_A_LAYERS, D, D), DEEPNORM_BETA * D ** -0.5),
        'swa_w_qkv': nrm((N_B_LAYERS, D, qkv_b), D ** -0.5),
        'swa_w_o': nrm((N_B_LAYERS, HEADS_B * HEAD_DIM_B, D), DEEPNORM_BETA * D ** -0.5),
        'swa_sink': nrm((N_B_LAYERS, HEADS_B), 1.0),
        'gqa_w_qkv': nrm((N_C_LAYERS, D, qkv_c), D ** -0.5),
        'gqa_w_o': nrm((N_C_LAYERS, HEADS_C * HEAD_DIM_C, D), DEEPNORM_BETA * D ** -0.5),
        'gqa_q_norm': 1.0 + nrm((N_C_LAYERS, HEAD_DIM_C), 0.02),
        'gqa_k_norm': 1.0 + nrm((N_C_LAYERS, HEAD_DIM_C), 0.02),
    }


def reference(x, c, ctx, c_ctx, mod_w, mod_b, ln_g, ln_b, router_w, router_b,
              moe_w1, moe_w3, moe_w2,
              s5_a_re, s5_a_im, s5_log_dt, s5_b_re, s5_b_im, s5_c_re, s5_c_im, s5_d, s5_w_gate, s5_w_val,
              swa_w_qkv, swa_w_o, swa_sink,
              gqa_w_qkv, gqa_w_o, gqa_q_norm, gqa_k_norm):
    Bsz, S, D = x.shape
    n_ctx = ctx.shape[1]
    n_rows = S // GRID_W
    cos_b, sin_b = _axial_rope_tables(n_rows, HEAD_DIM_B)
    cos_c, sin_c = _axial_rope_tables(n_rows, HEAD_DIM_C)
    for i in range(DEPTH):
        kind, j = i % N_MIXERS, i // N_MIXERS
        ctx_out = i < DEPTH - 1
        sh1, sc1, g1, sh2, sc2, g2 = [t[:, None, :] for t in _modulation(c, mod_w[i], mod_b[i])]
        csh1, csc1, cg1, csh2, csc2, cg2 = _modulation(c_ctx, mod_w[i], mod_b[i])
        h_lat = x * (1 + sc1) + sh1
        h_ctx = ctx * (1 + csc1) + csh1
        if kind == 0:
            y_lat, y_ctx = _s5_mixer(h_lat, h_ctx, s5_a_re[j], s5_a_im[j], s5_log_dt[j], s5_b_re[j], s5_b_im[j],
                                     s5_c_re[j], s5_c_im[j], s5_d[j], s5_w_gate[j], s5_w_val[j], ctx_out)
        elif kind == 1:
            y_lat, y_ctx = _windowed_gqa(h_lat, h_ctx, swa_w_qkv[j], swa_w_o[j], swa_sink[j], cos_b, sin_b, ctx_out)
        else:
            y_lat, y_ctx = _axial_gqa(h_lat, h_ctx, gqa_w_qkv[j], gqa_w_o[j], gqa_q_norm[j], gqa_k_norm[j],
                                      cos_c, sin_c, ctx_out)
        x = _layer_norm(DEEPNORM_ALPHA * x + g1 * y_lat, ln_g[i, 0], ln_b[i, 0])
        h_lat = x * (1 + sc2) + sh2
        if ctx_out:
            ctx = _layer_norm(DEEPNORM_ALPHA * ctx + cg1 * y_ctx, ln_g[i, 0], ln_b[i, 0])
            h_ctx = ctx * (1 + csc2) + csh2
            tokens = jnp.concatenate([h_ctx, h_lat], axis=1).reshape(-1, D)
            y = _moe(tokens, router_w, router_b, moe_w1[i], moe_w3[i], moe_w2[i]).reshape(Bsz, n_ctx + S, D)
            ctx = _layer_norm(DEEPNORM_ALPHA * ctx + cg2 * y[:, :n_ctx], ln_g[i, 1], ln_b[i, 1])
            y_lat = y[:, n_ctx:]
        else:
            y_lat = _moe(h_lat.reshape(-1, D), router_w, router_b, moe_w1[i], moe_w3[i], moe_w2[i]).reshape(Bsz, S, D)
        x = _layer_norm(DEEPNORM_ALPHA * x + g2 * y_lat, ln_g[i, 1], ln_b[i, 1])
    return x
```

```python
import numpy as np
from contextlib import ExitStack
import concourse.bass as bass
import concourse.mybir as mybir
from concourse.bass_utils import run_bass_kernel_spmd
import ml_dtypes

F32 = mybir.dt.float32
BF16 = mybir.dt.bfloat16
ALU = mybir.AluOpType
AF = mybir.ActivationFunctionType
AX = mybir.AxisListType
NPBF = ml_dtypes.bfloat16

EPOCH = 30000
NCORES = 8
D = 1024
SEQ = 16384
BATCH = 2
NCTX = 256
DEPTH = 4
NE = 16
DEXP = 512
ALPHA = (2.0 * DEPTH) ** 0.25
LN_EPS = 1e-6
BIG = 1.0e4
DEBUG = False


class Buf:
    def __init__(self, name, h, kind):
        self.name, self.h, self.kind = name, h, kind
        self.lw = None
        self.rd = []

    def __getitem__(self, idx):
        return self.h[idx]


class DSem:
    def __init__(self, h):
        self.h = h
        self.total = 0


class Prog:
    def __init__(self):
        self.nc = bass.Bass("TRN2", target_bir_lowering=False)
        self.es = ExitStack()
        nc = self.nc
        self.engs = {"pe": nc.tensor, "act": nc.scalar, "dve": nc.vector,
                     "pool": nc.gpsimd, "sp": nc.sync}
        self.esem = {e: [] for e in self.engs}
        self.ecnt = {e: 0 for e in self.engs}
        self.seen = {e: {} for e in self.engs}
        self.nsem = 0
        self.ninst = {e: 0 for e in self.engs}
        self.dsems = {}
        self.out_events = []
        self.uid = 0
        self.log = {e: [] for e in self.engs}
        for e in self.engs:
            self._new_epoch(e)

    def _sem(self, name):
        self.nsem += 1
        return self.es.enter_context(self.nc.semaphore(f"{name}_{self.nsem}"))

    def _new_epoch(self, e):
        self.esem[e].append(self._sem(f"s_{e}"))
        self.ecnt[e] = 0

    def sbuf(self, name, shape, dt):
        h = self.es.enter_context(self.nc.sbuf_tensor(name, list(shape), dt))
        return Buf(name, h, "sb")

    def psum(self, name, shape, dt=F32):
        h = self.es.enter_context(self.nc.psum_tensor(name, list(shape), dt))
        return Buf(name, h, "ps")

    def dram(self, name, shape, dt, kind="Internal"):
        t = self.nc.dram_tensor(name, list(shape), dt, kind=kind)
        return Buf(name, t.ap(), "dr")

    def dsem_for(self, buf):
        if buf.name not in self.dsems:
            self.dsems[buf.name] = DSem(self._sem("d_" + buf.name[:12]))
        return self.dsems[buf.name]

    def _wait(self, eng, ev):
        if ev is None:
            return
        if ev[0] == "e":
            _, e2, ep, cnt = ev
            key = ("e", e2, ep)
            sem = self.esem[e2][ep]
            val = cnt
        else:
            _, ds = ev
            key = ("d", id(ds))
            sem = ds.h
            val = ds.total
        if self.seen[eng].get(key, 0) >= val:
            return
        self.seen[eng][key] = val
        self.log[eng].append(("wait", key, val))
        self.engs[eng].wait_ge(sem, val)
        self.ninst[eng] += 1

    def _deps(self, eng, reads, writes):
        evs = []
        for b in reads:
            if b.lw is not None:
                evs.append(b.lw)
        for b in writes:
            if b.lw is not None:
                evs.append(b.lw)
            for ev in b.rd:
                if ev[0] == "e" and ev[1] == eng:
                    continue
                evs.append(ev)
        for ev in evs:
            if ev[0] == "e" and ev[1] == eng and eng == "pe":
                continue
            self._wait(eng, ev)

    def _record(self, ev, reads, writes):
        for b in reads:
            b.rd.append(ev)
            if len(b.rd) > 48:
                b.rd = b.rd[-48:]
        for b in writes:
            b.lw = ev
            b.rd = []

    def op(self, eng, reads, writes, fn, n_inst=1):
        self._deps(eng, reads, writes)
        if self.ecnt[eng] >= EPOCH:
            self._new_epoch(eng)
        ins = fn(self.engs[eng])
        self.ecnt[eng] += 1
        ep = len(self.esem[eng]) - 1
        ins.then_inc(self.esem[eng][ep], 1)
        self.log[eng].append(("inc", ("e", eng, ep), 1, False))
        ev = ("e", eng, ep, self.ecnt[eng])
        self.ninst[eng] += n_inst
        self._record(ev, reads, writes)
        return ev

    def dma(self, out_buf, out_ap, in_buf, in_ap, q="sp", sem_buf=None, **kw):
        if sem_buf is None:
            sem_buf = out_buf if out_buf.kind != "dr" else in_buf
        ds = self.dsem_for(sem_buf)
        self._deps(q, [in_buf], [out_buf])
        ins = self.engs[q].dma_start(out=out_ap, in_=in_ap, **kw)
        ds.total += 16
        ins.then_inc(ds.h, 16)
        self.log[q].append(("inc", ("d", id(ds)), 16, True))
        ev = ("d", ds)
        self.ninst[q] += 1
        self._record(ev, [in_buf], [out_buf])
        if out_buf.kind == "dr":
            self.out_events.append(ev)
        return ev

    def finish(self):
        for ev in self.out_events:
            self._wait("sp", ev)
        self.es.close()


def run_prog(P, in_maps):
    res = run_bass_kernel_spmd(P.nc, in_maps, core_ids=list(range(NCORES)))
    return res.results


def make_ident(P, name="ident", dt=F32):
    src = P.dram(name + "_in", [128, 128], dt, kind="ExternalInput")
    t = P.sbuf(name, [128, 128], dt)
    P.dma(t, t[:], src, src[:])
    return t


def layer_norm_tile(P, X, xap, gB, bB, tmp_stats, epst):
    st, mv, rstd = tmp_stats
    for h in range(2):
        P.op("dve", [X], [st], lambda e, h=h: e.bn_stats(out=st[:, h * 6:(h + 1) * 6],
                                                         in_=xap[:, h * 512:(h + 1) * 512]))
    P.op("dve", [st], [mv], lambda e: e.bn_aggr(out=mv[:], in_=st[:]))
    P.op("act", [mv, epst], [rstd], lambda e: e.activation(out=rstd[:, 0:1], in_=mv[:, 1:2], func=AF.Sqrt,
                                                           bias=epst[:, 0:1], scale=1.0))
    P.op("dve", [rstd], [rstd], lambda e: e.reciprocal(out=rstd[:, 1:2], in_=rstd[:, 0:1]))
    P.op("dve", [X, mv, rstd], [X], lambda e: e.tensor_scalar(out=xap, in0=xap, scalar1=mv[:, 0:1],
                                                              scalar2=rstd[:, 1:2], op0=ALU.subtract,
                                                              op1=ALU.mult))
    P.op("dve", [X, gB], [X], lambda e: e.tensor_tensor(out=xap, in0=xap, in1=gB[:], op=ALU.mult))
    P.op("dve", [X, bB], [X], lambda e: e.tensor_tensor(out=xap, in0=xap, in1=bB[:], op=ALU.add))


PREP_CH = 4096


def build_prep(ncols):
    P = Prog()
    w = P.dram("wflat", [128, ncols], F32, kind="ExternalInput")
    wo = P.dram("wbf", [128, ncols], BF16, kind="ExternalOutput")
    cT = P.dram("cT", [128, 8, 3], F32, kind="ExternalInput")
    mw = P.dram("modw", [DEPTH, D, 768], F32, kind="ExternalInput")
    mb = P.dram("modb", [DEPTH, 768], F32, kind="ExternalInput")
    mo = P.dram("modv", [DEPTH, 3, 768], F32, kind="ExternalOutput")
    nch = ncols // PREP_CH
    stg = [P.sbuf(f"stg{i}", [128, PREP_CH], F32) for i in range(3)]
    obf = [P.sbuf(f"obf{i}", [128, PREP_CH], BF16) for i in range(3)]
    cs = P.sbuf("cs", [128, 8, 3], F32)
    P.dma(cs, cs[:], cT, cT[:])
    ss = P.sbuf("ss", [128, 8, 3], F32)
    P.op("act", [cs], [ss], lambda e: e.activation(out=ss[:], in_=cs[:], func=AF.Silu))
    mws = [P.sbuf(f"mws{i}", [128, 8, 768], F32) for i in range(2)]
    mbs = P.sbuf("mbs", [3, DEPTH, 768], F32)
    for i in range(DEPTH):
        P.dma(mbs, mbs[:, i, :], mb, mb[i:i + 1, :].partition_broadcast(3))
    pm = [P.psum(f"pm{i}", [3, 512], F32) for i in range(4)]
    mres = P.sbuf("mres", [3, DEPTH, 768], F32)
    for i in range(DEPTH):
        ws = mws[i % 2]
        P.dma(ws, ws[:], mw, mw[i].rearrange("(kc p) n -> p kc n", p=128))
        for h in range(2):
            pp = pm[(2 * i + h) % 4]

            def mm(e, ws=ws, pp=pp, h=h):
                for kc in range(8):
                    ins = e.matmul(pp[:, 0:384], lhsT=ss[:, kc, :], rhs=ws[:, kc, h * 384:(h + 1) * 384],
                                   start=(kc == 0), stop=(kc == 7))
                return ins
            P.op("pe", [ss, ws], [pp], mm, n_inst=8)
            P.op("dve", [pp, mbs], [mres], lambda e, pp=pp, h=h, i=i: e.tensor_tensor(
                out=mres[:, i, h * 384:(h + 1) * 384], in0=pp[:, 0:384],
                in1=mbs[:, i, h * 384:(h + 1) * 384], op=ALU.add))
    P.dma(mo, mo[:].rearrange("l r n -> r l n"), mres, mres[:])
    engs = ["dve", "act"]

    def load(c):
        P.dma(stg[c % 3], stg[c % 3][:], w, w[:, c * PREP_CH:(c + 1) * PREP_CH])
    for c in range(min(2, nch)):
        load(c)
    for c in range(nch):
        s = stg[c % 3]
        o = obf[c % 3]
        if c + 2 < nch:
            load(c + 2)
        if engs[c % 2] == "act":
            P.op("act", [s], [o], lambda e, s=s, o=o: e.activation(out=o[:], in_=s[:], func=AF.Copy))
        else:
            P.op("dve", [s], [o], lambda e, s=s, o=o: e.tensor_copy(out=o[:], in_=s[:]))
        P.dma(wo, wo[:, c * PREP_CH:(c + 1) * PREP_CH], o, o[:])
    P.finish()
    return P


CAST_NAMES = ["moe_w1", "moe_w3", "moe_w2", "s5_w_gate", "s5_w_val", "swa_w_qkv", "swa_w_o",
              "gqa_w_qkv", "gqa_w_o", "router_w"]


def run_prep(inputs):
    flats = [np.ascontiguousarray(inputs[n], dtype=np.float32).reshape(-1) for n in CAST_NAMES]
    sizes = [f.size for f in flats]
    tot = sum(sizes)
    unit = NCORES * 128 * PREP_CH
    padded = ((tot + unit - 1) // unit) * unit
    flat = np.zeros(padded, np.float32)
    flat[:tot] = np.concatenate(flats)
    ncols = padded // (NCORES * 128)
    per = flat.reshape(NCORES, 128, ncols)
    c3 = np.concatenate([inputs["c"], inputs["c_ctx"][None, :]], 0).astype(np.float32)
    cT = np.ascontiguousarray(c3.reshape(3, 8, 128).transpose(2, 1, 0))
    P = build_prep(ncols)
    in_maps = []
    for c in range(NCORES):
        in_maps.append({
            "wflat": per[c], "cT": cT,
            "modw": np.ascontiguousarray(inputs["mod_w"][:, :, c * 768:(c + 1) * 768]),
            "modb": np.ascontiguousarray(inputs["mod_b"][:, c * 768:(c + 1) * 768]),
        })
    res = run_prog(P, in_maps)
    wbf = np.concatenate([np.asarray(r["wbf"]).reshape(-1) for r in res])[:tot]
    out = {}
    off = 0
    for n, sz in zip(CAST_NAMES, sizes):
        out[n] = wbf[off:off + sz].reshape(inputs[n].shape)
        off += sz
    modv = np.concatenate([np.asarray(r["modv"]) for r in res], axis=2)
    return out, modv


TG = 1024
NLAT = SEQ // 4


def build_tail(kind, ctx_out):
    P = Prog()
    ntok = NLAT + (NCTX if ctx_out else 0)
    groups = [(g * TG, TG, 0) for g in range(NLAT // TG)]
    if ctx_out:
        groups.append((NLAT, NCTX, 1))
    aT = P.dram("aT", [D, ntok], BF16, kind="ExternalInput")
    wp0 = P.dram("wp0", [D, D], BF16, kind="ExternalInput")
    wp1 = P.dram("wp1", [D, D], BF16, kind="ExternalInput") if kind == "s5" else None
    xin = P.dram("xin", [ntok, D], F32, kind="ExternalInput")
    modrows = P.dram("modrows", [2, 6 * D], F32, kind="ExternalInput")
    modcols = P.dram("modcols", [128, 2 * 6 * 8], F32, kind="ExternalInput")
    lng = P.dram("lng", [2, D], F32, kind="ExternalInput")
    lnb = P.dram("lnb", [2, D], F32, kind="ExternalInput")
    rw = P.dram("rw", [D, NE], BF16, kind="ExternalInput")
    rb = P.dram("rb", [1, NE], F32, kind="ExternalInput")
    w1 = P.dram("w1", [NE, D, DEXP], BF16, kind="ExternalInput")
    w3 = P.dram("w3", [NE, D, DEXP], BF16, kind="ExternalInput")
    w2 = P.dram("w2", [NE, DEXP, D], BF16, kind="ExternalInput")
    xout = P.dram("xout", [ntok, D], F32, kind="ExternalOutput")
    x1scr = P.dram("x1scr", [ntok, D], F32, kind="ExternalOutput" if DEBUG else "Internal")
    combo = P.dram("combo", [ntok, NE], F32, kind="ExternalOutput") if DEBUG else None

    ident = make_ident(P)
    epst = P.sbuf("epst", [128, 1], F32)
    P.op("dve", [], [epst], lambda e: e.memset(epst[:], LN_EPS))
    aTs = P.sbuf("aTs", [128, 8, TG], BF16)
    h2T = P.sbuf("h2T", [128, 8, TG], BF16)
    wps = [P.sbuf("wps0", [128, 8, D], BF16)]
    P.dma(wps[0], wps[0][:], wp0, wp0[:].rearrange("(kc p) n -> p kc n", p=128))
    if kind == "s5":
        wps.append(P.sbuf("wps1", [128, 8, D], BF16))
        P.dma(wps[1], wps[1][:], wp1, wp1[:].rearrange("(kc p) n -> p kc n", p=128))
    xb = [P.sbuf(f"xb{i}", [128, D], F32) for i in range(TG // 128)]
    w13b = [P.sbuf(f"w13b{i}", [128, 2, 8, DEXP], BF16) for i in range(2)]
    w2b = [P.sbuf(f"w2b{i}", [128, 4, D], BF16) for i in range(2)]
    gTb = [P.sbuf(f"gTb{i}", [128, 4, 512], BF16) for i in range(2)]
    silu_t = [P.sbuf(f"silu{i}", [128, 512], F32) for i in range(2)]
    tmpXs = [P.sbuf(f"tmpX{i}", [128, D], F32) for i in range(2)]
    tmpA = [P.sbuf(f"tmpA{i}", [128, 512], F32) for i in range(2)]
    sgt = [P.sbuf(f"sgt{i}", [128, 512], F32) for i in range(2)]
    g1B = P.sbuf("g1B", [128, D], F32)
    g2B = P.sbuf("g2B", [128, D], F32)
    lnGB = [P.sbuf(f"lnGB{i}", [128, D], F32) for i in range(2)]
    lnBB = [P.sbuf(f"lnBB{i}", [128, D], F32) for i in range(2)]
    mcols = P.sbuf("mcols", [128, 2, 6, 8], F32)
    P.dma(mcols, mcols[:].rearrange("p a b c -> p (a b c)"), modcols, modcols[:])
    P.op("dve", [mcols], [mcols], lambda e: e.tensor_scalar(out=mcols[:, :, 4, :], in0=mcols[:, :, 4, :],
                                                            scalar1=1.0, scalar2=None, op0=ALU.add))
    rws = P.sbuf("rws", [128, 8, NE], BF16)
    P.dma(rws, rws[:], rw, rw[:].rearrange("(kc p) n -> p kc n", p=128))
    rbB = P.sbuf("rbB", [128, NE], F32)
    P.dma(rbB, rbB[:], rb, rb[0:1, :].partition_broadcast(128))
    for i in range(2):
        P.dma(lnGB[i], lnGB[i][:], lng, lng[i:i + 1, :].partition_broadcast(128))
        P.dma(lnBB[i], lnBB[i][:], lnb, lnb[i:i + 1, :].partition_broadcast(128))
    comb = P.sbuf("comb", [128, TG // 128, NE], F32)
    rt = {n: P.sbuf("rt_" + n, [128, NE], F32) for n in
          ["aff", "sel", "eq1", "sel2", "masked", "e1", "masked2", "e2", "gate"]}
    rs = {n: P.sbuf("rs_" + n, [128, 4], F32) for n in ["m1", "m2", "gs", "gmask", "pen"]}
    r1 = {n: P.sbuf("r1_" + n, [128, 1], F32) for n in ["gm", "t1", "t2", "den", "rden"]}
    stats = (P.sbuf("st", [128, 12], F32), P.sbuf("mv", [128, 2], F32), P.sbuf("rstd", [128, 2], F32))
    pb = [P.psum(f"pb{i}", [128, 512], F32) for i in range(8)]

    jobs = [(gi, e) for gi in range(len(groups)) for e in range(NE)]
    wstate = {"next": 0}

    def prefetch_weights():
        j = wstate["next"]
        if j >= len(jobs):
            return
        _, e = jobs[j]
        b13, b2 = w13b[j % 2], w2b[j % 2]
        P.dma(b13, b13[:, 0], w1, w1[e].rearrange("(kc p) n -> p kc n", p=128))
        P.dma(b13, b13[:, 1], w3, w3[e].rearrange("(kc p) n -> p kc n", p=128))
        P.dma(b2, b2[:], w2, w2[e].rearrange("(mc p) n -> p mc n", p=128))
        wstate["next"] = j + 1

    prefetch_weights()
    prefetch_weights()
    cur_row = [-1]

    def load_row(r):
        if cur_row[0] == r:
            return
        cur_row[0] = r
        P.dma(g1B, g1B[:], modrows, modrows[r:r + 1, 2 * D:3 * D].partition_broadcast(128))
        P.dma(g2B, g2B[:], modrows, modrows[r:r + 1, 5 * D:6 * D].partition_broadcast(128))

    def topk(t, lp):
        A = rt
        P.op("act", [lp], [A["aff"]], lambda e: e.activation(out=A["aff"][:], in_=lp[:, 0:NE], func=AF.Sigmoid))
        P.op("dve", [A["aff"], rbB], [A["sel"]], lambda e: e.tensor_tensor(
            out=A["sel"][:], in0=A["aff"][:], in1=rbB[:], op=ALU.add))
        sel3 = A["sel"][:].rearrange("p (g k) -> p g k", k=4)
        P.op("dve", [A["sel"]], [rs["m1"]], lambda e: e.tensor_reduce(out=rs["m1"][:], in_=sel3, axis=AX.X, op=ALU.max))
        eq3 = A["eq1"][:].rearrange("p (g k) -> p g k", k=4)
        P.op("dve", [A["sel"], rs["m1"]], [A["eq1"]], lambda e: e.tensor_tensor(
            out=eq3, in0=sel3, in1=rs["m1"][:].unsqueeze(2).to_broadcast([128, 4, 4]), op=ALU.is_equal))
        P.op("dve", [A["eq1"], A["sel"]], [A["sel2"]], lambda e: e.scalar_tensor_tensor(
            out=A["sel2"][:], in0=A["eq1"][:], scalar=-BIG, in1=A["sel"][:], op0=ALU.mult, op1=ALU.add))
        P.op("dve", [A["sel2"]], [rs["m2"]], lambda e: e.tensor_reduce(
            out=rs["m2"][:], in_=A["sel2"][:].rearrange("p (g k) -> p g k", k=4), axis=AX.X, op=ALU.max))
        P.op("dve", [rs["m1"], rs["m2"]], [rs["gs"]], lambda e: e.tensor_tensor(
            out=rs["gs"][:], in0=rs["m1"][:], in1=rs["m2"][:], op=ALU.add))
        P.op("dve", [rs["gs"]], [r1["gm"]], lambda e: e.tensor_reduce(out=r1["gm"][:], in_=rs["gs"][:], axis=AX.X, op=ALU.max))
        P.op("dve", [rs["gs"], r1["gm"]], [rs["pen"]], lambda e: e.tensor_scalar(
            out=rs["pen"][:], in0=rs["gs"][:], scalar1=r1["gm"][:, 0:1], scalar2=-BIG, op0=ALU.is_lt, op1=ALU.mult))
        P.op("dve", [A["sel"], rs["pen"]], [A["masked"]], lambda e: e.tensor_tensor(
            out=A["masked"][:].rearrange("p (g k) -> p g k", k=4), in0=sel3,
            in1=rs["pen"][:].unsqueeze(2).to_broadcast([128, 4, 4]), op=ALU.add))
        P.op("dve", [A["masked"]], [r1["t1"]], lambda e: e.tensor_reduce(out=r1["t1"][:], in_=A["masked"][:], axis=AX.X, op=ALU.max))
        P.op("dve", [A["masked"], r1["t1"]], [A["e1"]], lambda e: e.tensor_scalar(
            out=A["e1"][:], in0=A["masked"][:], scalar1=r1["t1"][:, 0:1], scalar2=None, op0=ALU.is_equal))
        P.op("dve", [A["e1"], A["masked"]], [A["masked2"]], lambda e: e.scalar_tensor_tensor(
            out=A["masked2"][:], in0=A["e1"][:], scalar=-BIG, in1=A["masked"][:], op0=ALU.mult, op1=ALU.add))
        P.op("dve", [A["masked2"]], [r1["t2"]], lambda e: e.tensor_reduce(out=r1["t2"][:], in_=A["masked2"][:], axis=AX.X, op=ALU.max))
        P.op("dve", [A["masked2"], r1["t2"]], [A["e2"]], lambda e: e.tensor_scalar(
            out=A["e2"][:], in0=A["masked2"][:], scalar1=r1["t2"][:, 0:1], scalar2=None, op0=ALU.is_equal))
        P.op("dve", [A["e1"], A["e2"]], [A["e1"]], lambda e: e.tensor_tensor(
            out=A["e1"][:], in0=A["e1"][:], in1=A["e2"][:], op=ALU.add))
        P.op("dve", [A["e1"], A["aff"]], [A["gate"]], lambda e: e.tensor_tensor(
            out=A["gate"][:], in0=A["e1"][:], in1=A["aff"][:], op=ALU.mult))
        P.op("dve", [A["gate"]], [r1["den"]], lambda e: e.tensor_reduce(out=r1["den"][:], in_=A["gate"][:], axis=AX.X, op=ALU.add))
        P.op("dve", [r1["den"]], [r1["rden"]], lambda e: e.reciprocal(out=r1["rden"][:], in_=r1["den"][:]))
        P.op("dve", [A["gate"], r1["rden"]], [comb], lambda e: e.tensor_scalar(
            out=comb[:, t, :], in0=A["gate"][:], scalar1=r1["rden"][:, 0:1], scalar2=None, op0=ALU.mult))

    cnt = {"pbB": 0, "tmp": 0}

    for gi, (g0, tg, row) in enumerate(groups):
        nt = tg // 128
        load_row(row)
        P.dma(aTs, aTs[:, :, 0:tg], aT, aT[:, g0:g0 + tg].rearrange("(kc p) n -> p kc n", p=128))
        for t in range(nt):
            P.dma(xb[t], xb[t][:], xin, xin[g0 + t * 128:g0 + (t + 1) * 128, :])

        def phaseBC(t):
            xbuf = xb[t]
            xa = xbuf[:]
            for h in range(2):
                base = (cnt["pbB"] % 2) * 4
                cnt["pbB"] += 1
                tA = tmpA[cnt["tmp"] % 2]
                sg = sgt[cnt["tmp"] % 2]
                cnt["tmp"] += 1
                pv = pb[base + 0]

                def mm(e, pp, wsb):
                    for kc in range(8):
                        ins = e.matmul(pp[:], lhsT=aTs[:, kc, t * 128:(t + 1) * 128],
                                       rhs=wsb[:, kc, h * 512:(h + 1) * 512], start=(kc == 0), stop=(kc == 7))
                    return ins
                P.op("pe", [aTs, wps[0]], [pv], lambda e: mm(e, pv, wps[0]), n_inst=8)
                if kind == "s5":
                    pg = pb[base + 1]
                    P.op("pe", [aTs, wps[1]], [pg], lambda e: mm(e, pg, wps[1]), n_inst=8)
                    P.op("act", [pg], [sg], lambda e: e.activation(out=sg[:], in_=pg[:], func=AF.Sigmoid))
                    P.op("dve", [pv, sg], [tA], lambda e: e.tensor_tensor(out=tA[:], in0=pv[:], in1=sg[:], op=ALU.mult))
                    P.op("dve", [tA, g1B], [tA], lambda e: e.tensor_tensor(
                        out=tA[:], in0=tA[:], in1=g1B[:, h * 512:(h + 1) * 512], op=ALU.mult))
                else:
                    P.op("dve", [pv, g1B], [tA], lambda e: e.tensor_tensor(
                        out=tA[:], in0=pv[:], in1=g1B[:, h * 512:(h + 1) * 512], op=ALU.mult))
                P.op("dve", [xbuf, tA], [xbuf], lambda e: e.scalar_tensor_tensor(
                    out=xa[:, h * 512:(h + 1) * 512], in0=xa[:, h * 512:(h + 1) * 512], scalar=ALPHA,
                    in1=tA[:], op0=ALU.mult, op1=ALU.add))
            layer_norm_tile(P, xbuf, xa, lnGB[0], lnBB[0], stats, epst)
            P.dma(x1scr, x1scr[g0 + t * 128:g0 + (t + 1) * 128, :], xbuf, xa)

        def phaseDE(t):
            xbuf = xb[t]
            xa = xbuf[:]
            for half in range(2):
                pt = pb[2 + half] if True else None

                def tr(e, pt=pt, half=half):
                    for j in range(4):
                        kc = half * 4 + j
                        ins = e.transpose(pt[:, j * 128:(j + 1) * 128], xa[:, kc * 128:(kc + 1) * 128], ident[:])
                    return ins
                P.op("pe", [xbuf, ident], [pt], tr, n_inst=4)
                for j in range(4):
                    kc = half * 4 + j
                    P.op("act", [pt, mcols], [h2T], lambda e, kc=kc, j=j, pt=pt: e.activation(
                        out=h2T[:, kc, t * 128:(t + 1) * 128], in_=pt[:, j * 128:(j + 1) * 128],
                        func=AF.Identity, scale=mcols[:, row, 4, kc:kc + 1], bias=mcols[:, row, 3, kc:kc + 1]))
            lp = pb[6]

            def rmm(e):
                for kc in range(8):
                    ins = e.matmul(lp[:, 0:NE], lhsT=h2T[:, kc, t * 128:(t + 1) * 128], rhs=rws[:, kc, :],
                                   start=(kc == 0), stop=(kc == 7))
                return ins
            P.op("pe", [h2T, rws], [lp], rmm, n_inst=8)
            topk(t, lp)

        for t in range(nt + 1):
            if t < nt:
                phaseBC(t)
            if t >= 1:
                phaseDE(t - 1)

        if DEBUG:
            for t in range(nt):
                P.dma(combo, combo[g0 + t * 128:g0 + (t + 1) * 128, :], comb, comb[:, t, :])
        blocks = [(b0, min(512, tg - b0)) for b0 in range(0, tg, 512)]
        items = [(e, bi) for e in range(NE) for bi in range(len(blocks))]
        l1cnt = [0]

        def L1(idx):
            e, bi = items[idx]
            b0, bn = blocks[bi]
            j = gi * NE + e
            b13 = w13b[j % 2]
            gT = gTb[idx % 2]
            for m in range(4):
                k2 = l1cnt[0] % 2
                l1cnt[0] += 1
                p1, p3 = pb[k2 * 2], pb[k2 * 2 + 1]

                def mm(e_, pp, which):
                    for kc in range(8):
                        ins = e_.matmul(pp[:, 0:bn], lhsT=b13[:, which, kc, m * 128:(m + 1) * 128],
                                        rhs=h2T[:, kc, b0:b0 + bn], start=(kc == 0), stop=(kc == 7))
                    return ins
                P.op("pe", [b13, h2T], [p1], lambda e_: mm(e_, p1, 0), n_inst=8)
                P.op("pe", [b13, h2T], [p3], lambda e_: mm(e_, p3, 1), n_inst=8)
                sl = silu_t[k2]
                P.op("act", [p1], [sl], lambda e_: e_.activation(out=sl[:, 0:bn], in_=p1[:, 0:bn], func=AF.Silu))
                P.op("dve", [sl, p3], [gT], lambda e_: e_.tensor_tensor(
                    out=gT[:, m, 0:bn], in0=sl[:, 0:bn], in1=p3[:, 0:bn], op=ALU.mult))

        l2cnt = [0]

        def L2(idx):
            e, bi = items[idx]
            b0, bn = blocks[bi]
            j = gi * NE + e
            b2 = w2b[j % 2]
            gT = gTb[idx % 2]
            for s in range(bn // 128):
                t = (b0 // 128) + s
                for h in range(2):
                    po = pb[4 + (l2cnt[0] % 4)]
                    l2cnt[0] += 1

                    def mm(e_, po=po):
                        for m in range(4):
                            ins = e_.matmul(po[:], lhsT=gT[:, m, s * 128:(s + 1) * 128],
                                            rhs=b2[:, m, h * 512:(h + 1) * 512], start=(m == 0), stop=(m == 3))
                        return ins
                    P.op("pe", [gT, b2], [po], mm, n_inst=4)
                    xbuf = xb[t]
                    ya = xbuf[:, h * 512:(h + 1) * 512]
                    if e == 0:
                        P.op("dve", [po, comb], [xbuf], lambda e_, po=po, ya=ya, t=t: e_.tensor_scalar(
                            out=ya, in0=po[:], scalar1=comb[:, t, e:e + 1], scalar2=None, op0=ALU.mult))
                    else:
                        P.op("dve", [po, comb, xbuf], [xbuf], lambda e_, po=po, ya=ya, t=t: e_.scalar_tensor_tensor(
                            out=ya, in0=po[:], scalar=comb[:, t, e:e + 1], in1=ya, op0=ALU.mult, op1=ALU.add))

        L1(0)
        for idx in range(len(items)):
            if idx + 1 < len(items):
                L1(idx + 1)
            L2(idx)
            if items[idx][1] == len(blocks) - 1:
                prefetch_weights()

        def ld(t):
            P.dma(tmpXs[t % 2], tmpXs[t % 2][:], x1scr, x1scr[g0 + t * 128:g0 + (t + 1) * 128, :])
        ld(0)
        for t in range(nt):
            if t + 1 < nt:
                ld(t + 1)
            xbuf = xb[t]
            xa = xbuf[:]
            tmpX = tmpXs[t % 2]
            P.op("dve", [xbuf, g2B], [xbuf], lambda e: e.tensor_tensor(out=xa, in0=xa, in1=g2B[:], op=ALU.mult))
            P.op("dve", [tmpX, xbuf], [xbuf], lambda e: e.scalar_tensor_tensor(
                out=xa, in0=tmpX[:], scalar=ALPHA, in1=xa, op0=ALU.mult, op1=ALU.add))
            layer_norm_tile(P, xbuf, xa, lnGB[1], lnBB[1], stats, epst)
            P.dma(xout, xout[g0 + t * 128:g0 + (t + 1) * 128, :], xbuf, xa)
    P.finish()
    return P


RMS_EPS = 1e-6


def build_qkv(hd, H, KV, qknorm, ctx_rows):
    P = Prog()
    ntok = NLAT + ctx_rows
    nqkv = (H + 2 * KV) * hd
    xin = P.dram("xin", [ntok, D], F32, kind="ExternalInput")
    modcols = P.dram("modcols", [128, 2 * 6 * 8], F32, kind="ExternalInput")
    wq = P.dram("wqkv", [D, nqkv], BF16, kind="ExternalInput")
    cosd = P.dram("cos", [NLAT, hd // 2], F32, kind="ExternalInput")
    sind = P.dram("sin", [NLAT, hd // 2], F32, kind="ExternalInput")
    qTo = P.dram("qT", [hd, H, ntok], BF16, kind="ExternalOutput")
    kTo = P.dram("kT", [hd, KV, ntok], BF16, kind="ExternalOutput")
    vo = P.dram("v", [ntok, KV * hd], BF16, kind="ExternalOutput")
    if qknorm:
        qg = P.dram("qg", [1, hd], F32, kind="ExternalInput")
        kg = P.dram("kg", [1, hd], F32, kind="ExternalInput")
    ident = make_ident(P)
    mcols = P.sbuf("mcols", [128, 2, 6, 8], F32)
    P.dma(mcols, mcols[:].rearrange("p a b c -> p (a b c)"), modcols, modcols[:])
    P.op("dve", [mcols], [mcols], lambda e: e.tensor_scalar(out=mcols[:, :, 1, :], in0=mcols[:, :, 1, :],
                                                            scalar1=1.0, scalar2=None, op0=ALU.add))
    ws = P.sbuf("ws", [128, 8, nqkv], BF16)
    P.dma(ws, ws[:], wq, wq[:].rearrange("(kc p) n -> p kc n", p=128))
    epst = P.sbuf("epst", [128, 1], F32)
    P.op("dve", [], [epst], lambda e: e.memset(epst[:], RMS_EPS))
    if qknorm:
        qgB = P.sbuf("qgB", [128, hd], F32)
        kgB = P.sbuf("kgB", [128, hd], F32)
        P.dma(qgB, qgB[:], qg, qg[0:1, :].partition_broadcast(128))
        P.dma(kgB, kgB[:], kg, kg[0:1, :].partition_broadcast(128))
    xs = [P.sbuf(f"xs{i}", [128, D], F32) for i in range(2)]
    hT = [P.sbuf(f"hT{i}", [128, 8, 128], BF16) for i in range(2)]
    qkv = [P.sbuf(f"qkv{i}", [128, nqkv], F32) for i in range(2)]
    cs = [P.sbuf(f"cs{i}", [128, 2, hd // 2], F32) for i in range(2)]
    NH = H + KV
    ro = [P.sbuf(f"ro{i}", [128, NH * hd], F32) for i in range(2)]
    t1 = P.sbuf("t1", [128, NH * hd // 2], F32)
    t2 = P.sbuf("t2", [128, NH * hd // 2], F32)
    sq = P.sbuf("sq", [128, NH * hd], F32)
    ms = P.sbuf("ms", [128, NH], F32)
    rs = P.sbuf("rs", [128, NH], F32)
    vb = [P.sbuf(f"vb{i}", [128, KV * hd], BF16) for i in range(2)]
    qTs = [P.sbuf(f"qTs{i}", [hd, NH, 128], BF16) for i in range(2)]
    pb = [P.psum(f"pb{i}", [128, 512], F32) for i in range(8)]
    nt = ntok // 128
    pc = [0]
    for t in range(nt):
        row = 0 if t < NLAT // 128 else 1
        x_ = xs[t % 2]
        h_ = hT[t % 2]
        q_ = qkv[t % 2]
        P.dma(x_, x_[:], xin, xin[t * 128:(t + 1) * 128, :])
        if row == 0:
            c_ = cs[t % 2]
            P.dma(c_, c_[:, 0, :], cosd, cosd[t * 128:(t + 1) * 128, :])
            P.dma(c_, c_[:, 1, :], sind, sind[t * 128:(t + 1) * 128, :])
        for half in range(2):
            pt = pb[half]

            def tr(e, pt=pt, half=half):
                for j in range(4):
                    kc = half * 4 + j
                    ins = e.transpose(pt[:, j * 128:(j + 1) * 128], x_[:, kc * 128:(kc + 1) * 128], ident[:])
                return ins
            P.op("pe", [x_, ident], [pt], tr, n_inst=4)
            for j in range(4):
                kc = half * 4 + j
                P.op("act", [pt, mcols], [h_], lambda e, kc=kc, j=j, pt=pt: e.activation(
                    out=h_[:, kc, :], in_=pt[:, j * 128:(j + 1) * 128], func=AF.Identity,
                    scale=mcols[:, row, 1, kc:kc + 1], bias=mcols[:, row, 0, kc:kc + 1]))
        for c0 in range(0, nqkv, 512):
            cn = min(512, nqkv - c0)
            pp = pb[2 + (pc[0] % 2)]
            pc[0] += 1

            def mm(e, pp=pp, c0=c0, cn=cn):
                for kc in range(8):
                    ins = e.matmul(pp[:, 0:cn], lhsT=h_[:, kc, :], rhs=ws[:, kc, c0:c0 + cn],
                                   start=(kc == 0), stop=(kc == 7))
                return ins
            P.op("pe", [h_, ws], [pp], mm, n_inst=8)
            P.op("act", [pp], [q_], lambda e, pp=pp, c0=c0, cn=cn: e.activation(
                out=q_[:, c0:c0 + cn], in_=pp[:, 0:cn], func=AF.Copy))
        v_ = vb[t % 2]
        P.op("dve", [q_], [v_], lambda e: e.tensor_copy(out=v_[:], in_=q_[:, NH * hd:nqkv]))
        P.dma(vo, vo[t * 128:(t + 1) * 128, :], v_, v_[:])
        qk = q_[:, 0:NH * hd]
        if qknorm:
            P.op("dve", [q_], [sq], lambda e: e.tensor_tensor(out=sq[:], in0=qk, in1=qk, op=ALU.mult))
            P.op("dve", [sq], [ms], lambda e: e.tensor_reduce(
                out=ms[:], in_=sq[:].rearrange("p (h d) -> p h d", d=hd), axis=AX.X, op=ALU.add))
            P.op("act", [ms, epst], [rs], lambda e: e.activation(out=rs[:], in_=ms[:], func=AF.Sqrt,
                                                                 bias=epst[:, 0:1], scale=1.0 / hd))
            P.op("dve", [rs], [rs], lambda e: e.reciprocal(out=rs[:], in_=rs[:]))
            q3 = qk.rearrange("p (h d) -> p h d", d=hd)
            P.op("dve", [q_, rs], [q_], lambda e: e.tensor_tensor(
                out=q3, in0=q3, in1=rs[:].unsqueeze(2).to_broadcast([128, NH, hd]), op=ALU.mult))
            P.op("dve", [q_, qgB], [q_], lambda e: e.tensor_tensor(
                out=q3[:, 0:H, :], in0=q3[:, 0:H, :], in1=qgB[:].unsqueeze(1).to_broadcast([128, H, hd]), op=ALU.mult))
            P.op("dve", [q_, kgB], [q_], lambda e: e.tensor_tensor(
                out=q3[:, H:NH, :], in0=q3[:, H:NH, :], in1=kgB[:].unsqueeze(1).to_broadcast([128, KV, hd]), op=ALU.mult))
        r_ = ro[t % 2]
        if row == 0:
            qd = hd // 4
            x5 = qk.rearrange("p (h a two f) -> p h a two f", a=2, two=2, f=qd)
            o5 = r_[:].rearrange("p (h a two f) -> p h a two f", a=2, two=2, f=qd)
            x1, x2 = x5[:, :, :, 0, :], x5[:, :, :, 1, :]
            cB = c_[:, 0, :].rearrange("p (a f) -> p a f", f=qd).unsqueeze(1).to_broadcast([128, NH, 2, qd])
            sB = c_[:, 1, :].rearrange("p (a f) -> p a f", f=qd).unsqueeze(1).to_broadcast([128, NH, 2, qd])
            t1v = t1[:].rearrange("p (h a f) -> p h a f", a=2, f=qd)
            t2v = t2[:].rearrange("p (h a f) -> p h a f", a=2, f=qd)
            P.op("dve", [q_, c_], [t1], lambda e: e.tensor_tensor(out=t1v, in0=x1, in1=cB, op=ALU.mult))
            P.op("dve", [q_, c_], [t2], lambda e: e.tensor_tensor(out=t2v, in0=x2, in1=sB, op=ALU.mult))
            P.op("dve", [t1, t2], [r_], lambda e: e.tensor_tensor(out=o5[:, :, :, 0, :], in0=t1v, in1=t2v, op=ALU.subtract))
            P.op("dve", [q_, c_], [t1], lambda e: e.tensor_tensor(out=t1v, in0=x2, in1=cB, op=ALU.mult))
            P.op("dve", [q_, c_], [t2], lambda e: e.tensor_tensor(out=t2v, in0=x1, in1=sB, op=ALU.mult))
            P.op("dve", [t1, t2], [r_], lambda e: e.tensor_tensor(out=o5[:, :, :, 1, :], in0=t1v, in1=t2v, op=ALU.add))
            src, SRC = r_[:], r_
        else:
            src, SRC = qk, q_
        qt_ = qTs[t % 2]
        per = 512 // 128 if hd <= 128 else 1
        for h0 in range(0, NH, 4):
            hn = min(4, NH - h0)
            pt = pb[4 + ((h0 // 4) % 4)]

            def trq(e, pt=pt, h0=h0, hn=hn):
                for j in range(hn):
                    ins = e.transpose(pt[0:hd, j * 128:(j + 1) * 128], src[:, (h0 + j) * hd:(h0 + j + 1) * hd], ident[:])
                return ins
            P.op("pe", [SRC, ident], [pt], trq, n_inst=hn)
            P.op("act", [pt], [qt_], lambda e, pt=pt, h0=h0, hn=hn: e.activation(
                out=qt_[:, h0:h0 + hn, :], in_=pt[0:hd, 0:hn * 128].rearrange("p (h n) -> p h n", n=128), func=AF.Copy))
        P.dma(qTo, qTo[:, :, t * 128:(t + 1) * 128], qt_, qt_[:, 0:H, :])
        P.dma(kTo, kTo[:, :, t * 128:(t + 1) * 128], qt_, qt_[:, H:NH, :])
    P.finish()
    return P


def build_attn(hd, H, KV, NK, NQtot, qblocks, use_sink, nmask, maskw):
    P = Prog()
    R = H // KV
    scale = hd ** -0.5
    nkb = NK // 128
    qTd = P.dram("qT", [hd, H, NQtot], BF16, kind="ExternalInput")
    kTd = P.dram("kT", [hd, KV, NK], BF16, kind="ExternalInput")
    vd = P.dram("v", [NK, KV * hd], BF16, kind="ExternalInput")
    oTd = P.dram("oT", [H * hd, NQtot], BF16, kind="ExternalOutput")
    onesd = P.dram("ones_in", [128, 128], BF16, kind="ExternalInput")
    if nmask:
        maskd = P.dram("masks", [128, nmask, maskw], BF16, kind="ExternalInput")
        msk = P.sbuf("msk", [128, nmask, maskw], BF16)
        P.dma(msk, msk[:], maskd, maskd[:])
    if use_sink:
        sinkd = P.dram("sink", [1, H], F32, kind="ExternalInput")
        sinkB = P.sbuf("sinkB", [128, H], F32)
        P.dma(sinkB, sinkB[:], sinkd, sinkd[0:1, :].partition_broadcast(128))
        esink = P.sbuf("esink", [128, H], F32)
    ones = P.sbuf("ones", [128, 128], BF16)
    P.dma(ones, ones[:], onesd, onesd[:])
    onesf = P.sbuf("onesf", [1, 128], F32)
    P.op("dve", [], [onesf], lambda e: e.memset(onesf[:], 1.0))
    kT = P.sbuf("kT_s", [hd, KV, NK], BF16)
    for g in range(KV):
        P.dma(kT, kT[:, g, :], kTd, kTd[:, g, :])
    vs = P.sbuf("v_s", [128, nkb, KV * hd], BF16)
    P.dma(vs, vs[:], vd, vd[:].rearrange("(b p) n -> p b n", p=128))
    NQ = max(q[1] for q in qblocks)
    qTs = [P.sbuf(f"qTs{i}", [hd, H, NQ], BF16) for i in range(2)]
    sqb = P.sbuf("sqb", [hd, 512], BF16)
    pTs = [P.sbuf(f"pT{i}", [128, NQ], BF16) for i in range(3)]
    rden = [P.sbuf(f"rden{i}", [hd, NQ], F32) for i in range(2)]
    osb = [P.sbuf(f"osb{i}", [hd, NQ], BF16) for i in range(2)]
    kmax = P.sbuf("kmax", [1, 2], F32)
    qmax = P.sbuf("qmax", [1, 2], F32)
    cur = P.sbuf("curmx", [1, 1], F32)
    nshift = P.sbuf("nshift", [128, 1], F32)
    pS = [P.psum(f"pS{i}", [128, 512], F32) for i in range(3)]
    pO = [P.psum(f"pO{i}", [128, 512], F32) for i in range(2)]
    pD = [P.psum(f"pD{i}", [128, 512], F32) for i in range(2)]
    pX = P.psum("pX", [128, 512], F32)

    def max_sq_norm(SRC, src_ap_fn, ncols, dst):
        first = True
        for c0 in range(0, ncols, 512):
            cn = min(512, ncols - c0)
            ap = src_ap_fn(c0, cn)
            P.op("dve", [SRC], [sqb], lambda e, ap=ap, cn=cn: e.tensor_tensor(out=sqb[:, 0:cn], in0=ap, in1=ap, op=ALU.mult))
            P.op("pe", [sqb, ones], [pX], lambda e, cn=cn: e.matmul(pX[0:1, 0:cn], lhsT=ones[0:hd, 0:1], rhs=sqb[:, 0:cn],
                                                                    start=True, stop=True))
            if first:
                P.op("dve", [pX], [dst], lambda e, cn=cn: e.tensor_reduce(out=dst[:, 0:1], in_=pX[0:1, 0:cn], axis=AX.X, op=ALU.max))
                first = False
            else:
                P.op("dve", [pX], [cur], lambda e, cn=cn: e.tensor_reduce(out=cur[:, 0:1], in_=pX[0:1, 0:cn], axis=AX.X, op=ALU.max))
                P.op("dve", [cur, dst], [dst], lambda e: e.tensor_tensor(out=dst[:, 0:1], in0=dst[:, 0:1], in1=cur[:, 0:1], op=ALU.max))

    kflat = kT[:].rearrange("p g n -> p (g n)")
    max_sq_norm(kT, lambda c0, cn: kflat[:, c0:c0 + cn], KV * NK, kmax)

    cnt = {"s": 0, "p": 0, "o": 0}
    for bi, (q0, nq, klist) in enumerate(qblocks):
        qt = qTs[bi % 2]
        P.dma(qt, qt[:, :, 0:nq], qTd, qTd[:, :, q0:q0 + nq])
        for h in range(H):
            pass
        first = [True]

        def qsrc(c0, cn):
            return None
        for h in range(H):
            ap = qt[:, h, 0:nq]
            P.op("dve", [qt], [sqb], lambda e, ap=ap: e.tensor_tensor(out=sqb[:, 0:nq], in0=ap, in1=ap, op=ALU.mult))
            P.op("pe", [sqb, ones], [pX], lambda e: e.matmul(pX[0:1, 0:nq], lhsT=ones[0:hd, 0:1], rhs=sqb[:, 0:nq],
                                                             start=True, stop=True))
            if h == 0:
                P.op("dve", [pX], [qmax], lambda e: e.tensor_reduce(out=qmax[:, 0:1], in_=pX[0:1, 0:nq], axis=AX.X, op=ALU.max))
            else:
                P.op("dve", [pX], [cur], lambda e: e.tensor_reduce(out=cur[:, 0:1], in_=pX[0:1, 0:nq], axis=AX.X, op=ALU.max))
                P.op("dve", [cur, qmax], [qmax], lambda e: e.tensor_tensor(out=qmax[:, 0:1], in0=qmax[:, 0:1], in1=cur[:, 0:1], op=ALU.max))
        P.op("dve", [qmax, kmax], [qmax], lambda e: e.tensor_tensor(out=qmax[:, 1:2], in0=qmax[:, 0:1], in1=kmax[:, 0:1], op=ALU.mult))
        P.op("act", [qmax], [qmax], lambda e: e.activation(out=qmax[:, 1:2], in_=qmax[:, 1:2], func=AF.Sqrt))
        P.op("dve", [qmax], [qmax], lambda e: e.tensor_scalar(out=qmax[:, 1:2], in0=qmax[:, 1:2], scalar1=-scale, scalar2=None, op0=ALU.mult))
        P.op("pe", [onesf, qmax], [pX], lambda e: e.matmul(pX[:, 0:1], lhsT=onesf[0:1, :], rhs=qmax[0:1, 1:2], start=True, stop=True))
        P.op("dve", [pX], [nshift], lambda e: e.tensor_copy(out=nshift[:], in_=pX[:, 0:1]))
        if use_sink:
            P.op("act", [sinkB, nshift], [esink], lambda e: e.activation(out=esink[:], in_=sinkB[:], func=AF.Exp,
                                                                        bias=nshift[:, 0:1], scale=1.0))
        for h in range(H):
            g = h // R
            po = pO[cnt["o"] % 2]
            pd = pD[cnt["o"] % 2]
            rd = rden[cnt["o"] % 2]
            ob = osb[cnt["o"] % 2]
            cnt["o"] += 1
            nk = len(klist)
            sbuf_of = {}

            def S(i):
                kb, _ = klist[i]
                ps = pS[cnt["s"] % 3]
                cnt["s"] += 1
                sbuf_of[i] = ps
                P.op("pe", [kT, qt], [ps], lambda e: e.matmul(ps[:, 0:nq], lhsT=kT[:, g, kb * 128:(kb + 1) * 128],
                                                              rhs=qt[:, h, 0:nq], start=True, stop=True))

            def PV(i):
                kb, mid = klist[i]
                ps = sbuf_of.pop(i)
                pt = pTs[cnt["p"] % 3]
                cnt["p"] += 1
                P.op("act", [ps, nshift], [pt], lambda e: e.activation(out=pt[:, 0:nq], in_=ps[:, 0:nq], func=AF.Exp,
                                                                       bias=nshift[:, 0:1], scale=scale))
                if mid is not None:
                    P.op("dve", [pt, msk], [pt], lambda e: e.tensor_tensor(out=pt[:, 0:nq], in0=pt[:, 0:nq],
                                                                           in1=msk[:, mid, 0:nq], op=ALU.mult))
                P.op("pe", [vs, pt], [po], lambda e: e.matmul(po[0:hd, 0:nq], lhsT=vs[:, kb, g * hd:(g + 1) * hd],
                                                              rhs=pt[:, 0:nq], start=(i == 0), stop=(i == nk - 1)))
                P.op("pe", [ones, pt], [pd], lambda e: e.matmul(pd[0:hd, 0:nq], lhsT=ones[:, 0:hd],
                                                                rhs=pt[:, 0:nq], start=(i == 0), stop=(i == nk - 1)))
            S(0)
            for i in range(nk):
                if i + 1 < nk:
                    S(i + 1)
                PV(i)
            if use_sink:
                P.op("dve", [pd, esink], [rd], lambda e: e.tensor_scalar(out=rd[:, 0:nq], in0=pd[0:hd, 0:nq],
                                                                         scalar1=esink[0:hd, h:h + 1], scalar2=None, op0=ALU.add))
                P.op("dve", [rd], [rd], lambda e: e.reciprocal(out=rd[:, 0:nq], in_=rd[:, 0:nq]))
            else:
                P.op("dve", [pd], [rd], lambda e: e.reciprocal(out=rd[:, 0:nq], in_=pd[0:hd, 0:nq]))
            P.op("dve", [po, rd], [ob], lambda e: e.tensor_tensor(out=ob[:, 0:nq], in0=po[0:hd, 0:nq], in1=rd[:, 0:nq], op=ALU.mult))
            P.dma(oTd, oTd[h * hd:(h + 1) * hd, q0:q0 + nq], ob, ob[:, 0:nq])
    P.finish()
    return P


def swa_blocks(ctx_out):
    qb = []
    nb = NLAT // 128
    for i in range(nb):
        left = (i, 2 if i == 0 else 0)
        right = (i + 2, 3 if i == nb - 1 else 1)
        qb.append((i * 128, 128, [left, (i + 1, None), right, (34, None), (35, None)]))
    if ctx_out:
        for j in range(2):
            qb.append((NLAT + j * 128, 128, [(34, None), (35, None)]))
    return qb


def gqa_blocks(ctx_out):
    allk = [(kb, None) for kb in range(130)]
    qb = [(i * 512, 512, allk) for i in range(NLAT // 512)]
    if ctx_out:
        qb.append((NLAT, 256, [(128, None), (129, None)]))
    return qb


GRID_W = 64
ROPE_THETA = 10000.0
_PROGS = {}


def _prog(key, fn):
    if key not in _PROGS:
        _PROGS[key] = fn()
    return _PROGS[key]


def rope_tables(hd):
    quarter = hd // 4
    inv_freq = (ROPE_THETA ** (-np.arange(quarter, dtype=np.float32) / quarter)).astype(np.float32)
    n_rows = SEQ // GRID_W
    rows = np.repeat(np.arange(n_rows, dtype=np.float32), GRID_W)
    cols = np.tile(np.arange(GRID_W, dtype=np.float32), n_rows)
    ang = np.stack([rows[:, None] * inv_freq, cols[:, None] * inv_freq], axis=1)
    return (np.cos(ang).astype(np.float32).reshape(SEQ, hd // 2),
            np.sin(ang).astype(np.float32).reshape(SEQ, hd // 2))


def core_bq(c):
    return c // 4, c % 4


def modcols_for(modv, i, b):
    rows = np.stack([modv[i, b], modv[i, 2]])
    cols = np.ascontiguousarray(rows.reshape(2, 6, 8, 128).transpose(3, 0, 1, 2)).reshape(128, 96)
    return np.ascontiguousarray(rows), cols


IDENT = np.eye(128, dtype=np.float32)
ONES_BF = np.ones((128, 128), dtype=NPBF)


def tri_masks(c):
    b, q = core_bq(c)
    kl = np.arange(128)[:, None]
    ql = np.arange(128)[None, :]
    L = (kl >= ql).astype(np.float32)
    Rm = (kl <= ql).astype(np.float32)
    Le = L * (0.0 if q == 0 else 1.0)
    Re = Rm * (0.0 if q == 3 else 1.0)
    return np.ascontiguousarray(np.stack([L, Rm, Le, Re], axis=1)).astype(NPBF)


def run_tail(kind, ctx_out, i, aT_list, x, ctx, wp, wb, modv, inputs):
    P = _prog(("tail", kind, ctx_out), lambda: build_tail(kind, ctx_out))
    in_maps = []
    for c in range(NCORES):
        b, q = core_bq(c)
        rows, cols = modcols_for(modv, i, b)
        xin = x[b, q * NLAT:(q + 1) * NLAT]
        if ctx_out:
            xin = np.concatenate([xin, ctx[b]], 0)
        m = {"ident_in": IDENT, "aT": aT_list[c], "wp0": wp[0], "xin": np.ascontiguousarray(xin),
             "modrows": rows, "modcols": cols, "lng": inputs["ln_g"][i], "lnb": inputs["ln_b"][i],
             "rw": wb["router_w"], "rb": inputs["router_b"][None, :].astype(np.float32),
             "w1": wb["moe_w1"][i], "w3": wb["moe_w3"][i], "w2": wb["moe_w2"][i]}
        if kind == "s5":
            m["wp1"] = wp[1]
        in_maps.append(m)
    res = run_prog(P, in_maps)
    xn = np.empty_like(x)
    cn = np.array(ctx, copy=True)
    for c in range(NCORES):
        b, q = core_bq(c)
        o = np.asarray(res[c]["xout"])
        xn[b, q * NLAT:(q + 1) * NLAT] = o[:NLAT]
        if ctx_out and q == 0:
            cn[b] = o[NLAT:]
    return xn, cn


def run_attn_layer(i, which, ctx_out, x, ctx, wb, modv, inputs):
    if which == "swa":
        hd, H, KV = 64, 16, 2
        wqkv, wo = wb["swa_w_qkv"][0], wb["swa_w_o"][0]
    else:
        hd, H, KV = 128, 8, 2
        wqkv, wo = wb["gqa_w_qkv"][0], wb["gqa_w_o"][0]
    cos, sin = rope_tables(hd)
    Pq = _prog(("qkv", which), lambda: build_qkv(hd, H, KV, which == "gqa", NCTX))
    in_maps = []
    for c in range(NCORES):
        b, q = core_bq(c)
        _, cols = modcols_for(modv, i, b)
        m = {"ident_in": IDENT, "xin": np.ascontiguousarray(np.concatenate([x[b, q * NLAT:(q + 1) * NLAT], ctx[b]], 0)),
             "modcols": cols, "wqkv": wqkv, "cos": np.ascontiguousarray(cos[q * NLAT:(q + 1) * NLAT]),
             "sin": np.ascontiguousarray(sin[q * NLAT:(q + 1) * NLAT])}
        if which == "gqa":
            m["qg"] = inputs["gqa_q_norm"].astype(np.float32).reshape(1, hd)
            m["kg"] = inputs["gqa_k_norm"].astype(np.float32).reshape(1, hd)
        in_maps.append(m)
    rq = run_prog(Pq, in_maps)
    qT = [np.asarray(r["qT"]) for r in rq]
    kT = [np.asarray(r["kT"]) for r in rq]
    vv = [np.asarray(r["v"]) for r in rq]
    nq_tot = NLAT + (NCTX if ctx_out else 0)
    in_maps = []
    if which == "swa":
        Pa = _prog(("attn", which, ctx_out), lambda: build_attn(hd, H, KV, 4608, nq_tot, swa_blocks(ctx_out), True, 4, 128))
        for c in range(NCORES):
            b, q = core_bq(c)
            zk = np.zeros((hd, KV, 128), NPBF)
            zv = np.zeros((128, KV * hd), NPBF)
            kl = kT[c - 1][:, :, NLAT - 128:NLAT] if q > 0 else zk
            kr = kT[c + 1][:, :, 0:128] if q < 3 else zk
            vl = vv[c - 1][NLAT - 128:NLAT] if q > 0 else zv
            vr = vv[c + 1][0:128] if q < 3 else zv
            kk = np.concatenate([kl, kT[c][:, :, :NLAT], kr, kT[c][:, :, NLAT:]], axis=2)
            vk = np.concatenate([vl, vv[c][:NLAT], vr, vv[c][NLAT:]], axis=0)
            in_maps.append({"qT": np.ascontiguousarray(qT[c][:, :, :nq_tot]), "kT": np.ascontiguousarray(kk),
                            "v": np.ascontiguousarray(vk), "ones_in": ONES_BF, "masks": tri_masks(c),
                            "sink": inputs["swa_sink"].astype(np.float32).reshape(1, H)})
    else:
        Pa = _prog(("attn", which, ctx_out), lambda: build_attn(hd, H, KV, 16640, nq_tot, gqa_blocks(ctx_out), False, 0, 0))
        for c in range(NCORES):
            b, q = core_bq(c)
            cs = [4 * b + j for j in range(4)]
            kk = np.concatenate([kT[j][:, :, :NLAT] for j in cs] + [kT[c][:, :, NLAT:]], axis=2)
            vk = np.concatenate([vv[j][:NLAT] for j in cs] + [vv[c][NLAT:]], axis=0)
            in_maps.append({"qT": np.ascontiguousarray(qT[c][:, :, :nq_tot]), "kT": np.ascontiguousarray(kk),
                            "v": np.ascontiguousarray(vk), "ones_in": ONES_BF})
    ra = run_prog(Pa, in_maps)
    oT = [np.asarray(r["oT"]) for r in ra]
    return run_tail("attn", ctx_out, i, oT, x, ctx, [wo], wb, modv, inputs), (qT, kT, vv, oT)


NCH = 130
PI = float(np.pi)


def build_s5():
    P = Prog()
    ntk = NCH * 128
    xtok = P.dram("xtok", [2, ntk, 128], F32, kind="ExternalInput")
    xT = P.dram("xT", [128, 2 * ntk], F32, kind="ExternalInput")
    mrow = P.dram("mrow", [3, 2, 128], F32, kind="ExternalInput")
    mcol = P.dram("mcol", [128, 6], F32, kind="ExternalInput")
    ared = P.dram("are", [64, 16], F32, kind="ExternalInput")
    aimd = P.dram("aim", [64, 16], F32, kind="ExternalInput")
    ldtd = P.dram("ldt", [1, 16], F32, kind="ExternalInput")
    bred = P.dram("bre", [64, 256], F32, kind="ExternalInput")
    bimd = P.dram("bim", [64, 256], F32, kind="ExternalInput")
    cred = P.dram("cre", [64, 256], F32, kind="ExternalInput")
    cimd = P.dram("cim", [64, 256], F32, kind="ExternalInput")
    dskd = P.dram("dsk", [128, 1], F32, kind="ExternalInput")
    antid = P.dram("anti_in", [128, 128], BF16, kind="ExternalInput")
    gTo = P.dram("gT", [128, 2 * ntk], BF16, kind="ExternalOutput")
    hk = P.dram("hk", [8, 256, 256], BF16, kind="Internal")
    ident = make_ident(P)
    anti = P.sbuf("anti", [128, 128], BF16)
    P.dma(anti, anti[:], antid, antid[:])

    def sb(name, shape, dt=F32):
        return P.sbuf(name, shape, dt)

    def ld(name, src, shape, ap=None):
        t = sb(name, shape)
        P.dma(t, t[:], src, src[:] if ap is None else ap)
        return t

    def tt(out_b, out_ap, a_b, a_ap, b_b, b_ap, op):
        P.op("dve", [a_b, b_b], [out_b], lambda e: e.tensor_tensor(out=out_ap, in0=a_ap, in1=b_ap, op=op))

    are = ld("are_s", ared, [64, 16])
    aim = ld("aim_s", aimd, [64, 16])
    dt = sb("dt_s", [64, 16])
    P.dma(dt, dt[:], ldtd, ldtd[0:1, :].partition_broadcast(64))
    bre = ld("bre_s", bred, [64, 256])
    bim = ld("bim_s", bimd, [64, 256])
    cre = ld("cre_s", cred, [64, 256])
    cim = ld("cim_s", cimd, [64, 256])
    dsk = ld("dsk_s", dskd, [128, 1])
    mc = ld("mc_s", mcol, [128, 6])
    P.op("act", [dt], [dt], lambda e: e.activation(out=dt[:], in_=dt[:], func=AF.Exp))
    adr = sb("adr", [64, 16])
    adi = sb("adi", [64, 16])
    tt(adr, adr[:], are, are[:], dt, dt[:], ALU.mult)
    tt(adi, adi[:], aim, aim[:], dt, dt[:], ALU.mult)
    mag = sb("mag", [64, 16])
    P.op("act", [adr], [mag], lambda e: e.activation(out=mag[:], in_=adr[:], func=AF.Exp))
    rr = sb("rr", [64, 32])
    rm = sb("rm", [64, 32])
    P.op("dve", [adi], [rr], lambda e: e.tensor_copy(out=rr[:, 0:16], in_=adi[:]))
    P.op("dve", [adi], [rr], lambda e: e.tensor_scalar(out=rr[:, 16:32], in0=adi[:], scalar1=PI / 2, scalar2=None, op0=ALU.add))
    for _ in range(5):
        P.op("dve", [rr], [rm], lambda e: e.tensor_scalar(out=rm[:], in0=rr[:], scalar1=PI, scalar2=2 * PI,
                                                          op0=ALU.is_gt, op1=ALU.mult))
        tt(rr, rr[:], rr, rr[:], rm, rm[:], ALU.subtract)
    sc = sb("sincos", [64, 32])
    P.op("act", [rr], [sc], lambda e: e.activation(out=sc[:], in_=rr[:], func=AF.Sin))
    lre = sb("lre", [64, 16])
    lim = sb("lim", [64, 16])
    tt(lre, lre[:], mag, mag[:], sc, sc[:, 16:32], ALU.mult)
    tt(lim, lim[:], mag, mag[:], sc, sc[:, 0:16], ALU.mult)
    den = sb("den", [64, 16])
    tmp = sb("tmp16", [64, 16])
    tmp2 = sb("tmp16b", [64, 16])
    tt(den, den[:], are, are[:], are, are[:], ALU.mult)
    tt(tmp, tmp[:], aim, aim[:], aim, aim[:], ALU.mult)
    tt(den, den[:], den, den[:], tmp, tmp[:], ALU.add)
    P.op("dve", [den], [den], lambda e: e.reciprocal(out=den[:], in_=den[:]))
    nre = sb("nre", [64, 16])
    P.op("dve", [lre], [nre], lambda e: e.tensor_scalar(out=nre[:], in0=lre[:], scalar1=-1.0, scalar2=None, op0=ALU.add))
    fre = sb("fre", [64, 16])
    fim = sb("fim", [64, 16])
    tt(fre, fre[:], nre, nre[:], are, are[:], ALU.mult)
    tt(tmp, tmp[:], lim, lim[:], aim, aim[:], ALU.mult)
    tt(fre, fre[:], fre, fre[:], tmp, tmp[:], ALU.add)
    tt(fre, fre[:], fre, fre[:], den, den[:], ALU.mult)
    tt(fim, fim[:], lim, lim[:], are, are[:], ALU.mult)
    tt(tmp, tmp[:], nre, nre[:], aim, aim[:], ALU.mult)
    tt(fim, fim[:], fim, fim[:], tmp, tmp[:], ALU.subtract)
    tt(fim, fim[:], fim, fim[:], den, den[:], ALU.mult)

    def v3(t):
        return t[:].rearrange("p (a c) -> p a c", c=16)

    def bc3(t):
        return t[:].unsqueeze(2).to_broadcast([64, 16, 16])
    bbre = sb("bbre", [64, 256])
    bbim = sb("bbim", [64, 256])
    t256 = sb("t256", [64, 256])
    tt(bbre, v3(bbre), bre, v3(bre), fre, bc3(fre), ALU.mult)
    tt(t256, v3(t256), bim, v3(bim), fim, bc3(fim), ALU.mult)
    tt(bbre, bbre[:], bbre, bbre[:], t256, t256[:], ALU.subtract)
    tt(bbim, v3(bbim), bim, v3(bim), fre, bc3(fre), ALU.mult)
    tt(t256, v3(t256), bre, v3(bre), fim, bc3(fim), ALU.mult)
    tt(bbim, bbim[:], bbim, bbim[:], t256, t256[:], ALU.add)
    clre = sb("clre", [64, 256])
    clim = sb("clim", [64, 256])
    tt(clre, v3(clre), cre, v3(cre), lre, bc3(lre), ALU.mult)
    tt(t256, v3(t256), cim, v3(cim), lim, bc3(lim), ALU.mult)
    tt(clre, clre[:], clre, clre[:], t256, t256[:], ALU.subtract)
    tt(clim, v3(clim), cre, v3(cre), lim, bc3(lim), ALU.mult)
    tt(t256, v3(t256), cim, v3(cim), lre, bc3(lre), ALU.mult)
    tt(clim, clim[:], clim, clim[:], t256, t256[:], ALU.add)
    pw = sb("pw", [64, 8, 2, 16])
    P.op("dve", [lre], [pw], lambda e: e.tensor_copy(out=pw[:, 0, 0, :], in_=lre[:]))
    P.op("dve", [lim], [pw], lambda e: e.tensor_copy(out=pw[:, 0, 1, :], in_=lim[:]))
    for i in range(7):
        tt(tmp, tmp[:], pw, pw[:, i, 0, :], pw, pw[:, i, 0, :], ALU.mult)
        tt(tmp2, tmp2[:], pw, pw[:, i, 1, :], pw, pw[:, i, 1, :], ALU.mult)
        tt(pw, pw[:, i + 1, 0, :], tmp, tmp[:], tmp2, tmp2[:], ALU.subtract)
        tt(tmp, tmp[:], pw, pw[:, i, 0, :], pw, pw[:, i, 1, :], ALU.mult)
        P.op("dve", [tmp], [pw], lambda e, i=i: e.tensor_scalar(out=pw[:, i + 1, 1, :], in0=tmp[:], scalar1=2.0,
                                                                scalar2=None, op0=ALU.mult))
    tre = sb("tre", [64, 16, 128])
    tim = sb("tim", [64, 16, 128])
    ta = sb("ta", [64, 8, 64])
    tb = sb("tb", [64, 8, 64])
    P.op("dve", [], [tre], lambda e: e.memset(tre[:], 1.0))
    P.op("dve", [], [tim], lambda e: e.memset(tim[:], 0.0))
    for i in range(7):
        m = 1 << i
        for d in range(2):
            gs = slice(d * 8, d * 8 + 8)
            if d == 0:
                src, dst = slice(128 - m, 128), slice(128 - 2 * m, 128 - m)
            else:
                src, dst = slice(0, m), slice(m, 2 * m)
            pr = pw[:, i, 0, gs].unsqueeze(2).to_broadcast([64, 8, m])
            pi_ = pw[:, i, 1, gs].unsqueeze(2).to_broadcast([64, 8, m])
            tt(ta, ta[:, :, 0:m], tre, tre[:, gs, src], pw, pr, ALU.mult)
            tt(tb, tb[:, :, 0:m], tim, tim[:, gs, src], pw, pi_, ALU.mult)
            tt(tre, tre[:, gs, dst], ta, ta[:, :, 0:m], tb, tb[:, :, 0:m], ALU.subtract)
            tt(ta, ta[:, :, 0:m], tre, tre[:, gs, src], pw, pi_, ALU.mult)
            tt(tb, tb[:, :, 0:m], tim, tim[:, gs, src], pw, pr, ALU.mult)
            tt(tim, tim[:, gs, dst], ta, ta[:, :, 0:m], tb, tb[:, :, 0:m], ALU.add)
    treb = sb("treb", [64, 16, 128], BF16)
    timnb = sb("timnb", [64, 16, 128], BF16)
    P.op("dve", [tre], [treb], lambda e: e.tensor_copy(out=treb[:], in_=tre[:]))
    P.op("dve", [tim], [timnb], lambda e: e.tensor_scalar(out=timnb[:], in0=tim[:], scalar1=-1.0, scalar2=None, op0=ALU.mult))
    pb = [P.psum(f"pb{i}", [128, 512], F32) for i in range(8)]
    cbre = sb("cbre", [64, 256], BF16)
    cbim = sb("cbim", [64, 256], BF16)
    cbt1 = sb("cbt1", [64, 256])
    cbt2 = sb("cbt2", [64, 256])
    hks = [sb(f"hks{i}", [128, 256], BF16) for i in range(2)]
    hcnt = [0]
    for g in range(8):
        pss = {}
        for d in range(2):
            dg = d * 8 + g
            cB = lambda t: t[:, dg * 16:(dg + 1) * 16].unsqueeze(2).to_broadcast([64, 16, 16])
            bB = lambda t: t[:, dg * 16:(dg + 1) * 16].unsqueeze(1).to_broadcast([64, 16, 16])
            o1 = cbt1[:].rearrange("p (c k) -> p c k", k=16)
            o2 = cbt2[:].rearrange("p (c k) -> p c k", k=16)
            tt(cbt1, o1, cre, cB(cre), bbre, bB(bbre), ALU.mult)
            tt(cbt2, o2, cim, cB(cim), bbim, bB(bbim), ALU.mult)
            tt(cbre, cbre[:], cbt1, cbt1[:], cbt2, cbt2[:], ALU.subtract)
            tt(cbt1, o1, cre, cB(cre), bbim, bB(bbim), ALU.mult)
            tt(cbt2, o2, cim, cB(cim), bbre, bB(bbre), ALU.mult)
            tt(cbim, cbim[:], cbt1, cbt1[:], cbt2, cbt2[:], ALU.add)
            for half in range(2):
                ps = pb[d * 2 + half]

                def mm(e, ps=ps, half=half, dg=dg):
                    e.matmul(ps[:, 0:128], lhsT=cbre[:, half * 128:(half + 1) * 128], rhs=treb[:, dg, :], start=True, stop=False)
                    return e.matmul(ps[:, 0:128], lhsT=cbim[:, half * 128:(half + 1) * 128], rhs=timnb[:, dg, :], start=False, stop=True)
                P.op("pe", [cbre, cbim, treb, timnb], [ps], mm, n_inst=2)
                pss[(d, half)] = ps
        for half in range(2):
            hs = hks[hcnt[0] % 2]
            hcnt[0] += 1
            pa, pbb = pss[(0, half)], pss[(1, half)]
            P.op("act", [pa], [hs], lambda e: e.activation(out=hs[:, 0:127], in_=pa[:, 0:127], func=AF.Copy))
            P.op("act", [pbb], [hs], lambda e: e.activation(out=hs[:, 128:255], in_=pbb[:, 1:128], func=AF.Copy))
            P.op("act", [pa], [cbt1], lambda e: e.activation(out=cbt1[0:64, 0:1], in_=pa[0:64, 127:128], func=AF.Copy))
            t128 = sb(f"t128_{g}_{half}", [128, 2])
            P.op("act", [pa], [t128], lambda e: e.activation(out=t128[:, 0:1], in_=pa[:, 127:128], func=AF.Copy))
            P.op("dve", [pbb, t128], [hs], lambda e: e.tensor_tensor(out=hs[:, 127:128], in0=pbb[:, 0:1], in1=t128[:, 0:1], op=ALU.add))
            P.op("dve", [], [hs], lambda e: e.memset(hs[:, 255:256], 0.0))
            P.dma(hk, hk[g, half * 128:(half + 1) * 128, :], hs, hs[:])
    U = sb("U", [128, NCH, 128], BF16)
    Ysb = sb("Ysb", [128, NCH, 128], BF16)
    xl = [sb(f"xl{i}", [128, 8, 128]) for i in range(2)]
    scB = sb("scB", [128, 2, 128])
    Sst = sb("Sst", [64, 16, 2, NCH])
    Hin = sb("Hin", [64, 16, 2, NCH], BF16)
    Hs = sb("Hs", [64, 2, 8])
    hn = sb("hn", [64, 2, 8])
    h1 = sb("h1", [64, 8])
    h2 = sb("h2", [64, 8])
    scr = [sb(f"scr{i}", [64, 1024]) for i in range(3)]
    dg3v = [t[:].rearrange("p (c q) -> p c q", q=64) for t in scr]
    WinS = [sb(f"WinS{i}", [128, 16, 2, 64], BF16) for i in range(2)]
    Wo = sb("Wo", [64, 2, 2, 16, 128], BF16)
    HK = [sb(f"HK{i}", [128, 16, 128], BF16) for i in range(2)]
    xTb = [sb(f"xTb{i}", [128, 512]) for i in range(2)]
    zv = [sb(f"zv{i}", [128, 512]) for i in range(2)]
    gv = [sb(f"gv{i}", [128, 512], BF16) for i in range(2)]
    AB = sb("AB", [128, 3, 2])
    for r in range(3):
        P.op("dve", [mc, dsk], [AB], lambda e, r=r: e.tensor_scalar(out=AB[:, r, 0:1], in0=mc[:, 2 * r + 1:2 * r + 2], scalar1=1.0,
                                                                    scalar2=dsk[:, 0:1], op0=ALU.add, op1=ALU.mult))
        P.op("dve", [mc, dsk], [AB], lambda e, r=r: e.tensor_scalar(out=AB[:, r, 1:2], in0=mc[:, 2 * r:2 * r + 1], scalar1=dsk[:, 0:1],
                                                                    scalar2=None, op0=ALU.mult))
    ident64 = ident[0:64, 0:64]
    pcnt = [0]

    def nextpb():
        pcnt[0] += 1
        return pb[pcnt[0] % 8]

    for b in range(2):
        def load_rowmod(r):
            P.dma(scB, scB[:, 0, :], mrow, mrow[r:r + 1, 0, :].partition_broadcast(128))
            P.dma(scB, scB[:, 1, :], mrow, mrow[r:r + 1, 1, :].partition_broadcast(128))
            P.op("dve", [scB], [scB], lambda e: e.tensor_scalar(out=scB[:, 1, :], in0=scB[:, 1, :], scalar1=1.0, scalar2=None, op0=ALU.add))
        li = [0]

        def do_chunks(k0, kn):
            x_ = xl[li[0] % 2]
            li[0] += 1
            P.dma(x_, x_[:, 0:kn, :], xtok, xtok[b, k0 * 128:(k0 + kn) * 128, :].rearrange("(k p) c -> p k c", p=128))
            tt(x_, x_[:, 0:kn, :], x_, x_[:, 0:kn, :], scB, scB[:, 1, :].unsqueeze(1).to_broadcast([128, kn, 128]), ALU.mult)
            tt(U, U[:, k0:k0 + kn, :], x_, x_[:, 0:kn, :], scB, scB[:, 0, :].unsqueeze(1).to_broadcast([128, kn, 128]), ALU.add)
        load_rowmod(2)
        do_chunks(0, 2)
        load_rowmod(b)
        for k0 in range(2, NCH, 8):
            do_chunks(k0, 8)
        for dg in range(16):
            g = dg % 8
            W = WinS[dg % 2]
            idb = ident64.unsqueeze(1).to_broadcast([64, 16, 64])
            bsl = lambda t: t[:, dg * 16:(dg + 1) * 16].unsqueeze(2).to_broadcast([64, 16, 64])
            tt(scr[0], dg3v[0], ident, idb, bbre, bsl(bbre), ALU.mult)
            tt(scr[1], dg3v[1], ident, idb, bbim, bsl(bbim), ALU.mult)
            P.op("dve", [scr[1]], [scr[2]], lambda e: e.tensor_scalar(out=scr[2][:], in0=scr[1][:], scalar1=-1.0, scalar2=None, op0=ALU.mult))
            for hc in range(2):
                for ri in range(2):
                    ps = nextpb()
                    r1 = scr[0] if ri == 0 else scr[1]
                    r2 = scr[2] if ri == 0 else scr[0]

                    def mm(e, ps=ps, r1=r1, r2=r2, hc=hc, dg=dg):
                        e.matmul(ps[:, 0:512], lhsT=tre[:, dg, :], rhs=r1[:, hc * 512:(hc + 1) * 512],
                                 start=True, stop=False)
                        return e.matmul(ps[:, 0:512], lhsT=tim[:, dg, :], rhs=r2[:, hc * 512:(hc + 1) * 512],
                                        start=False, stop=True)
                    P.op("pe", [tre, tim, r1, r2], [ps], mm, n_inst=2)
                    P.op("act", [ps], [W], lambda e, ps=ps, hc=hc, ri=ri, W=W: e.activation(
                        out=W[:, hc * 8:(hc + 1) * 8, ri, :], in_=ps[:, 0:512].rearrange("p (c q) -> p c q", q=64), func=AF.Copy))
            for ri in range(2):
                ps = nextpb()

                def mm2(e, ps=ps, ri=ri, W=W, g=g):
                    for c_ in range(16):
                        ins = e.matmul(ps[0:64, 0:NCH], lhsT=W[:, c_, ri, :], rhs=U[:, :, g * 16 + c_],
                                       start=(c_ == 0), stop=(c_ == 15))
                    return ins
                P.op("pe", [W, U], [ps], mm2, n_inst=16)
                P.op("act", [ps], [Sst], lambda e, ps=ps, ri=ri, dg=dg: e.activation(out=Sst[:, dg, ri, :], in_=ps[0:64, 0:NCH], func=AF.Copy))
        for d in range(2):
            gs = slice(d * 8, d * 8 + 8)
            order = list(range(NCH)) if d == 0 else [1, 0] + list(range(NCH - 1, 1, -1))
            lr, li_ = pw[:, 7, 0, gs], pw[:, 7, 1, gs]
            P.op("dve", [], [Hs], lambda e: e.memset(Hs[:], 0.0))
            for k in order:
                P.op("act", [Hs], [Hin], lambda e, k=k: e.activation(out=Hin[:, gs, :, k].rearrange("p g r -> p r g"), in_=Hs[:], func=AF.Copy))
                tt(h1, h1[:], Hs, Hs[:, 0, :], pw, lr, ALU.mult)
                tt(h2, h2[:], Hs, Hs[:, 1, :], pw, li_, ALU.mult)
                tt(h1, h1[:], h1, h1[:], h2, h2[:], ALU.subtract)
                tt(hn, hn[:, 0, :], h1, h1[:], Sst, Sst[:, gs, 0, k], ALU.add)
                tt(h1, h1[:], Hs, Hs[:, 0, :], pw, li_, ALU.mult)
                tt(h2, h2[:], Hs, Hs[:, 1, :], pw, lr, ALU.mult)
                tt(h1, h1[:], h1, h1[:], h2, h2[:], ALU.add)
                tt(hn, hn[:, 1, :], h1, h1[:], Sst, Sst[:, gs, 1, k], ALU.add)
                P.op("dve", [hn], [Hs], lambda e: e.tensor_copy(out=Hs[:], in_=hn[:]))
        hkc = [0]
        for g in range(8):
            for d in range(2):
                dg = d * 8 + g
                w1v = scr[0][:].rearrange("p (c t) -> p c t", t=128)
                w2v = scr[1][:].rearrange("p (c t) -> p c t", t=128)
                for hw in range(2):
                    cB = lambda t: t[:, dg * 16 + hw * 8:dg * 16 + hw * 8 + 8].unsqueeze(2).to_broadcast([64, 8, 128])
                    tB = lambda t: t[:, dg, :].unsqueeze(1).to_broadcast([64, 8, 128])
                    cs_ = slice(hw * 8, hw * 8 + 8)
                    tt(scr[0], w1v, clre, cB(clre), tre, tB(tre), ALU.mult)
                    tt(scr[1], w2v, clim, cB(clim), tim, tB(tim), ALU.mult)
                    tt(Wo, Wo[:, d, 0, cs_, :], scr[0], w1v, scr[1], w2v, ALU.subtract)
                    tt(scr[0], w1v, clre, cB(clre), tim, tB(tim), ALU.mult)
                    tt(scr[1], w2v, clim, cB(clim), tre, tB(tre), ALU.mult)
                    P.op("dve", [scr[0], scr[1]], [Wo], lambda e, d=d, cs_=cs_: e.scalar_tensor_tensor(
                        out=Wo[:, d, 1, cs_, :], in0=w1v, scalar=-1.0, in1=w2v, op0=ALU.mult, op1=ALU.subtract))
            for c_ in range(16):
                Hk_ = HK[hkc[0] % 2]
                hkc[0] += 1
                base = hk[g, c_ * 16, 0]
                src = bass.AP(hk.h.tensor, (g * 256 + c_ * 16) * 256, [[1, 128], [256, 16], [1, 128]])
                P.dma(Hk_, Hk_[:], hk, src)
                ps = nextpb()

                def mm3(e, ps=ps, Hk_=Hk_, g=g, c_=c_):
                    for k_ in range(16):
                        e.matmul(ps[:, 0:NCH], lhsT=Hk_[:, k_, :], rhs=U[:, :, g * 16 + k_], start=(k_ == 0), stop=False)
                    for d in range(2):
                        dg = d * 8 + g
                        e.matmul(ps[:, 0:NCH], lhsT=Wo[:, d, 0, c_, :], rhs=Hin[:, dg, 0, :], start=False, stop=False)
                        ins = e.matmul(ps[:, 0:NCH], lhsT=Wo[:, d, 1, c_, :], rhs=Hin[:, dg, 1, :], start=False, stop=(d == 1))
                    return ins
                P.op("pe", [Hk_, U, Wo, Hin], [ps], mm3, n_inst=20)
                P.op("act", [ps], [Ysb], lambda e, ps=ps, g=g, c_=c_: e.activation(out=Ysb[:, :, g * 16 + c_], in_=ps[:, 0:NCH], func=AF.Copy))
        blocks = [(0, 2, 2)] + [(k0, 4, b) for k0 in range(2, NCH, 4)]
        for bi, (k0, kn, r) in enumerate(blocks):
            nt_ = kn * 128
            col0 = b * ntk + k0 * 128
            xt_ = xTb[bi % 2]
            P.dma(xt_, xt_[:, 0:nt_], xT, xT[:, col0:col0 + nt_])
            ps = nextpb()

            def trj(e, ps=ps, k0=k0, kn=kn):
                for j in range(kn):
                    ins = e.matmul(ps[:, j * 128:(j + 1) * 128], lhsT=Ysb[:, k0 + j, :], rhs=anti[:], start=True, stop=True)
                return ins
            P.op("pe", [Ysb, anti], [ps], trj, n_inst=kn)
            z_, g_ = zv[bi % 2], gv[bi % 2]
            y_ = xt_
            P.op("act", [xt_, AB], [xt_], lambda e, r=r: e.activation(out=xt_[:, 0:nt_], in_=xt_[:, 0:nt_], func=AF.Identity,
                                                                      scale=AB[:, r, 0:1], bias=AB[:, r, 1:2]))
            tt(y_, y_[:, 0:nt_], ps, ps[:, 0:nt_], xt_, xt_[:, 0:nt_], ALU.add)
            tt(z_, z_[:, 0:nt_], y_, y_[:, 0:nt_], y_, y_[:, 0:nt_], ALU.mult)
            P.op("dve", [z_], [z_], lambda e: e.tensor_scalar(out=z_[:, 0:nt_], in0=z_[:, 0:nt_], scalar1=0.044715, scalar2=1.0,
                                                              op0=ALU.mult, op1=ALU.add))
            tt(z_, z_[:, 0:nt_], z_, z_[:, 0:nt_], y_, y_[:, 0:nt_], ALU.mult)
            P.op("act", [z_], [z_], lambda e: e.activation(out=z_[:, 0:nt_], in_=z_[:, 0:nt_], func=AF.Sigmoid, scale=1.5957691216057308))
            tt(g_, g_[:, 0:nt_], y_, y_[:, 0:nt_], z_, z_[:, 0:nt_], ALU.mult)
            P.dma(gTo, gTo[:, col0:col0 + nt_], g_, g_[:, 0:nt_])
    P.finish()
    return P


ANTI_BF = np.ascontiguousarray(np.eye(128, dtype=np.float32)[::-1]).astype(NPBF)


def run_s5_layer(i, j, ctx_out, x, ctx, wb, modv, inputs):
    P = _prog(("s5",), build_s5)
    in_maps = []
    for c in range(NCORES):
        ch = slice(128 * c, 128 * c + 128)
        gsl = slice(8 * c, 8 * c + 8)
        xtok = np.ascontiguousarray(np.concatenate([ctx[:, :, ch], x[:, :, ch]], axis=1))
        xT = np.ascontiguousarray(xtok.transpose(2, 0, 1).reshape(128, -1))
        mrow = np.ascontiguousarray(np.stack([modv[i, :, 0 * D:1 * D][:, ch], modv[i, :, 1 * D:2 * D][:, ch]], axis=1))
        mcol = np.ascontiguousarray(mrow.transpose(2, 0, 1).reshape(128, 6))

        def pg(a):
            return np.ascontiguousarray(a[:, gsl, :].transpose(2, 0, 1).reshape(64, 16)).astype(np.float32)
        bre = np.ascontiguousarray(inputs["s5_b_re"][j][:, gsl].transpose(2, 0, 1, 3).reshape(64, 256)).astype(np.float32)
        bim = np.ascontiguousarray(inputs["s5_b_im"][j][:, gsl].transpose(2, 0, 1, 3).reshape(64, 256)).astype(np.float32)
        cre = np.ascontiguousarray(inputs["s5_c_re"][j][:, gsl].transpose(3, 0, 1, 2).reshape(64, 256)).astype(np.float32)
        cim = np.ascontiguousarray(inputs["s5_c_im"][j][:, gsl].transpose(3, 0, 1, 2).reshape(64, 256)).astype(np.float32)
        in_maps.append({"ident_in": IDENT, "anti_in": ANTI_BF, "xtok": xtok, "xT": xT, "mrow": mrow, "mcol": mcol,
                        "are": pg(inputs["s5_a_re"][j]), "aim": pg(inputs["s5_a_im"][j]),
                        "ldt": np.ascontiguousarray(inputs["s5_log_dt"][j][:, gsl].reshape(1, 16)).astype(np.float32),
                        "bre": bre, "bim": bim, "cre": cre, "cim": cim,
                        "dsk": np.ascontiguousarray(inputs["s5_d"][j][ch].reshape(128, 1)).astype(np.float32)})
    res = run_prog(P, in_maps)
    gT = np.concatenate([np.asarray(r["gT"]) for r in res], axis=0)
    ntk = NCH * 128
    aT = []
    for c in range(NCORES):
        b, q = core_bq(c)
        lat = gT[:, b * ntk + NCTX + q * NLAT: b * ntk + NCTX + (q + 1) * NLAT]
        if ctx_out:
            lat = np.concatenate([lat, gT[:, b * ntk: b * ntk + NCTX]], axis=1)
        aT.append(np.ascontiguousarray(lat))
    return run_tail("s5", ctx_out, i, aT, x, ctx, [wb["s5_w_val"][j], wb["s5_w_gate"][j]], wb, modv, inputs), gT


def kernel(**inputs):
    inputs = {k: np.asarray(v) for k, v in inputs.items()}
    wb, modv = run_prep(inputs)
    x = np.ascontiguousarray(inputs["x"], dtype=np.float32)
    ctx = np.ascontiguousarray(inputs["ctx"], dtype=np.float32)
    (x, ctx), _ = run_s5_layer(0, 0, True, x, ctx, wb, modv, inputs)
    (x, ctx), _ = run_attn_layer(1, "swa", True, x, ctx, wb, modv, inputs)
    (x, ctx), _ = run_attn_layer(2, "gqa", True, x, ctx, wb, modv, inputs)
    (x, ctx), _ = run_s5_layer(3, 1, False, x, ctx, wb, modv, inputs)
    return np.ascontiguousarray(x, dtype=np.float32)
```

```python
import numpy as np
from contextlib import ExitStack
import concourse.bass as bass
import concourse.mybir as mybir
from concourse.bass_utils import run_bass_kernel_spmd
import ml_dtypes

F32 = mybir.dt.float32
BF16 = mybir.dt.bfloat16
ALU = mybir.AluOpType
AF = mybir.ActivationFunctionType
AX = mybir.AxisListType
NPBF = ml_dtypes.bfloat16

EPOCH = 30000
NCORES = 8
D = 1024
SEQ = 16384
BATCH = 2
NCTX = 256
DEPTH = 4
NE = 16
DEXP = 512
ALPHA = (2.0 * DEPTH) ** 0.25
LN_EPS = 1e-6
BIG = 1.0e4
DEBUG = False


class Buf:
    def __init__(self, name, h, kind):
        self.name, self.h, self.kind = name, h, kind
        self.lw = None
        self.rd = []

    def __getitem__(self, idx):
        return self.h[idx]


class DSem:
    def __init__(self, h):
        self.h = h
        self.total = 0


class Prog:
    def __init__(self):
        self.nc = bass.Bass("TRN2", target_bir_lowering=False)
        self.es = ExitStack()
        nc = self.nc
        self.engs = {"pe": nc.tensor, "act": nc.scalar, "dve": nc.vector,
                     "pool": nc.gpsimd, "sp": nc.sync}
        self.esem = {e: [] for e in self.engs}
        self.ecnt = {e: 0 for e in self.engs}
        self.seen = {e: {} for e in self.engs}
        self.nsem = 0
        self.ninst = {e: 0 for e in self.engs}
        self.dsems = {}
        self.out_events = []
        self.uid = 0
        self.log = {e: [] for e in self.engs}
        for e in self.engs:
            self._new_epoch(e)

    def _sem(self, name):
        self.nsem += 1
        return self.es.enter_context(self.nc.semaphore(f"{name}_{self.nsem}"))

    def _new_epoch(self, e):
        self.esem[e].append(self._sem(f"s_{e}"))
        self.ecnt[e] = 0

    def sbuf(self, name, shape, dt):
        h = self.es.enter_context(self.nc.sbuf_tensor(name, list(shape), dt))
        return Buf(name, h, "sb")

    def psum(self, name, shape, dt=F32):
        h = self.es.enter_context(self.nc.psum_tensor(name, list(shape), dt))
        return Buf(name, h, "ps")

    def dram(self, name, shape, dt, kind="Internal"):
        t = self.nc.dram_tensor(name, list(shape), dt, kind=kind)
        return Buf(name, t.ap(), "dr")

    def dsem_for(self, buf):
        if buf.name not in self.dsems:
            self.dsems[buf.name] = DSem(self._sem("d_" + buf.name[:12]))
        return self.dsems[buf.name]

    def _wait(self, eng, ev):
        if ev is None:
            return
        if ev[0] == "e":
            _, e2, ep, cnt = ev
            key = ("e", e2, ep)
            sem = self.esem[e2][ep]
            val = cnt
        else:
            _, ds = ev
            key = ("d", id(ds))
            sem = ds.h
            val = ds.total
        if self.seen[eng].get(key, 0) >= val:
            return
        self.seen[eng][key] = val
        self.log[eng].append(("wait", key, val))
        self.engs[eng].wait_ge(sem, val)
        self.ninst[eng] += 1

    def _deps(self, eng, reads, writes):
        evs = []
        for b in reads:
            if b.lw is not None:
                evs.append(b.lw)
        for b in writes:
            if b.lw is not None:
                evs.append(b.lw)
            for ev in b.rd:
                if ev[0] == "e" and ev[1] == eng:
                    continue
                evs.append(ev)
        for ev in evs:
            if ev[0] == "e" and ev[1] == eng and eng == "pe":
                continue
            self._wait(eng, ev)

    def _record(self, ev, reads, writes):
        for b in reads:
            b.rd.append(ev)
            if len(b.rd) > 48:
                b.rd = b.rd[-48:]
        for b in writes:
            b.lw = ev
            b.rd = []

    def op(self, eng, reads, writes, fn, n_inst=1):
        self._deps(eng, reads, writes)
        if self.ecnt[eng] >= EPOCH:
            self._new_epoch(eng)
        ins = fn(self.engs[eng])
        self.ecnt[eng] += 1
        ep = len(self.esem[eng]) - 1
        ins.then_inc(self.esem[eng][ep], 1)
        self.log[eng].append(("inc", ("e", eng, ep), 1, False))
        ev = ("e", eng, ep, self.ecnt[eng])
        self.ninst[eng] += n_inst
        self._record(ev, reads, writes)
        return ev

    def dma(self, out_buf, out_ap, in_buf, in_ap, q="sp", sem_buf=None, **kw):
        if sem_buf is None:
            sem_buf = out_buf if out_buf.kind != "dr" else in_buf
        ds = self.dsem_for(sem_buf)
        self._deps(q, [in_buf], [out_buf])
        ins = self.engs[q].dma_start(out=out_ap, in_=in_ap, **kw)
        ds.total += 16
        ins.then_inc(ds.h, 16)
        self.log[q].append(("inc", ("d", id(ds)), 16, True))
        ev = ("d", ds)
        self.ninst[q] += 1
        self._record(ev, [in_buf], [out_buf])
        if out_buf.kind == "dr":
            self.out_events.append(ev)
        return ev

    def finish(self):
        for ev in self.out_events:
            self._wait("sp", ev)
        self.es.close()


def run_prog(P, in_maps):
    res = run_bass_kernel_spmd(P.nc, in_maps, core_ids=list(range(NCORES)))
    return res.results


def make_ident(P, name="ident", dt=F32):
    src = P.dram(name + "_in", [128, 128], dt, kind="ExternalInput")
    t = P.sbuf(name, [128, 128], dt)
    P.dma(t, t[:], src, src[:])
    return t


def layer_norm_tile(P, X, xap, gB, bB, tmp_stats, epst):
    st, mv, rstd = tmp_stats
    for h in range(2):
        P.op("dve", [X], [st], lambda e, h=h: e.bn_stats(out=st[:, h * 6:(h + 1) * 6],
                                                         in_=xap[:, h * 512:(h + 1) * 512]))
    P.op("dve", [st], [mv], lambda e: e.bn_aggr(out=mv[:], in_=st[:]))
    P.op("act", [mv, epst], [rstd], lambda e: e.activation(out=rstd[:, 0:1], in_=mv[:, 1:2], func=AF.Sqrt,
                                                           bias=epst[:, 0:1], scale=1.0))
    P.op("dve", [rstd], [rstd], lambda e: e.reciprocal(out=rstd[:, 1:2], in_=rstd[:, 0:1]))
    P.op("dve", [X, mv, rstd], [X], lambda e: e.tensor_scalar(out=xap, in0=xap, scalar1=mv[:, 0:1],
                                                              scalar2=rstd[:, 1:2], op0=ALU.subtract,
                                                              op1=ALU.mult))
    P.op("dve", [X, gB], [X], lambda e: e.tensor_tensor(out=xap, in0=xap, in1=gB[:], op=ALU.mult))
    P.op("dve", [X, bB], [X], lambda e: e.tensor_tensor(out=xap, in0=xap, in1=bB[:], op=ALU.add))


PREP_CH = 4096


def build_prep(ncols):
    P = Prog()
    w = P.dram("wflat", [128, ncols], F32, kind="ExternalInput")
    wo = P.dram("wbf", [128, ncols], BF16, kind="ExternalOutput")
    cT = P.dram("cT", [128, 8, 3], F32, kind="ExternalInput")
    mw = P.dram("modw", [DEPTH, D, 768], F32, kind="ExternalInput")
    mb = P.dram("modb", [DEPTH, 768], F32, kind="ExternalInput")
    mo = P.dram("modv", [DEPTH, 3, 768], F32, kind="ExternalOutput")
    nch = ncols // PREP_CH
    stg = [P.sbuf(f"stg{i}", [128, PREP_CH], F32) for i in range(3)]
    obf = [P.sbuf(f"obf{i}", [128, PREP_CH], BF16) for i in range(3)]
    cs = P.sbuf("cs", [128, 8, 3], F32)
    P.dma(cs, cs[:], cT, cT[:])
    ss = P.sbuf("ss", [128, 8, 3], F32)
    P.op("act", [cs], [ss], lambda e: e.activation(out=ss[:], in_=cs[:], func=AF.Silu))
    mws = [P.sbuf(f"mws{i}", [128, 8, 768], F32) for i in range(2)]
    mbs = P.sbuf("mbs", [3, DEPTH, 768], F32)
    for i in range(DEPTH):
        P.dma(mbs, mbs[:, i, :], mb, mb[i:i + 1, :].partition_broadcast(3))
    pm = [P.psum(f"pm{i}", [3, 512], F32) for i in range(4)]
    mres = P.sbuf("mres", [3, DEPTH, 768], F32)
    for i in range(DEPTH):
        ws = mws[i % 2]
        P.dma(ws, ws[:], mw, mw[i].rearrange("(kc p) n -> p kc n", p=128))
        for h in range(2):
            pp = pm[(2 * i + h) % 4]

            def mm(e, ws=ws, pp=pp, h=h):
                for kc in range(8):
                    ins = e.matmul(pp[:, 0:384], lhsT=ss[:, kc, :], rhs=ws[:, kc, h * 384:(h + 1) * 384],
                                   start=(kc == 0), stop=(kc == 7))
                return ins
            P.op("pe", [ss, ws], [pp], mm, n_inst=8)
            P.op("dve", [pp, mbs], [mres], lambda e, pp=pp, h=h, i=i: e.tensor_tensor(
                out=mres[:, i, h * 384:(h + 1) * 384], in0=pp[:, 0:384],
                in1=mbs[:, i, h * 384:(h + 1) * 384], op=ALU.add))
    P.dma(mo, mo[:].rearrange("l r n -> r l n"), mres, mres[:])
    engs = ["dve", "act"]

    def load(c):
        P.dma(stg[c % 3], stg[c % 3][:], w, w[:, c * PREP_CH:(c + 1) * PREP_CH])
    for c in range(min(2, nch)):
        load(c)
    for c in range(nch):
        s = stg[c % 3]
        o = obf[c % 3]
        if c + 2 < nch:
            load(c + 2)
        if engs[c % 2] == "act":
            P.op("act", [s], [o], lambda e, s=s, o=o: e.activation(out=o[:], in_=s[:], func=AF.Copy))
        else:
            P.op("dve", [s], [o], lambda e, s=s, o=o: e.tensor_copy(out=o[:], in_=s[:]))
        P.dma(wo, wo[:, c * PREP_CH:(c + 1) * PREP_CH], o, o[:])
    P.finish()
    return P


CAST_NAMES = ["moe_w1", "moe_w3", "moe_w2", "s5_w_gate", "s5_w_val", "swa_w_qkv", "swa_w_o",
              "gqa_w_qkv", "gqa_w_o", "router_w"]


def run_prep(inputs):
    flats = [np.ascontiguousarray(inputs[n], dtype=np.float32).reshape(-1) for n in CAST_NAMES]
    sizes = [f.size for f in flats]
    tot = sum(sizes)
    unit = NCORES * 128 * PREP_CH
    padded = ((tot + unit - 1) // unit) * unit
    flat = np.zeros(padded, np.float32)
    flat[:tot] = np.concatenate(flats)
    ncols = padded // (NCORES * 128)
    per = flat.reshape(NCORES, 128, ncols)
    c3 = np.concatenate([inputs["c"], inputs["c_ctx"][None, :]], 0).astype(np.float32)
    cT = np.ascontiguousarray(c3.reshape(3, 8, 128).transpose(2, 1, 0))
    P = build_prep(ncols)
    in_maps = []
    for c in range(NCORES):
        in_maps.append({
            "wflat": per[c], "cT": cT,
            "modw": np.ascontiguousarray(inputs["mod_w"][:, :, c * 768:(c + 1) * 768]),
            "modb": np.ascontiguousarray(inputs["mod_b"][:, c * 768:(c + 1) * 768]),
        })
    res = run_prog(P, in_maps)
    wbf = np.concatenate([np.asarray(r["wbf"]).reshape(-1) for r in res])[:tot]
    out = {}
    off = 0
    for n, sz in zip(CAST_NAMES, sizes):
        out[n] = wbf[off:off + sz].reshape(inputs[n].shape)
        off += sz
    modv = np.concatenate([np.asarray(r["modv"]) for r in res], axis=2)
    return out, modv


TG = 1024
NLAT = SEQ // 4


def build_tail(kind, ctx_out):
    P = Prog()
    ntok = NLAT + (NCTX if ctx_out else 0)
    groups = [(g * TG, TG, 0) for g in range(NLAT // TG)]
    if ctx_out:
        groups.append((NLAT, NCTX, 1))
    aT = P.dram("aT", [D, ntok], BF16, kind="ExternalInput")
    wp0 = P.dram("wp0", [D, D], BF16, kind="ExternalInput")
    wp1 = P.dram("wp1", [D, D], BF16, kind="ExternalInput") if kind == "s5" else None
    xin = P.dram("xin", [ntok, D], F32, kind="ExternalInput")
    modrows = P.dram("modrows", [2, 6 * D], F32, kind="ExternalInput")
    modcols = P.dram("modcols", [128, 2 * 6 * 8], F32, kind="ExternalInput")
    lng = P.dram("lng", [2, D], F32, kind="ExternalInput")
    lnb = P.dram("lnb", [2, D], F32, kind="ExternalInput")
    rw = P.dram("rw", [D, NE], BF16, kind="ExternalInput")
    rb = P.dram("rb", [1, NE], F32, kind="ExternalInput")
    w1 = P.dram("w1", [NE, D, DEXP], BF16, kind="ExternalInput")
    w3 = P.dram("w3", [NE, D, DEXP], BF16, kind="ExternalInput")
    w2 = P.dram("w2", [NE, DEXP, D], BF16, kind="ExternalInput")
    xout = P.dram("xout", [ntok, D], F32, kind="ExternalOutput")
    x1scr = P.dram("x1scr", [ntok, D], F32, kind="ExternalOutput" if DEBUG else "Internal")
    combo = P.dram("combo", [ntok, NE], F32, kind="ExternalOutput") if DEBUG else None

    ident = make_ident(P)
    epst = P.sbuf("epst", [128, 1], F32)
    P.op("dve", [], [epst], lambda e: e.memset(epst[:], LN_EPS))
    aTs = P.sbuf("aTs", [128, 8, TG], BF16)
    h2T = P.sbuf("h2T", [128, 8, TG], BF16)
    wps = [P.sbuf("wps0", [128, 8, D], BF16)]
    P.dma(wps[0], wps[0][:], wp0, wp0[:].rearrange("(kc p) n -> p kc n", p=128))
    if kind == "s5":
        wps.append(P.sbuf("wps1", [128, 8, D], BF16))
        P.dma(wps[1], wps[1][:], wp1, wp1[:].rearrange("(kc p) n -> p kc n", p=128))
    xb = [P.sbuf(f"xb{i}", [128, D], F32) for i in range(TG // 128)]
    w13b = [P.sbuf(f"w13b{i}", [128, 2, 8, DEXP], BF16) for i in range(2)]
    w2b = [P.sbuf(f"w2b{i}", [128, 4, D], BF16) for i in range(2)]
    gTb = [P.sbuf(f"gTb{i}", [128, 4, 512], BF16) for i in range(2)]
    silu_t = [P.sbuf(f"silu{i}", [128, 512], F32) for i in range(2)]
    tmpXs = [P.sbuf(f"tmpX{i}", [128, D], F32) for i in range(2)]
    tmpA = [P.sbuf(f"tmpA{i}", [128, 512], F32) for i in range(2)]
    sgt = [P.sbuf(f"sgt{i}", [128, 512], F32) for i in range(2)]
    g1B = P.sbuf("g1B", [128, D], F32)
    g2B = P.sbuf("g2B", [128, D], F32)
    lnGB = [P.sbuf(f"lnGB{i}", [128, D], F32) for i in range(2)]
    lnBB = [P.sbuf(f"lnBB{i}", [128, D], F32) for i in range(2)]
    mcols = P.sbuf("mcols", [128, 2, 6, 8], F32)
    P.dma(mcols, mcols[:].rearrange("p a b c -> p (a b c)"), modcols, modcols[:])
    P.op("dve", [mcols], [mcols], lambda e: e.tensor_scalar(out=mcols[:, :, 4, :], in0=mcols[:, :, 4, :],
                                                            scalar1=1.0, scalar2=None, op0=ALU.add))
    rws = P.sbuf("rws", [128, 8, NE], BF16)
    P.dma(rws, rws[:], rw, rw[:].rearrange("(kc p) n -> p kc n", p=128))
    rbB = P.sbuf("rbB", [128, NE], F32)
    P.dma(rbB, rbB[:], rb, rb[0:1, :].partition_broadcast(128))
    for i in range(2):
        P.dma(lnGB[i], lnGB[i][:], lng, lng[i:i + 1, :].partition_broadcast(128))
        P.dma(lnBB[i], lnBB[i][:], lnb, lnb[i:i + 1, :].partition_broadcast(128))
    comb = P.sbuf("comb", [128, TG // 128, NE], F32)
    rt = {n: P.sbuf("rt_" + n, [128, NE], F32) for n in
          ["aff", "sel", "eq1", "sel2", "masked", "e1", "masked2", "e2", "gate"]}
    rs = {n: P.sbuf("rs_" + n, [128, 4], F32) for n in ["m1", "m2", "gs", "gmask", "pen"]}
    r1 = {n: P.sbuf("r1_" + n, [128, 1], F32) for n in ["gm", "t1", "t2", "den", "rden"]}
    stats = (P.sbuf("st", [128, 12], F32), P.sbuf("mv", [128, 2], F32), P.sbuf("rstd", [128, 2], F32))
    pb = [P.psum(f"pb{i}", [128, 512], F32) for i in range(8)]

    jobs = [(gi, e) for gi in range(len(groups)) for e in range(NE)]
    wstate = {"next": 0}

    def prefetch_weights():
        j = wstate["next"]
        if j >= len(jobs):
            return
        _, e = jobs[j]
        b13, b2 = w13b[j % 2], w2b[j % 2]
        P.dma(b13, b13[:, 0], w1, w1[e].rearrange("(kc p) n -> p kc n", p=128))
        P.dma(b13, b13[:, 1], w3, w3[e].rearrange("(kc p) n -> p kc n", p=128))
        P.dma(b2, b2[:], w2, w2[e].rearrange("(mc p) n -> p mc n", p=128))
        wstate["next"] = j + 1

    prefetch_weights()
    prefetch_weights()
    cur_row = [-1]

    def load_row(r):
        if cur_row[0] == r:
            return
        cur_row[0] = r
        P.dma(g1B, g1B[:], modrows, modrows[r:r + 1, 2 * D:3 * D].partition_broadcast(128))
        P.dma(g2B, g2B[:], modrows, modrows[r:r + 1, 5 * D:6 * D].partition_broadcast(128))

    def topk(t, lp):
        A = rt
        P.op("act", [lp], [A["aff"]], lambda e: e.activation(out=A["aff"][:], in_=lp[:, 0:NE], func=AF.Sigmoid))
        P.op("dve", [A["aff"], rbB], [A["sel"]], lambda e: e.tensor_tensor(
            out=A["sel"][:], in0=A["aff"][:], in1=rbB[:], op=ALU.add))
        sel3 = A["sel"][:].rearrange("p (g k) -> p g k", k=4)
        P.op("dve", [A["sel"]], [rs["m1"]], lambda e: e.tensor_reduce(out=rs["m1"][:], in_=sel3, axis=AX.X, op=ALU.max))
        eq3 = A["eq1"][:].rearrange("p (g k) -> p g k", k=4)
        P.op("dve", [A["sel"], rs["m1"]], [A["eq1"]], lambda e: e.tensor_tensor(
            out=eq3, in0=sel3, in1=rs["m1"][:].unsqueeze(2).to_broadcast([128, 4, 4]), op=ALU.is_equal))
        P.op("dve", [A["eq1"], A["sel"]], [A["sel2"]], lambda e: e.scalar_tensor_tensor(
            out=A["sel2"][:], in0=A["eq1"][:], scalar=-BIG, in1=A["sel"][:], op0=ALU.mult, op1=ALU.add))
        P.op("dve", [A["sel2"]], [rs["m2"]], lambda e: e.tensor_reduce(
            out=rs["m2"][:], in_=A["sel2"][:].rearrange("p (g k) -> p g k", k=4), axis=AX.X, op=ALU.max))
        P.op("dve", [rs["m1"], rs["m2"]], [rs["gs"]], lambda e: e.tensor_tensor(
            out=rs["gs"][:], in0=rs["m1"][:], in1=rs["m2"][:], op=ALU.add))
        P.op("dve", [rs["gs"]], [r1["gm"]], lambda e: e.tensor_reduce(out=r1["gm"][:], in_=rs["gs"][:], axis=AX.X, op=ALU.max))
        P.op("dve", [rs["gs"], r1["gm"]], [rs["pen"]], lambda e: e.tensor_scalar(
            out=rs["pen"][:], in0=rs["gs"][:], scalar1=r1["gm"][:, 0:1], scalar2=-BIG, op0=ALU.is_lt, op1=ALU.mult))
        P.op("dve", [A["sel"], rs["pen"]], [A["masked"]], lambda e: e.tensor_tensor(
            out=A["masked"][:].rearrange("p (g k) -> p g k", k=4), in0=sel3,
            in1=rs["pen"][:].unsqueeze(2).to_broadcast([128, 4, 4]), op=ALU.add))
        P.op("dve", [A["masked"]], [r1["t1"]], lambda e: e.tensor_reduce(out=r1["t1"][:], in_=A["masked"][:], axis=AX.X, op=ALU.max))
        P.op("dve", [A["masked"], r1["t1"]], [A["e1"]], lambda e: e.tensor_scalar(
            out=A["e1"][:], in0=A["masked"][:], scalar1=r1["t1"][:, 0:1], scalar2=None, op0=ALU.is_equal))
        P.op("dve", [A["e1"], A["masked"]], [A["masked2"]], lambda e: e.scalar_tensor_tensor(
            out=A["masked2"][:], in0=A["e1"][:], scalar=-BIG, in1=A["masked"][:], op0=ALU.mult, op1=ALU.add))
        P.op("dve", [A["masked2"]], [r1["t2"]], lambda e: e.tensor_reduce(out=r1["t2"][:], in_=A["masked2"][:], axis=AX.X, op=ALU.max))
        P.op("dve", [A["masked2"], r1["t2"]], [A["e2"]], lambda e: e.tensor_scalar(
            out=A["e2"][:], in0=A["masked2"][:], scalar1=r1["t2"][:, 0:1], scalar2=None, op0=ALU.is_equal))
        P.op("dve", [A["e1"], A["e2"]], [A["e1"]], lambda e: e.tensor_tensor(
            out=A["e1"][:], in0=A["e1"][:], in1=A["e2"][:], op=ALU.add))
        P.op("dve", [A["e1"], A["aff"]], [A["gate"]], lambda e: e.tensor_tensor(
            out=A["gate"][:], in0=A["e1"][:], in1=A["aff"][:], op=ALU.mult))
        P.op("dve", [A["gate"]], [r1["den"]], lambda e: e.tensor_reduce(out=r1["den"][:], in_=A["gate"][:], axis=AX.X, op=ALU.add))
        P.op("dve", [r1["den"]], [r1["rden"]], lambda e: e.reciprocal(out=r1["rden"][:], in_=r1["den"][:]))
        P.op("dve", [A["gate"], r1["rden"]], [comb], lambda e: e.tensor_scalar(
            out=comb[:, t, :], in0=A["gate"][:], scalar1=r1["rden"][:, 0:1], scalar2=None, op0=ALU.mult))

    cnt = {"pbB": 0, "tmp": 0}

    for gi, (g0, tg, row) in enumerate(groups):
        nt = tg // 128
        load_row(row)
        P.dma(aTs, aTs[:, :, 0:tg], aT, aT[:, g0:g0 + tg].rearrange("(kc p) n -> p kc n", p=128))
        for t in range(nt):
            P.dma(xb[t], xb[t][:], xin, xin[g0 + t * 128:g0 + (t + 1) * 128, :])

        def phaseBC(t):
            xbuf = xb[t]
            xa = xbuf[:]
            for h in range(2):
                base = (cnt["pbB"] % 2) * 4
                cnt["pbB"] += 1
                tA = tmpA[cnt["tmp"] % 2]
                sg = sgt[cnt["tmp"] % 2]
                cnt["tmp"] += 1
                pv = pb[base + 0]

                def mm(e, pp, wsb):
                    for kc in range(8):
                        ins = e.matmul(pp[:], lhsT=aTs[:, kc, t * 128:(t + 1) * 128],
                                       rhs=wsb[:, kc, h * 512:(h + 1) * 512], start=(kc == 0), stop=(kc == 7))
                    return ins
                P.op("pe", [aTs, wps[0]], [pv], lambda e: mm(e, pv, wps[0]), n_inst=8)
                if kind == "s5":
                    pg = pb[base + 1]
                    P.op("pe", [aTs, wps[1]], [pg], lambda e: mm(e, pg, wps[1]), n_inst=8)
                    P.op("act", [pg], [sg], lambda e: e.activation(out=sg[:], in_=pg[:], func=AF.Sigmoid))
                    P.op("dve", [pv, sg], [tA], lambda e: e.tensor_tensor(out=tA[:], in0=pv[:], in1=sg[:], op=ALU.mult))
                    P.op("dve", [tA, g1B], [tA], lambda e: e.tensor_tensor(
                        out=tA[:], in0=tA[:], in1=g1B[:, h * 512:(h + 1) * 512], op=ALU.mult))
                else:
                    P.op("dve", [pv, g1B], [tA], lambda e: e.tensor_tensor(
                        out=tA[:], in0=pv[:], in1=g1B[:, h * 512:(h + 1) * 512], op=ALU.mult))
                P.op("dve", [xbuf, tA], [xbuf], lambda e: e.scalar_tensor_tensor(
                    out=xa[:, h * 512:(h + 1) * 512], in0=xa[:, h * 512:(h + 1) * 512], scalar=ALPHA,
                    in1=tA[:], op0=ALU.mult, op1=ALU.add))
            layer_norm_tile(P, xbuf, xa, lnGB[0], lnBB[0], stats, epst)
            P.dma(x1scr, x1scr[g0 + t * 128:g0 + (t + 1) * 128, :], xbuf, xa)

        def phaseDE(t):
            xbuf = xb[t]
            xa = xbuf[:]
            for half in range(2):
                pt = pb[2 + half] if True else None

                def tr(e, pt=pt, half=half):
                    for j in range(4):
                        kc = half * 4 + j
                        ins = e.transpose(pt[:, j * 128:(j + 1) * 128], xa[:, kc * 128:(kc + 1) * 128], ident[:])
                    return ins
                P.op("pe", [xbuf, ident], [pt], tr, n_inst=4)
                for j in range(4):
                    kc = half * 4 + j
                    P.op("act", [pt, mcols], [h2T], lambda e, kc=kc, j=j, pt=pt: e.activation(
                        out=h2T[:, kc, t * 128:(t + 1) * 128], in_=pt[:, j * 128:(j + 1) * 128],
                        func=AF.Identity, scale=mcols[:, row, 4, kc:kc + 1], bias=mcols[:, row, 3, kc:kc + 1]))
            lp = pb[6]

            def rmm(e):
                for kc in range(8):
                    ins = e.matmul(lp[:, 0:NE], lhsT=h2T[:, kc, t * 128:(t + 1) * 128], rhs=rws[:, kc, :],
                                   start=(kc == 0), stop=(kc == 7))
                return ins
            P.op("pe", [h2T, rws], [lp], rmm, n_inst=8)
            topk(t, lp)

        for t in range(nt + 1):
            if t < nt:
                phaseBC(t)
            if t >= 1:
                phaseDE(t - 1)

        if DEBUG:
            for t in range(nt):
                P.dma(combo, combo[g0 + t * 128:g0 + (t + 1) * 128, :], comb, comb[:, t, :])
        blocks = [(b0, min(512, tg - b0)) for b0 in range(0, tg, 512)]
        items = [(e, bi) for e in range(NE) for bi in range(len(blocks))]
        l1cnt = [0]

        def L1(idx):
            e, bi = items[idx]
            b0, bn = blocks[bi]
            j = gi * NE + e
            b13 = w13b[j % 2]
            gT = gTb[idx % 2]
            for m in range(4):
                k2 = l1cnt[0] % 2
                l1cnt[0] += 1
                p1, p3 = pb[k2 * 2], pb[k2 * 2 + 1]

                def mm(e_, pp, which):
                    for kc in range(8):
                        ins = e_.matmul(pp[:, 0:bn], lhsT=b13[:, which, kc, m * 128:(m + 1) * 128],
                                        rhs=h2T[:, kc, b0:b0 + bn], start=(kc == 0), stop=(kc == 7))
                    return ins
                P.op("pe", [b13, h2T], [p1], lambda e_: mm(e_, p1, 0), n_inst=8)
                P.op("pe", [b13, h2T], [p3], lambda e_: mm(e_, p3, 1), n_inst=8)
                sl = silu_t[k2]
                P.op("act", [p1], [sl], lambda e_: e_.activation(out=sl[:, 0:bn], in_=p1[:, 0:bn], func=AF.Silu))
                P.op("dve", [sl, p3], [gT], lambda e_: e_.tensor_tensor(
                    out=gT[:, m, 0:bn], in0=sl[:, 0:bn], in1=p3[:, 0:bn], op=ALU.mult))

        l2cnt = [0]

        def L2(idx):
            e, bi = items[idx]
            b0, bn = blocks[bi]
            j = gi * NE + e
            b2 = w2b[j % 2]
            gT = gTb[idx % 2]
            for s in range(bn // 128):
                t = (b0 // 128) + s
                for h in range(2):
                    po = pb[4 + (l2cnt[0] % 4)]
                    l2cnt[0] += 1

                    def mm(e_, po=po):
                        for m in range(4):
                            ins = e_.matmul(po[:], lhsT=gT[:, m, s * 128:(s + 1) * 128],
                                            rhs=b2[:, m, h * 512:(h + 1) * 512], start=(m == 0), stop=(m == 3))
                        return ins
                    P.op("pe", [gT, b2], [po], mm, n_inst=4)
                    xbuf = xb[t]
                    ya = xbuf[:, h * 512:(h + 1) * 512]
                    if e == 0:
                        P.op("dve", [po, comb], [xbuf], lambda e_, po=po, ya=ya, t=t: e_.tensor_scalar(
                            out=ya, in0=po[:], scalar1=comb[:, t, e:e + 1], scalar2=None, op0=ALU.mult))
                    else:
                        P.op("dve", [po, comb, xbuf], [xbuf], lambda e_, po=po, ya=ya, t=t: e_.scalar_tensor_tensor(
                            out=ya, in0=po[:], scalar=comb[:, t, e:e + 1], in1=ya, op0=ALU.mult, op1=ALU.add))

        L1(0)
        for idx in range(len(items)):
            if idx + 1 < len(items):
                L1(idx + 1)
            L2(idx)
            if items[idx][1] == len(blocks) - 1:
                prefetch_weights()

        def ld(t):
            P.dma(tmpXs[t % 2], tmpXs[t % 2][:], x1scr, x1scr[g0 + t * 128:g0 + (t + 1) * 128, :])
        ld(0)
        for t in range(nt):
            if t + 1 < nt:
                ld(t + 1)
            xbuf = xb[t]
            xa = xbuf[:]
            tmpX = tmpXs[t % 2]
            P.op("dve", [xbuf, g2B], [xbuf], lambda e: e.tensor_tensor(out=xa, in0=xa, in1=g2B[:], op=ALU.mult))
            P.op("dve", [tmpX, xbuf], [xbuf], lambda e: e.scalar_tensor_tensor(
                out=xa, in0=tmpX[:], scalar=ALPHA, in1=xa, op0=ALU.mult, op1=ALU.add))
            layer_norm_tile(P, xbuf, xa, lnGB[1], lnBB[1], stats, epst)
            P.dma(xout, xout[g0 + t * 128:g0 + (t + 1) * 128, :], xbuf, xa)
    P.finish()
    return P


RMS_EPS = 1e-6


def build_qkv(hd, H, KV, qknorm, ctx_rows):
    P = Prog()
    ntok = NLAT + ctx_rows
    nqkv = (H + 2 * KV) * hd
    xin = P.dram("xin", [ntok, D], F32, kind="ExternalInput")
    modcols = P.dram("modcols", [128, 2 * 6 * 8], F32, kind="ExternalInput")
    wq = P.dram("wqkv", [D, nqkv], BF16, kind="ExternalInput")
    cosd = P.dram("cos", [NLAT, hd // 2], F32, kind="ExternalInput")
    sind = P.dram("sin", [NLAT, hd // 2], F32, kind="ExternalInput")
    qTo = P.dram("qT", [hd, H, ntok], BF16, kind="ExternalOutput")
    kTo = P.dram("kT", [hd, KV, ntok], BF16, kind="ExternalOutput")
    vo = P.dram("v", [ntok, KV * hd], BF16, kind="ExternalOutput")
    if qknorm:
        qg = P.dram("qg", [1, hd], F32, kind="ExternalInput")
        kg = P.dram("kg", [1, hd], F32, kind="ExternalInput")
    ident = make_ident(P)
    mcols = P.sbuf("mcols", [128, 2, 6, 8], F32)
    P.dma(mcols, mcols[:].rearrange("p a b c -> p (a b c)"), modcols, modcols[:])
    P.op("dve", [mcols], [mcols], lambda e: e.tensor_scalar(out=mcols[:, :, 1, :], in0=mcols[:, :, 1, :],
                                                            scalar1=1.0, scalar2=None, op0=ALU.add))
    ws = P.sbuf("ws", [128, 8, nqkv], BF16)
    P.dma(ws, ws[:], wq, wq[:].rearrange("(kc p) n -> p kc n", p=128))
    epst = P.sbuf("epst", [128, 1], F32)
    P.op("dve", [], [epst], lambda e: e.memset(epst[:], RMS_EPS))
    if qknorm:
        qgB = P.sbuf("qgB", [128, hd], F32)
        kgB = P.sbuf("kgB", [128, hd], F32)
        P.dma(qgB, qgB[:], qg, qg[0:1, :].partition_broadcast(128))
        P.dma(kgB, kgB[:], kg, kg[0:1, :].partition_broadcast(128))
    xs = [P.sbuf(f"xs{i}", [128, D], F32) for i in range(2)]
    hT = [P.sbuf(f"hT{i}", [128, 8, 128], BF16) for i in range(2)]
    qkv = [P.sbuf(f"qkv{i}", [128, nqkv], F32) for i in range(2)]
    cs = [P.sbuf(f"cs{i}", [128, 2, hd // 2], F32) for i in range(2)]
    NH = H + KV
    ro = [P.sbuf(f"ro{i}", [128, NH * hd], F32) for i in range(2)]
    t1 = P.sbuf("t1", [128, NH * hd // 2], F32)
    t2 = P.sbuf("t2", [128, NH * hd // 2], F32)
    sq = P.sbuf("sq", [128, NH * hd], F32)
    ms = P.sbuf("ms", [128, NH], F32)
    rs = P.sbuf("rs", [128, NH], F32)
    vb = [P.sbuf(f"vb{i}", [128, KV * hd], BF16) for i in range(2)]
    qTs = [P.sbuf(f"qTs{i}", [hd, NH, 128], BF16) for i in range(2)]
    pb = [P.psum(f"pb{i}", [128, 512], F32) for i in range(8)]
    nt = ntok // 128
    pc = [0]
    for t in range(nt):
        row = 0 if t < NLAT // 128 else 1
        x_ = xs[t % 2]
        h_ = hT[t % 2]
        q_ = qkv[t % 2]
        P.dma(x_, x_[:], xin, xin[t * 128:(t + 1) * 128, :])
        if row == 0:
            c_ = cs[t % 2]
            P.dma(c_, c_[:, 0, :], cosd, cosd[t * 128:(t + 1) * 128, :])
            P.dma(c_, c_[:, 1, :], sind, sind[t * 128:(t + 1) * 128, :])
        for half in range(2):
            pt = pb[half]

            def tr(e, pt=pt, half=half):
                for j in range(4):
                    kc = half * 4 + j
                    ins = e.transpose(pt[:, j * 128:(j + 1) * 128], x_[:, kc * 128:(kc + 1) * 128], ident[:])
                return ins
            P.op("pe", [x_, ident], [pt], tr, n_inst=4)
            for j in range(4):
                kc = half * 4 + j
                P.op("act", [pt, mcols], [h_], lambda e, kc=kc, j=j, pt=pt: e.activation(
                    out=h_[:, kc, :], in_=pt[:, j * 128:(j + 1) * 128], func=AF.Identity,
                    scale=mcols[:, row, 1, kc:kc + 1], bias=mcols[:, row, 0, kc:kc + 1]))
        for c0 in range(0, nqkv, 512):
            cn = min(512, nqkv - c0)
            pp = pb[2 + (pc[0] % 2)]
            pc[0] += 1

            def mm(e, pp=pp, c0=c0, cn=cn):
                for kc in range(8):
                    ins = e.matmul(pp[:, 0:cn], lhsT=h_[:, kc, :], rhs=ws[:, kc, c0:c0 + cn],
                                   start=(kc == 0), stop=(kc == 7))
                return ins
            P.op("pe", [h_, ws], [pp], mm, n_inst=8)
            P.op("act", [pp], [q_], lambda e, pp=pp, c0=c0, cn=cn: e.activation(
                out=q_[:, c0:c0 + cn], in_=pp[:, 0:cn], func=AF.Copy))
        v_ = vb[t % 2]
        P.op("dve", [q_], [v_], lambda e: e.tensor_copy(out=v_[:], in_=q_[:, NH * hd:nqkv]))
        P.dma(vo, vo[t * 128:(t + 1) * 128, :], v_, v_[:])
        qk = q_[:, 0:NH * hd]
        if qknorm:
            P.op("dve", [q_], [sq], lambda e: e.tensor_tensor(out=sq[:], in0=qk, in1=qk, op=ALU.mult))
            P.op("dve", [sq], [ms], lambda e: e.tensor_reduce(
                out=ms[:], in_=sq[:].rearrange("p (h d) -> p h d", d=hd), axis=AX.X, op=ALU.add))
            P.op("act", [ms, epst], [rs], lambda e: e.activation(out=rs[:], in_=ms[:], func=AF.Sqrt,
                                                                 bias=epst[:, 0:1], scale=1.0 / hd))
            P.op("dve", [rs], [rs], lambda e: e.reciprocal(out=rs[:], in_=rs[:]))
            q3 = qk.rearrange("p (h d) -> p h d", d=hd)
            P.op("dve", [q_, rs], [q_], lambda e: e.tensor_tensor(
                out=q3, in0=q3, in1=rs[:].unsqueeze(2).to_broadcast([128, NH, hd]), op=ALU.mult))
            P.op("dve", [q_, qgB], [q_], lambda e: e.tensor_tensor(
                out=q3[:, 0:H, :], in0=q3[:, 0:H, :], in1=qgB[:].unsqueeze(1).to_broadcast([128, H, hd]), op=ALU.mult))
            P.op("dve", [q_, kgB], [q_], lambda e: e.tensor_tensor(
                out=q3[:, H:NH, :], in0=q3[:, H:NH, :], in1=kgB[:].unsqueeze(1).to_broadcast([128, KV, hd]), op=ALU.mult))
        r_ = ro[t % 2]
        if row == 0:
            qd = hd // 4
            x5 = qk.rearrange("p (h a two f) -> p h a two f", a=2, two=2, f=qd)
            o5 = r_[:].rearrange("p (h a two f) -> p h a two f", a=2, two=2, f=qd)
            x1, x2 = x5[:, :, :, 0, :], x5[:, :, :, 1, :]
            cB = c_[:, 0, :].rearrange("p (a f) -> p a f", f=qd).unsqueeze(1).to_broadcast([128, NH, 2, qd])
            sB = c_[:, 1, :].rearrange("p (a f) -> p a f", f=qd).unsqueeze(1).to_broadcast([128, NH, 2, qd])
            t1v = t1[:].rearrange("p (h a f) -> p h a f", a=2, f=qd)
            t2v = t2[:].rearrange("p (h a f) -> p h a f", a=2, f=qd)
            P.op("dve", [q_, c_], [t1], lambda e: e.tensor_tensor(out=t1v, in0=x1, in1=cB, op=ALU.mult))
            P.op("dve", [q_, c_], [t2], lambda e: e.tensor_tensor(out=t2v, in0=x2, in1=sB, op=ALU.mult))
            P.op("dve", [t1, t2], [r_], lambda e: e.tensor_tensor(out=o5[:, :, :, 0, :], in0=t1v, in1=t2v, op=ALU.subtract))
            P.op("dve", [q_, c_], [t1], lambda e: e.tensor_tensor(out=t1v, in0=x2, in1=cB, op=ALU.mult))
            P.op("dve", [q_, c_], [t2], lambda e: e.tensor_tensor(out=t2v, in0=x1, in1=sB, op=ALU.mult))
            P.op("dve", [t1, t2], [r_], lambda e: e.tensor_tensor(out=o5[:, :, :, 1, :], in0=t1v, in1=t2v, op=ALU.add))
            src, SRC = r_[:], r_
        else:
            src, SRC = qk, q_
        qt_ = qTs[t % 2]
        per = 512 // 128 if hd <= 128 else 1
        for h0 in range(0, NH, 4):
            hn = min(4, NH - h0)
            pt = pb[4 + ((h0 // 4) % 4)]

            def trq(e, pt=pt, h0=h0, hn=hn):
                for j in range(hn):
                    ins = e.transpose(pt[0:hd, j * 128:(j + 1) * 128], src[:, (h0 + j) * hd:(h0 + j + 1) * hd], ident[:])
                return ins
            P.op("pe", [SRC, ident], [pt], trq, n_inst=hn)
            P.op("act", [pt], [qt_], lambda e, pt=pt, h0=h0, hn=hn: e.activation(
                out=qt_[:, h0:h0 + hn, :], in_=pt[0:hd, 0:hn * 128].rearrange("p (h n) -> p h n", n=128), func=AF.Copy))
        P.dma(qTo, qTo[:, :, t * 128:(t + 1) * 128], qt_, qt_[:, 0:H, :])
        P.dma(kTo, kTo[:, :, t * 128:(t + 1) * 128], qt_, qt_[:, H:NH, :])
    P.finish()
    return P


def build_attn(hd, H, KV, NK, NQtot, qblocks, use_sink, nmask, maskw, hg=1):
    P = Prog()
    R = H // KV
    assert R % hg == 0
    scale = hd ** -0.5
    nkb = NK // 128
    qTd = P.dram("qT", [hd, H, NQtot], BF16, kind="ExternalInput")
    kTd = P.dram("kT", [hd, KV, NK], BF16, kind="ExternalInput")
    vd = P.dram("v", [NK, KV * hd], BF16, kind="ExternalInput")
    oTd = P.dram("oT", [H * hd, NQtot], BF16, kind="ExternalOutput")
    onesd = P.dram("ones_in", [128, 128], BF16, kind="ExternalInput")
    if nmask:
        maskd = P.dram("masks", [128, nmask, maskw], BF16, kind="ExternalInput")
        msk = P.sbuf("msk", [128, nmask, maskw], BF16)
        P.dma(msk, msk[:], maskd, maskd[:])
    if use_sink:
        sinkd = P.dram("sink", [1, H], F32, kind="ExternalInput")
        sinkB = P.sbuf("sinkB", [128, H], F32)
        P.dma(sinkB, sinkB[:], sinkd, sinkd[0:1, :].partition_broadcast(128))
        esink = P.sbuf("esink", [128, H], F32)
    ones = P.sbuf("ones", [128, 128], BF16)
    P.dma(ones, ones[:], onesd, onesd[:])
    onesf = P.sbuf("onesf", [128, 128], F32)
    P.op("dve", [], [onesf], lambda e: e.memset(onesf[:], 1.0))
    kT = P.sbuf("kT_s", [hd, KV, NK], BF16)
    for g in range(KV):
        P.dma(kT, kT[:, g, :], kTd, kTd[:, g, :])
    vs = P.sbuf("v_s", [128, nkb, KV * hd], BF16)
    P.dma(vs, vs[:], vd, vd[:].rearrange("(b p) n -> p b n", p=128))
    NQ = max(q[1] for q in qblocks)
    NC_ = hg * NQ
    assert NC_ <= 512
    qTs = [P.sbuf(f"qTs{i}", [hd, H, NQ], BF16) for i in range(2)]
    sqb = P.sbuf("sqb", [hd, 512], BF16)
    pTs = [P.sbuf(f"pT{i}", [128, NC_], BF16) for i in range(4)]
    accD = [P.sbuf(f"accD{i}", [128, NC_], F32) for i in range(2)]
    rden = [P.sbuf(f"rden{i}", [hd, NC_], F32) for i in range(2)]
    osb = [P.sbuf(f"osb{i}", [hd, NC_], BF16) for i in range(2)]
    kmax = P.sbuf("kmax", [1, 2], F32)
    qmax = P.sbuf("qmax", [1, 2], F32)
    cur = P.sbuf("curmx", [1, 1], F32)
    nshift = P.sbuf("nshift", [128, 1], F32)
    pS = [P.psum(f"pS{i}", [128, 512], F32) for i in range(4)]
    pO = [P.psum(f"pO{i}", [128, 512], F32) for i in range(2)]
    pD = [P.psum(f"pD{i}", [128, 512], F32) for i in range(2)]
    pX = pD[0]

    def max_sq_norm(SRC, chunks, dst):
        first = True
        for ap in chunks:
            cn = ap.shape[-1] if len(ap.shape) == 2 else None
            cn = int(np.prod(ap.shape[1:]))
            P.op("dve", [SRC], [sqb], lambda e, ap=ap, cn=cn: e.tensor_tensor(out=sqb[:, 0:cn], in0=ap, in1=ap, op=ALU.mult))
            P.op("pe", [sqb, ones], [pX], lambda e, cn=cn: e.matmul(pX[0:1, 0:cn], lhsT=ones[0:hd, 0:1], rhs=sqb[:, 0:cn],
                                                                    start=True, stop=True))
            if first:
                P.op("dve", [pX], [dst], lambda e, cn=cn: e.tensor_reduce(out=dst[:, 0:1], in_=pX[0:1, 0:cn], axis=AX.X, op=ALU.max))
                first = False
            else:
                P.op("dve", [pX], [cur], lambda e, cn=cn: e.tensor_reduce(out=cur[:, 0:1], in_=pX[0:1, 0:cn], axis=AX.X, op=ALU.max))
                P.op("dve", [cur, dst], [dst], lambda e: e.tensor_tensor(out=dst[:, 0:1], in0=dst[:, 0:1], in1=cur[:, 0:1], op=ALU.max))

    kflat = kT[:].rearrange("p g n -> p (g n)")
    max_sq_norm(kT, [kflat[:, c0:min(c0 + 512, KV * NK)] for c0 in range(0, KV * NK, 512)], kmax)

    cnt = {"s": 0, "p": 0, "o": 0}
    for bi, (q0, nq, klist) in enumerate(qblocks):
        qt = qTs[bi % 2]
        P.dma(qt, qt[:, :, 0:nq], qTd, qTd[:, :, q0:q0 + nq])
        ncol = hg * nq
        if nq == NQ:
            qflat = qt[:].rearrange("p h n -> p (h n)")
            chunks = [qflat[:, c0:min(c0 + 512, H * NQ)] for c0 in range(0, H * NQ, 512)]
        else:
            chunks = [qt[:, h, 0:nq] for h in range(H)]
        max_sq_norm(qt, chunks, qmax)
        P.op("dve", [qmax, kmax], [qmax], lambda e: e.tensor_tensor(out=qmax[:, 1:2], in0=qmax[:, 0:1], in1=kmax[:, 0:1], op=ALU.mult))
        P.op("act", [qmax], [qmax], lambda e: e.activation(out=qmax[:, 1:2], in_=qmax[:, 1:2], func=AF.Sqrt))
        P.op("dve", [qmax], [qmax], lambda e: e.tensor_scalar(out=qmax[:, 1:2], in0=qmax[:, 1:2], scalar1=-scale, scalar2=None, op0=ALU.mult))
        P.op("pe", [onesf, qmax], [pX], lambda e: e.matmul(pX[:, 0:1], lhsT=onesf[0:1, :], rhs=qmax[0:1, 1:2], start=True, stop=True))
        P.op("dve", [pX], [nshift], lambda e: e.tensor_copy(out=nshift[:], in_=pX[:, 0:1]))
        if use_sink:
            P.op("act", [sinkB, nshift], [esink], lambda e: e.activation(out=esink[:], in_=sinkB[:], func=AF.Exp,
                                                                        bias=nshift[:, 0:1], scale=1.0))
        for v in range(H // hg):
            h0 = v * hg
            g = h0 // R
            po = pO[cnt["o"] % 2]
            pd = pD[cnt["o"] % 2]
            rd = rden[cnt["o"] % 2]
            ob = osb[cnt["o"] % 2]
            acc = accD[cnt["o"] % 2]
            cnt["o"] += 1
            nk = len(klist)
            ps_of = {}
            if hg == 1:
                qrhs = qt[:, h0, 0:nq]
            else:
                qrhs = qt[:, h0:h0 + hg, :].rearrange("p h n -> p (h n)")

            def S(i):
                kb, _ = klist[i]
                ps = pS[cnt["s"] % 4]
                cnt["s"] += 1
                ps_of[i] = ps
                P.op("pe", [kT, qt], [ps], lambda e: e.matmul(ps[:, 0:ncol], lhsT=kT[:, g, kb * 128:(kb + 1) * 128],
                                                              rhs=qrhs, start=True, stop=True))

            def PV(i):
                kb, mid = klist[i]
                ps = ps_of.pop(i)
                pt = pTs[cnt["p"] % 4]
                cnt["p"] += 1
                P.op("act", [ps, nshift], [pt], lambda e: e.activation(out=pt[:, 0:ncol], in_=ps[:, 0:ncol], func=AF.Exp,
                                                                       bias=nshift[:, 0:1], scale=scale))
                if mid is not None:
                    P.op("dve", [pt, msk], [pt], lambda e: e.tensor_tensor(out=pt[:, 0:ncol], in0=pt[:, 0:ncol],
                                                                           in1=msk[:, mid, 0:ncol], op=ALU.mult))
                P.op("pe", [vs, pt], [po], lambda e: e.matmul(po[0:hd, 0:ncol], lhsT=vs[:, kb, g * hd:(g + 1) * hd],
                                                              rhs=pt[:, 0:ncol], start=(i == 0), stop=(i == nk - 1)))
                if i == 0:
                    P.op("dve", [pt], [acc], lambda e: e.tensor_copy(out=acc[:, 0:ncol], in_=pt[:, 0:ncol]))
                else:
                    P.op("dve", [pt, acc], [acc], lambda e: e.tensor_tensor(out=acc[:, 0:ncol], in0=acc[:, 0:ncol],
                                                                            in1=pt[:, 0:ncol], op=ALU.add))
            for i in range(min(2, nk)):
                S(i)
            for i in range(nk):
                if i + 2 < nk:
                    S(i + 2)
                PV(i)
            P.op("pe", [onesf, acc], [pd], lambda e: e.matmul(pd[0:hd, 0:ncol], lhsT=onesf[:, 0:hd], rhs=acc[:, 0:ncol],
                                                              start=True, stop=True))
            if use_sink:
                for j in range(hg):
                    P.op("dve", [pd, esink], [rd], lambda e, j=j: e.tensor_scalar(
                        out=rd[:, j * nq:(j + 1) * nq], in0=pd[0:hd, j * nq:(j + 1) * nq],
                        scalar1=esink[0:hd, h0 + j:h0 + j + 1], scalar2=None, op0=ALU.add))
                P.op("dve", [rd], [rd], lambda e: e.reciprocal(out=rd[:, 0:ncol], in_=rd[:, 0:ncol]))
            else:
                P.op("dve", [pd], [rd], lambda e: e.reciprocal(out=rd[:, 0:ncol], in_=pd[0:hd, 0:ncol]))
            P.op("dve", [po, rd], [ob], lambda e: e.tensor_tensor(out=ob[:, 0:ncol], in0=po[0:hd, 0:ncol], in1=rd[:, 0:ncol], op=ALU.mult))
            for j in range(hg):
                P.dma(oTd, oTd[(h0 + j) * hd:(h0 + j + 1) * hd, q0:q0 + nq], ob, ob[:, j * nq:(j + 1) * nq])
    P.finish()
    return P


def swa_blocks(ctx_out):
    qb = []
    nb = NLAT // 128
    for i in range(nb):
        left = (i, 2 if i == 0 else 0)
        right = (i + 2, 3 if i == nb - 1 else 1)
        qb.append((i * 128, 128, [left, (i + 1, None), right, (34, None), (35, None)]))
    if ctx_out:
        for j in range(2):
            qb.append((NLAT + j * 128, 128, [(34, None), (35, None)]))
    return qb


def gqa_blocks(ctx_out):
    allk = [(kb, None) for kb in range(130)]
    qb = [(i * 512, 512, allk) for i in range(NLAT // 512)]
    if ctx_out:
        qb.append((NLAT, 256, [(128, None), (129, None)]))
    return qb


GRID_W = 64
ROPE_THETA = 10000.0
_PROGS = {}


def _prog(key, fn):
    if key not in _PROGS:
        _PROGS[key] = fn()
    return _PROGS[key]


def rope_tables(hd):
    quarter = hd // 4
    inv_freq = (ROPE_THETA ** (-np.arange(quarter, dtype=np.float32) / quarter)).astype(np.float32)
    n_rows = SEQ // GRID_W
    rows = np.repeat(np.arange(n_rows, dtype=np.float32), GRID_W)
    cols = np.tile(np.arange(GRID_W, dtype=np.float32), n_rows)
    ang = np.stack([rows[:, None] * inv_freq, cols[:, None] * inv_freq], axis=1)
    return (np.cos(ang).astype(np.float32).reshape(SEQ, hd // 2),
            np.sin(ang).astype(np.float32).reshape(SEQ, hd // 2))


def core_bq(c):
    return c // 4, c % 4


def modcols_for(modv, i, b):
    rows = np.stack([modv[i, b], modv[i, 2]])
    cols = np.ascontiguousarray(rows.reshape(2, 6, 8, 128).transpose(3, 0, 1, 2)).reshape(128, 96)
    return np.ascontiguousarray(rows), cols


IDENT = np.eye(128, dtype=np.float32)
ONES_BF = np.ones((128, 128), dtype=NPBF)


SWA_HG = 4


def tri_masks(c):
    b, q = core_bq(c)
    kl = np.arange(128)[:, None]
    ql = np.arange(128)[None, :]
    L = (kl >= ql).astype(np.float32)
    Rm = (kl <= ql).astype(np.float32)
    Le = L * (0.0 if q == 0 else 1.0)
    Re = Rm * (0.0 if q == 3 else 1.0)
    m = np.stack([L, Rm, Le, Re], axis=1)
    return np.ascontiguousarray(np.tile(m, (1, 1, SWA_HG))).astype(NPBF)


def run_tail(kind, ctx_out, i, aT_list, x, ctx, wp, wb, modv, inputs):
    P = _prog(("tail", kind, ctx_out), lambda: build_tail(kind, ctx_out))
    in_maps = []
    for c in range(NCORES):
        b, q = core_bq(c)
        rows, cols = modcols_for(modv, i, b)
        xin = x[b, q * NLAT:(q + 1) * NLAT]
        if ctx_out:
            xin = np.concatenate([xin, ctx[b]], 0)
        m = {"ident_in": IDENT, "aT": aT_list[c], "wp0": wp[0], "xin": np.ascontiguousarray(xin),
             "modrows": rows, "modcols": cols, "lng": inputs["ln_g"][i], "lnb": inputs["ln_b"][i],
             "rw": wb["router_w"], "rb": inputs["router_b"][None, :].astype(np.float32),
             "w1": wb["moe_w1"][i], "w3": wb["moe_w3"][i], "w2": wb["moe_w2"][i]}
        if kind == "s5":
            m["wp1"] = wp[1]
        in_maps.append(m)
    res = run_prog(P, in_maps)
    xn = np.empty_like(x)
    cn = np.array(ctx, copy=True)
    for c in range(NCORES):
        b, q = core_bq(c)
        o = np.asarray(res[c]["xout"])
        xn[b, q * NLAT:(q + 1) * NLAT] = o[:NLAT]
        if ctx_out and q == 0:
            cn[b] = o[NLAT:]
    return xn, cn


def run_attn_layer(i, which, ctx_out, x, ctx, wb, modv, inputs):
    if which == "swa":
        hd, H, KV = 64, 16, 2
        wqkv, wo = wb["swa_w_qkv"][0], wb["swa_w_o"][0]
    else:
        hd, H, KV = 128, 8, 2
        wqkv, wo = wb["gqa_w_qkv"][0], wb["gqa_w_o"][0]
    cos, sin = rope_tables(hd)
    Pq = _prog(("qkv", which), lambda: build_qkv(hd, H, KV, which == "gqa", NCTX))
    in_maps = []
    for c in range(NCORES):
        b, q = core_bq(c)
        _, cols = modcols_for(modv, i, b)
        m = {"ident_in": IDENT, "xin": np.ascontiguousarray(np.concatenate([x[b, q * NLAT:(q + 1) * NLAT], ctx[b]], 0)),
             "modcols": cols, "wqkv": wqkv, "cos": np.ascontiguousarray(cos[q * NLAT:(q + 1) * NLAT]),
             "sin": np.ascontiguousarray(sin[q * NLAT:(q + 1) * NLAT])}
        if which == "gqa":
            m["qg"] = inputs["gqa_q_norm"].astype(np.float32).reshape(1, hd)
            m["kg"] = inputs["gqa_k_norm"].astype(np.float32).reshape(1, hd)
        in_maps.append(m)
    rq = run_prog(Pq, in_maps)
    qT = [np.asarray(r["qT"]) for r in rq]
    kT = [np.asarray(r["kT"]) for r in rq]
    vv = [np.asarray(r["v"]) for r in rq]
    nq_tot = NLAT + (NCTX if ctx_out else 0)
    in_maps = []
    if which == "swa":
        Pa = _prog(("attn", which, ctx_out), lambda: build_attn(hd, H, KV, 4608, nq_tot, swa_blocks(ctx_out), True, 4, 128 * SWA_HG, hg=SWA_HG))
        for c in range(NCORES):
            b, q = core_bq(c)
            zk = np.zeros((hd, KV, 128), NPBF)
            zv = np.zeros((128, KV * hd), NPBF)
            kl = kT[c - 1][:, :, NLAT - 128:NLAT] if q > 0 else zk
            kr = kT[c + 1][:, :, 0:128] if q < 3 else zk
            vl = vv[c - 1][NLAT - 128:NLAT] if q > 0 else zv
            vr = vv[c + 1][0:128] if q < 3 else zv
            kk = np.concatenate([kl, kT[c][:, :, :NLAT], kr, kT[c][:, :, NLAT:]], axis=2)
            vk = np.concatenate([vl, vv[c][:NLAT], vr, vv[c][NLAT:]], axis=0)
            in_maps.append({"qT": np.ascontiguousarray(qT[c][:, :, :nq_tot]), "kT": np.ascontiguousarray(kk),
                            "v": np.ascontiguousarray(vk), "ones_in": ONES_BF, "masks": tri_masks(c),
                            "sink": inputs["swa_sink"].astype(np.float32).reshape(1, H)})
    else:
        Pa = _prog(("attn", which, ctx_out), lambda: build_attn(hd, H, KV, 16640, nq_tot, gqa_blocks(ctx_out), False, 0, 0))
        for c in range(NCORES):
            b, q = core_bq(c)
            cs = [4 * b + j for j in range(4)]
            kk = np.concatenate([kT[j][:, :, :NLAT] for j in cs] + [kT[c][:, :, NLAT:]], axis=2)
            vk = np.concatenate([vv[j][:NLAT] for j in cs] + [vv[c][NLAT:]], axis=0)
            in_maps.append({"qT": np.ascontiguousarray(qT[c][:, :, :nq_tot]), "kT": np.ascontiguousarray(kk),
                            "v": np.ascontiguousarray(vk), "ones_in": ONES_BF})
    ra = run_prog(Pa, in_maps)
    oT = [np.asarray(r["oT"]) for r in ra]
    return run_tail("attn", ctx_out, i, oT, x, ctx, [wo], wb, modv, inputs), (qT, kT, vv, oT)


NCH = 130
PI = float(np.pi)


def build_s5():
    P = Prog()
    ntk = NCH * 128
    xtok = P.dram("xtok", [2, ntk, 128], F32, kind="ExternalInput")
    xT = P.dram("xT", [128, 2 * ntk], F32, kind="ExternalInput")
    mrow = P.dram("mrow", [3, 2, 128], F32, kind="ExternalInput")
    mcol = P.dram("mcol", [128, 6], F32, kind="ExternalInput")
    ared = P.dram("are", [64, 16], F32, kind="ExternalInput")
    aimd = P.dram("aim", [64, 16], F32, kind="ExternalInput")
    ldtd = P.dram("ldt", [1, 16], F32, kind="ExternalInput")
    bred = P.dram("bre", [64, 256], F32, kind="ExternalInput")
    bimd = P.dram("bim", [64, 256], F32, kind="ExternalInput")
    cred = P.dram("cre", [64, 256], F32, kind="ExternalInput")
    cimd = P.dram("cim", [64, 256], F32, kind="ExternalInput")
    dskd = P.dram("dsk", [128, 1], F32, kind="ExternalInput")
    antid = P.dram("anti_in", [128, 128], BF16, kind="ExternalInput")
    gTo = P.dram("gT", [128, 2 * ntk], BF16, kind="ExternalOutput")
    hk = P.dram("hk", [8, 256, 256], BF16, kind="Internal")
    ident = make_ident(P)
    anti = P.sbuf("anti", [128, 128], BF16)
    P.dma(anti, anti[:], antid, antid[:])

    def sb(name, shape, dt=F32):
        return P.sbuf(name, shape, dt)

    def ld(name, src, shape, ap=None):
        t = sb(name, shape)
        P.dma(t, t[:], src, src[:] if ap is None else ap)
        return t

    def tt(out_b, out_ap, a_b, a_ap, b_b, b_ap, op):
        P.op("dve", [a_b, b_b], [out_b], lambda e: e.tensor_tensor(out=out_ap, in0=a_ap, in1=b_ap, op=op))

    are = ld("are_s", ared, [64, 16])
    aim = ld("aim_s", aimd, [64, 16])
    dt = sb("dt_s", [64, 16])
    P.dma(dt, dt[:], ldtd, ldtd[0:1, :].partition_broadcast(64))
    bre = ld("bre_s", bred, [64, 256])
    bim = ld("bim_s", bimd, [64, 256])
    cre = ld("cre_s", cred, [64, 256])
    cim = ld("cim_s", cimd, [64, 256])
    dsk = ld("dsk_s", dskd, [128, 1])
    mc = ld("mc_s", mcol, [128, 6])
    P.op("act", [dt], [dt], lambda e: e.activation(out=dt[:], in_=dt[:], func=AF.Exp))
    adr = sb("adr", [64, 16])
    adi = sb("adi", [64, 16])
    tt(adr, adr[:], are, are[:], dt, dt[:], ALU.mult)
    tt(adi, adi[:], aim, aim[:], dt, dt[:], ALU.mult)
    mag = sb("mag", [64, 16])
    P.op("act", [adr], [mag], lambda e: e.activation(out=mag[:], in_=adr[:], func=AF.Exp))
    rr = sb("rr", [64, 32])
    rm = sb("rm", [64, 32])
    P.op("dve", [adi], [rr], lambda e: e.tensor_copy(out=rr[:, 0:16], in_=adi[:]))
    P.op("dve", [adi], [rr], lambda e: e.tensor_scalar(out=rr[:, 16:32], in0=adi[:], scalar1=PI / 2, scalar2=None, op0=ALU.add))
    for _ in range(5):
        P.op("dve", [rr], [rm], lambda e: e.tensor_scalar(out=rm[:], in0=rr[:], scalar1=PI, scalar2=2 * PI,
                                                          op0=ALU.is_gt, op1=ALU.mult))
        tt(rr, rr[:], rr, rr[:], rm, rm[:], ALU.subtract)
    sc = sb("sincos", [64, 32])
    P.op("act", [rr], [sc], lambda e: e.activation(out=sc[:], in_=rr[:], func=AF.Sin))
    lre = sb("lre", [64, 16])
    lim = sb("lim", [64, 16])
    tt(lre, lre[:], mag, mag[:], sc, sc[:, 16:32], ALU.mult)
    tt(lim, lim[:], mag, mag[:], sc, sc[:, 0:16], ALU.mult)
    den = sb("den", [64, 16])
    tmp = sb("tmp16", [64, 16])
    tmp2 = sb("tmp16b", [64, 16])
    tt(den, den[:], are, are[:], are, are[:], ALU.mult)
    tt(tmp, tmp[:], aim, aim[:], aim, aim[:], ALU.mult)
    tt(den, den[:], den, den[:], tmp, tmp[:], ALU.add)
    P.op("dve", [den], [den], lambda e: e.reciprocal(out=den[:], in_=den[:]))
    nre = sb("nre", [64, 16])
    P.op("dve", [lre], [nre], lambda e: e.tensor_scalar(out=nre[:], in0=lre[:], scalar1=-1.0, scalar2=None, op0=ALU.add))
    fre = sb("fre", [64, 16])
    fim = sb("fim", [64, 16])
    tt(fre, fre[:], nre, nre[:], are, are[:], ALU.mult)
    tt(tmp, tmp[:], lim, lim[:], aim, aim[:], ALU.mult)
    tt(fre, fre[:], fre, fre[:], tmp, tmp[:], ALU.add)
    tt(fre, fre[:], fre, fre[:], den, den[:], ALU.mult)
    tt(fim, fim[:], lim, lim[:], are, are[:], ALU.mult)
    tt(tmp, tmp[:], nre, nre[:], aim, aim[:], ALU.mult)
    tt(fim, fim[:], fim, fim[:], tmp, tmp[:], ALU.subtract)
    tt(fim, fim[:], fim, fim[:], den, den[:], ALU.mult)

    def v3(t):
        return t[:].rearrange("p (a c) -> p a c", c=16)

    def bc3(t):
        return t[:].unsqueeze(2).to_broadcast([64, 16, 16])
    bbre = sb("bbre", [64, 256])
    bbim = sb("bbim", [64, 256])
    t256 = sb("t256", [64, 256])
    tt(bbre, v3(bbre), bre, v3(bre), fre, bc3(fre), ALU.mult)
    tt(t256, v3(t256), bim, v3(bim), fim, bc3(fim), ALU.mult)
    tt(bbre, bbre[:], bbre, bbre[:], t256, t256[:], ALU.subtract)
    tt(bbim, v3(bbim), bim, v3(bim), fre, bc3(fre), ALU.mult)
    tt(t256, v3(t256), bre, v3(bre), fim, bc3(fim), ALU.mult)
    tt(bbim, bbim[:], bbim, bbim[:], t256, t256[:], ALU.add)
    clre = sb("clre", [64, 256])
    clim = sb("clim", [64, 256])
    tt(clre, v3(clre), cre, v3(cre), lre, bc3(lre), ALU.mult)
    tt(t256, v3(t256), cim, v3(cim), lim, bc3(lim), ALU.mult)
    tt(clre, clre[:], clre, clre[:], t256, t256[:], ALU.subtract)
    tt(clim, v3(clim), cre, v3(cre), lim, bc3(lim), ALU.mult)
    tt(t256, v3(t256), cim, v3(cim), lre, bc3(lre), ALU.mult)
    tt(clim, clim[:], clim, clim[:], t256, t256[:], ALU.add)
    pw = sb("pw", [64, 8, 2, 16])
    P.op("dve", [lre], [pw], lambda e: e.tensor_copy(out=pw[:, 0, 0, :], in_=lre[:]))
    P.op("dve", [lim], [pw], lambda e: e.tensor_copy(out=pw[:, 0, 1, :], in_=lim[:]))
    for i in range(7):
        tt(tmp, tmp[:], pw, pw[:, i, 0, :], pw, pw[:, i, 0, :], ALU.mult)
        tt(tmp2, tmp2[:], pw, pw[:, i, 1, :], pw, pw[:, i, 1, :], ALU.mult)
        tt(pw, pw[:, i + 1, 0, :], tmp, tmp[:], tmp2, tmp2[:], ALU.subtract)
        tt(tmp, tmp[:], pw, pw[:, i, 0, :], pw, pw[:, i, 1, :], ALU.mult)
        P.op("dve", [tmp], [pw], lambda e, i=i: e.tensor_scalar(out=pw[:, i + 1, 1, :], in0=tmp[:], scalar1=2.0,
                                                                scalar2=None, op0=ALU.mult))
    tre = sb("tre", [64, 16, 128])
    tim = sb("tim", [64, 16, 128])
    ta = sb("ta", [64, 8, 64])
    tb = sb("tb", [64, 8, 64])
    P.op("dve", [], [tre], lambda e: e.memset(tre[:], 1.0))
    P.op("dve", [], [tim], lambda e: e.memset(tim[:], 0.0))
    for i in range(7):
        m = 1 << i
        for d in range(2):
            gs = slice(d * 8, d * 8 + 8)
            if d == 0:
                src, dst = slice(128 - m, 128), slice(128 - 2 * m, 128 - m)
            else:
                src, dst = slice(0, m), slice(m, 2 * m)
            pr = pw[:, i, 0, gs].unsqueeze(2).to_broadcast([64, 8, m])
            pi_ = pw[:, i, 1, gs].unsqueeze(2).to_broadcast([64, 8, m])
            tt(ta, ta[:, :, 0:m], tre, tre[:, gs, src], pw, pr, ALU.mult)
            tt(tb, tb[:, :, 0:m], tim, tim[:, gs, src], pw, pi_, ALU.mult)
            tt(tre, tre[:, gs, dst], ta, ta[:, :, 0:m], tb, tb[:, :, 0:m], ALU.subtract)
            tt(ta, ta[:, :, 0:m], tre, tre[:, gs, src], pw, pi_, ALU.mult)
            tt(tb, tb[:, :, 0:m], tim, tim[:, gs, src], pw, pr, ALU.mult)
            tt(tim, tim[:, gs, dst], ta, ta[:, :, 0:m], tb, tb[:, :, 0:m], ALU.add)
    treb = sb("treb", [64, 16, 128], BF16)
    timnb = sb("timnb", [64, 16, 128], BF16)
    P.op("dve", [tre], [treb], lambda e: e.tensor_copy(out=treb[:], in_=tre[:]))
    P.op("dve", [tim], [timnb], lambda e: e.tensor_scalar(out=timnb[:], in0=tim[:], scalar1=-1.0, scalar2=None, op0=ALU.mult))
    pb = [P.psum(f"pb{i}", [128, 512], F32) for i in range(8)]
    cbre = sb("cbre", [64, 256], BF16)
    cbim = sb("cbim", [64, 256], BF16)
    cbt1 = sb("cbt1", [64, 256])
    cbt2 = sb("cbt2", [64, 256])
    hks = [sb(f"hks{i}", [128, 256], BF16) for i in range(2)]
    hcnt = [0]
    for g in range(8):
        pss = {}
        for d in range(2):
            dg = d * 8 + g
            cB = lambda t: t[:, dg * 16:(dg + 1) * 16].unsqueeze(2).to_broadcast([64, 16, 16])
            bB = lambda t: t[:, dg * 16:(dg + 1) * 16].unsqueeze(1).to_broadcast([64, 16, 16])
            o1 = cbt1[:].rearrange("p (c k) -> p c k", k=16)
            o2 = cbt2[:].rearrange("p (c k) -> p c k", k=16)
            tt(cbt1, o1, cre, cB(cre), bbre, bB(bbre), ALU.mult)
            tt(cbt2, o2, cim, cB(cim), bbim, bB(bbim), ALU.mult)
            tt(cbre, cbre[:], cbt1, cbt1[:], cbt2, cbt2[:], ALU.subtract)
            tt(cbt1, o1, cre, cB(cre), bbim, bB(bbim), ALU.mult)
            tt(cbt2, o2, cim, cB(cim), bbre, bB(bbre), ALU.mult)
            tt(cbim, cbim[:], cbt1, cbt1[:], cbt2, cbt2[:], ALU.add)
            for half in range(2):
                ps = pb[d * 2 + half]

                def mm(e, ps=ps, half=half, dg=dg):
                    e.matmul(ps[:, 0:128], lhsT=cbre[:, half * 128:(half + 1) * 128], rhs=treb[:, dg, :], start=True, stop=False)
                    return e.matmul(ps[:, 0:128], lhsT=cbim[:, half * 128:(half + 1) * 128], rhs=timnb[:, dg, :], start=False, stop=True)
                P.op("pe", [cbre, cbim, treb, timnb], [ps], mm, n_inst=2)
                pss[(d, half)] = ps
        for half in range(2):
            hs = hks[hcnt[0] % 2]
            hcnt[0] += 1
            pa, pbb = pss[(0, half)], pss[(1, half)]
            P.op("act", [pa], [hs], lambda e: e.activation(out=hs[:, 0:127], in_=pa[:, 0:127], func=AF.Copy))
            P.op("act", [pbb], [hs], lambda e: e.activation(out=hs[:, 128:255], in_=pbb[:, 1:128], func=AF.Copy))
            P.op("act", [pa], [cbt1], lambda e: e.activation(out=cbt1[0:64, 0:1], in_=pa[0:64, 127:128], func=AF.Copy))
            t128 = sb(f"t128_{g}_{half}", [128, 2])
            P.op("act", [pa], [t128], lambda e: e.activation(out=t128[:, 0:1], in_=pa[:, 127:128], func=AF.Copy))
            P.op("dve", [pbb, t128], [hs], lambda e: e.tensor_tensor(out=hs[:, 127:128], in0=pbb[:, 0:1], in1=t128[:, 0:1], op=ALU.add))
            P.op("dve", [], [hs], lambda e: e.memset(hs[:, 255:256], 0.0))
            P.dma(hk, hk[g, half * 128:(half + 1) * 128, :], hs, hs[:])
    U = sb("U", [128, NCH, 128], BF16)
    Ysb = sb("Ysb", [128, NCH, 128], BF16)
    xl = [sb(f"xl{i}", [128, 8, 128]) for i in range(2)]
    scB = sb("scB", [128, 2, 128])
    Sst = sb("Sst", [64, 16, 2, NCH])
    Hin = sb("Hin", [64, 16, 2, NCH], BF16)
    Hs = sb("Hs", [64, 2, 8])
    hn = sb("hn", [64, 2, 8])
    h1 = sb("h1", [64, 8])
    h2 = sb("h2", [64, 8])
    scr = [sb(f"scr{i}", [64, 1024]) for i in range(3)]
    dg3v = [t[:].rearrange("p (c q) -> p c q", q=64) for t in scr]
    WinS = [sb(f"WinS{i}", [128, 16, 2, 64], BF16) for i in range(2)]
    Wo = sb("Wo", [64, 2, 2, 16, 128], BF16)
    HK = [sb(f"HK{i}", [128, 16, 128], BF16) for i in range(2)]
    xTb = [sb(f"xTb{i}", [128, 512]) for i in range(2)]
    zv = [sb(f"zv{i}", [128, 512]) for i in range(2)]
    gv = [sb(f"gv{i}", [128, 512], BF16) for i in range(2)]
    AB = sb("AB", [128, 3, 2])
    for r in range(3):
        P.op("dve", [mc, dsk], [AB], lambda e, r=r: e.tensor_scalar(out=AB[:, r, 0:1], in0=mc[:, 2 * r + 1:2 * r + 2], scalar1=1.0,
                                                                    scalar2=dsk[:, 0:1], op0=ALU.add, op1=ALU.mult))
        P.op("dve", [mc, dsk], [AB], lambda e, r=r: e.tensor_scalar(out=AB[:, r, 1:2], in0=mc[:, 2 * r:2 * r + 1], scalar1=dsk[:, 0:1],
                                                                    scalar2=None, op0=ALU.mult))
    ident64 = ident[0:64, 0:64]
    pcnt = [0]

    def nextpb():
        pcnt[0] += 1
        return pb[pcnt[0] % 8]

    for b in range(2):
        def load_rowmod(r):
            P.dma(scB, scB[:, 0, :], mrow, mrow[r:r + 1, 0, :].partition_broadcast(128))
            P.dma(scB, scB[:, 1, :], mrow, mrow[r:r + 1, 1, :].partition_broadcast(128))
            P.op("dve", [scB], [scB], lambda e: e.tensor_scalar(out=scB[:, 1, :], in0=scB[:, 1, :], scalar1=1.0, scalar2=None, op0=ALU.add))
        li = [0]

        def do_chunks(k0, kn):
            x_ = xl[li[0] % 2]
            li[0] += 1
            P.dma(x_, x_[:, 0:kn, :], xtok, xtok[b, k0 * 128:(k0 + kn) * 128, :].rearrange("(k p) c -> p k c", p=128))
            tt(x_, x_[:, 0:kn, :], x_, x_[:, 0:kn, :], scB, scB[:, 1, :].unsqueeze(1).to_broadcast([128, kn, 128]), ALU.mult)
            tt(U, U[:, k0:k0 + kn, :], x_, x_[:, 0:kn, :], scB, scB[:, 0, :].unsqueeze(1).to_broadcast([128, kn, 128]), ALU.add)
        load_rowmod(2)
        do_chunks(0, 2)
        load_rowmod(b)
        for k0 in range(2, NCH, 8):
            do_chunks(k0, 8)
        for dg in range(16):
            g = dg % 8
            W = WinS[dg % 2]
            idb = ident64.unsqueeze(1).to_broadcast([64, 16, 64])
            bsl = lambda t: t[:, dg * 16:(dg + 1) * 16].unsqueeze(2).to_broadcast([64, 16, 64])
            tt(scr[0], dg3v[0], ident, idb, bbre, bsl(bbre), ALU.mult)
            tt(scr[1], dg3v[1], ident, idb, bbim, bsl(bbim), ALU.mult)
            P.op("dve", [scr[1]], [scr[2]], lambda e: e.tensor_scalar(out=scr[2][:], in0=scr[1][:], scalar1=-1.0, scalar2=None, op0=ALU.mult))
            for hc in range(2):
                for ri in range(2):
                    ps = nextpb()
                    r1 = scr[0] if ri == 0 else scr[1]
                    r2 = scr[2] if ri == 0 else scr[0]

                    def mm(e, ps=ps, r1=r1, r2=r2, hc=hc, dg=dg):
                        e.matmul(ps[:, 0:512], lhsT=tre[:, dg, :], rhs=r1[:, hc * 512:(hc + 1) * 512],
                                 start=True, stop=False)
                        return e.matmul(ps[:, 0:512], lhsT=tim[:, dg, :], rhs=r2[:, hc * 512:(hc + 1) * 512],
                                        start=False, stop=True)
                    P.op("pe", [tre, tim, r1, r2], [ps], mm, n_inst=2)
                    P.op("act", [ps], [W], lambda e, ps=ps, hc=hc, ri=ri, W=W: e.activation(
                        out=W[:, hc * 8:(hc + 1) * 8, ri, :], in_=ps[:, 0:512].rearrange("p (c q) -> p c q", q=64), func=AF.Copy))
            for ri in range(2):
                ps = nextpb()

                def mm2(e, ps=ps, ri=ri, W=W, g=g):
                    for c_ in range(16):
                        ins = e.matmul(ps[0:64, 0:NCH], lhsT=W[:, c_, ri, :], rhs=U[:, :, g * 16 + c_],
                                       start=(c_ == 0), stop=(c_ == 15))
                    return ins
                P.op("pe", [W, U], [ps], mm2, n_inst=16)
                P.op("act", [ps], [Sst], lambda e, ps=ps, ri=ri, dg=dg: e.activation(out=Sst[:, dg, ri, :], in_=ps[0:64, 0:NCH], func=AF.Copy))
        for d in range(2):
            gs = slice(d * 8, d * 8 + 8)
            order = list(range(NCH)) if d == 0 else [1, 0] + list(range(NCH - 1, 1, -1))
            lr, li_ = pw[:, 7, 0, gs], pw[:, 7, 1, gs]
            P.op("dve", [], [Hs], lambda e: e.memset(Hs[:], 0.0))
            for k in order:
                P.op("act", [Hs], [Hin], lambda e, k=k: e.activation(out=Hin[:, gs, :, k].rearrange("p g r -> p r g"), in_=Hs[:], func=AF.Copy))
                tt(h1, h1[:], Hs, Hs[:, 0, :], pw, lr, ALU.mult)
                tt(h2, h2[:], Hs, Hs[:, 1, :], pw, li_, ALU.mult)
                tt(h1, h1[:], h1, h1[:], h2, h2[:], ALU.subtract)
                tt(hn, hn[:, 0, :], h1, h1[:], Sst, Sst[:, gs, 0, k], ALU.add)
                tt(h1, h1[:], Hs, Hs[:, 0, :], pw, li_, ALU.mult)
                tt(h2, h2[:], Hs, Hs[:, 1, :], pw, lr, ALU.mult)
                tt(h1, h1[:], h1, h1[:], h2, h2[:], ALU.add)
                tt(hn, hn[:, 1, :], h1, h1[:], Sst, Sst[:, gs, 1, k], ALU.add)
                P.op("dve", [hn], [Hs], lambda e: e.tensor_copy(out=Hs[:], in_=hn[:]))
        hkc = [0]
        for g in range(8):
            for d in range(2):
                dg = d * 8 + g
                w1v = scr[0][:].rearrange("p (c t) -> p c t", t=128)
                w2v = scr[1][:].rearrange("p (c t) -> p c t", t=128)
                for hw in range(2):
                    cB = lambda t: t[:, dg * 16 + hw * 8:dg * 16 + hw * 8 + 8].unsqueeze(2).to_broadcast([64, 8, 128])
                    tB = lambda t: t[:, dg, :].unsqueeze(1).to_broadcast([64, 8, 128])
                    cs_ = slice(hw * 8, hw * 8 + 8)
                    tt(scr[0], w1v, clre, cB(clre), tre, tB(tre), ALU.mult)
                    tt(scr[1], w2v, clim, cB(clim), tim, tB(tim), ALU.mult)
                    tt(Wo, Wo[:, d, 0, cs_, :], scr[0], w1v, scr[1], w2v, ALU.subtract)
                    tt(scr[0], w1v, clre, cB(clre), tim, tB(tim), ALU.mult)
                    tt(scr[1], w2v, clim, cB(clim), tre, tB(tre), ALU.mult)
                    P.op("dve", [scr[0], scr[1]], [Wo], lambda e, d=d, cs_=cs_: e.scalar_tensor_tensor(
                        out=Wo[:, d, 1, cs_, :], in0=w1v, scalar=-1.0, in1=w2v, op0=ALU.mult, op1=ALU.subtract))
            for c_ in range(16):
                Hk_ = HK[hkc[0] % 2]
                hkc[0] += 1
                base = hk[g, c_ * 16, 0]
                src = bass.AP(hk.h.tensor, (g * 256 + c_ * 16) * 256, [[1, 128], [256, 16], [1, 128]])
                P.dma(Hk_, Hk_[:], hk, src)
                ps = nextpb()

                def mm3(e, ps=ps, Hk_=Hk_, g=g, c_=c_):
                    for k_ in range(16):
                        e.matmul(ps[:, 0:NCH], lhsT=Hk_[:, k_, :], rhs=U[:, :, g * 16 + k_], start=(k_ == 0), stop=False)
                    for d in range(2):
                        dg = d * 8 + g
                        e.matmul(ps[:, 0:NCH], lhsT=Wo[:, d, 0, c_, :], rhs=Hin[:, dg, 0, :], start=False, stop=False)
                        ins = e.matmul(ps[:, 0:NCH], lhsT=Wo[:, d, 1, c_, :], rhs=Hin[:, dg, 1, :], start=False, stop=(d == 1))
                    return ins
                P.op("pe", [Hk_, U, Wo, Hin], [ps], mm3, n_inst=20)
                P.op("act", [ps], [Ysb], lambda e, ps=ps, g=g, c_=c_: e.activation(out=Ysb[:, :, g * 16 + c_], in_=ps[:, 0:NCH], func=AF.Copy))
        blocks = [(0, 2, 2)] + [(k0, 4, b) for k0 in range(2, NCH, 4)]
        for bi, (k0, kn, r) in enumerate(blocks):
            nt_ = kn * 128
            col0 = b * ntk + k0 * 128
            xt_ = xTb[bi % 2]
            P.dma(xt_, xt_[:, 0:nt_], xT, xT[:, col0:col0 + nt_])
            ps = nextpb()

            def trj(e, ps=ps, k0=k0, kn=kn):
                for j in range(kn):
                    ins = e.matmul(ps[:, j * 128:(j + 1) * 128], lhsT=Ysb[:, k0 + j, :], rhs=anti[:], start=True, stop=True)
                return ins
            P.op("pe", [Ysb, anti], [ps], trj, n_inst=kn)
            z_, g_ = zv[bi % 2], gv[bi % 2]
            y_ = xt_
            P.op("act", [xt_, AB], [xt_], lambda e, r=r: e.activation(out=xt_[:, 0:nt_], in_=xt_[:, 0:nt_], func=AF.Identity,
                                                                      scale=AB[:, r, 0:1], bias=AB[:, r, 1:2]))
            tt(y_, y_[:, 0:nt_], ps, ps[:, 0:nt_], xt_, xt_[:, 0:nt_], ALU.add)
            tt(z_, z_[:, 0:nt_], y_, y_[:, 0:nt_], y_, y_[:, 0:nt_], ALU.mult)
            P.op("dve", [z_], [z_], lambda e: e.tensor_scalar(out=z_[:, 0:nt_], in0=z_[:, 0:nt_], scalar1=0.044715, scalar2=1.0,
                                                              op0=ALU.mult, op1=ALU.add))
            tt(z_, z_[:, 0:nt_], z_, z_[:, 0:nt_], y_, y_[:, 0:nt_], ALU.mult)
            P.op("act", [z_], [z_], lambda e: e.activation(out=z_[:, 0:nt_], in_=z_[:, 0:nt_], func=AF.Sigmoid, scale=1.5957691216057308))
            tt(g_, g_[:, 0:nt_], y_, y_[:, 0:nt_], z_, z_[:, 0:nt_], ALU.mult)
            P.dma(gTo, gTo[:, col0:col0 + nt_], g_, g_[:, 0:nt_])
    P.finish()
    return P


ANTI_BF = np.ascontiguousarray(np.eye(128, dtype=np.float32)[::-1]).astype(NPBF)


def run_s5_layer(i, j, ctx_out, x, ctx, wb, modv, inputs):
    P = _prog(("s5",), build_s5)
    in_maps = []
    for c in range(NCORES):
        ch = slice(128 * c, 128 * c + 128)
        gsl = slice(8 * c, 8 * c + 8)
        xtok = np.ascontiguousarray(np.concatenate([ctx[:, :, ch], x[:, :, ch]], axis=1))
        xT = np.ascontiguousarray(xtok.transpose(2, 0, 1).reshape(128, -1))
        mrow = np.ascontiguousarray(np.stack([modv[i, :, 0 * D:1 * D][:, ch], modv[i, :, 1 * D:2 * D][:, ch]], axis=1))
        mcol = np.ascontiguousarray(mrow.transpose(2, 0, 1).reshape(128, 6))

        def pg(a):
            return np.ascontiguousarray(a[:, gsl, :].transpose(2, 0, 1).reshape(64, 16)).astype(np.float32)
        bre = np.ascontiguousarray(inputs["s5_b_re"][j][:, gsl].transpose(2, 0, 1, 3).reshape(64, 256)).astype(np.float32)
        bim = np.ascontiguousarray(inputs["s5_b_im"][j][:, gsl].transpose(2, 0, 1, 3).reshape(64, 256)).astype(np.float32)
        cre = np.ascontiguousarray(inputs["s5_c_re"][j][:, gsl].transpose(3, 0, 1, 2).reshape(64, 256)).astype(np.float32)
        cim = np.ascontiguousarray(inputs["s5_c_im"][j][:, gsl].transpose(3, 0, 1, 2).reshape(64, 256)).astype(np.float32)
        in_maps.append({"ident_in": IDENT, "anti_in": ANTI_BF, "xtok": xtok, "xT": xT, "mrow": mrow, "mcol": mcol,
                        "are": pg(inputs["s5_a_re"][j]), "aim": pg(inputs["s5_a_im"][j]),
                        "ldt": np.ascontiguousarray(inputs["s5_log_dt"][j][:, gsl].reshape(1, 16)).astype(np.float32),
                        "bre": bre, "bim": bim, "cre": cre, "cim": cim,
                        "dsk": np.ascontiguousarray(inputs["s5_d"][j][ch].reshape(128, 1)).astype(np.float32)})
    res = run_prog(P, in_maps)
    gT = np.concatenate([np.asarray(r["gT"]) for r in res], axis=0)
    ntk = NCH * 128
    aT = []
    for c in range(NCORES):
        b, q = core_bq(c)
        lat = gT[:, b * ntk + NCTX + q * NLAT: b * ntk + NCTX + (q + 1) * NLAT]
        if ctx_out:
            lat = np.concatenate([lat, gT[:, b * ntk: b * ntk + NCTX]], axis=1)
        aT.append(np.ascontiguousarray(lat))
    return run_tail("s5", ctx_out, i, aT, x, ctx, [wb["s5_w_val"][j], wb["s5_w_gate"][j]], wb, modv, inputs), gT


def kernel(**inputs):
    inputs = {k: np.asarray(v) for k, v in inputs.items()}
    wb, modv = run_prep(inputs)
    x = np.ascontiguousarray(inputs["x"], dtype=np.float32)
    ctx = np.ascontiguousarray(inputs["ctx"], dtype=np.float32)
    (x, ctx), _ = run_s5_layer(0, 0, True, x, ctx, wb, modv, inputs)
    (x, ctx), _ = run_attn_layer(1, "swa", True, x, ctx, wb, modv, inputs)
    (x, ctx), _ = run_attn_layer(2, "gqa", True, x, ctx, wb, modv, inputs)
    (x, ctx), _ = run_s5_layer(3, 1, False, x, ctx, wb, modv, inputs)
    return np.ascontiguousarray(x, dtype=np.float32)
```

```python
import numpy as np
from contextlib import ExitStack
import concourse.bass as bass
import concourse.mybir as mybir
from concourse.bass_utils import run_bass_kernel_spmd
import ml_dtypes

F32 = mybir.dt.float32
BF16 = mybir.dt.bfloat16
ALU = mybir.AluOpType
AF = mybir.ActivationFunctionType
AX = mybir.AxisListType
NPBF = ml_dtypes.bfloat16

EPOCH = 30000
NCORES = 8
D = 1024
SEQ = 16384
BATCH = 2
NCTX = 256
DEPTH = 4
NE = 16
DEXP = 512
ALPHA = (2.0 * DEPTH) ** 0.25
LN_EPS = 1e-6
BIG = 1.0e4
DEBUG = False


class Buf:
    def __init__(self, name, h, kind):
        self.name, self.h, self.kind = name, h, kind
        self.lw = None
        self.rd = []

    def __getitem__(self, idx):
        return self.h[idx]


class DSem:
    def __init__(self, h):
        self.h = h
        self.total = 0


class Prog:
    def __init__(self):
        self.nc = bass.Bass("TRN2", target_bir_lowering=False)
        self.es = ExitStack()
        nc = self.nc
        self.engs = {"pe": nc.tensor, "act": nc.scalar, "dve": nc.vector,
                     "pool": nc.gpsimd, "sp": nc.sync}
        self.esem = {e: [] for e in self.engs}
        self.ecnt = {e: 0 for e in self.engs}
        self.seen = {e: {} for e in self.engs}
        self.nsem = 0
        self.ninst = {e: 0 for e in self.engs}
        self.dsems = {}
        self.out_events = []
        self.uid = 0
        self.log = {e: [] for e in self.engs}
        for e in self.engs:
            self._new_epoch(e)

    def _sem(self, name):
        self.nsem += 1
        return self.es.enter_context(self.nc.semaphore(f"{name}_{self.nsem}"))

    def _new_epoch(self, e):
        self.esem[e].append(self._sem(f"s_{e}"))
        self.ecnt[e] = 0

    def sbuf(self, name, shape, dt):
        h = self.es.enter_context(self.nc.sbuf_tensor(name, list(shape), dt))
        return Buf(name, h, "sb")

    def psum(self, name, shape, dt=F32):
        h = self.es.enter_context(self.nc.psum_tensor(name, list(shape), dt))
        return Buf(name, h, "ps")

    def dram(self, name, shape, dt, kind="Internal"):
        t = self.nc.dram_tensor(name, list(shape), dt, kind=kind)
        return Buf(name, t.ap(), "dr")

    def dsem_for(self, buf):
        if buf.name not in self.dsems:
            self.dsems[buf.name] = DSem(self._sem("d_" + buf.name[:12]))
        return self.dsems[buf.name]

    def _wait(self, eng, ev):
        if ev is None:
            return
        if ev[0] == "e":
            _, e2, ep, cnt = ev
            key = ("e", e2, ep)
            sem = self.esem[e2][ep]
            val = cnt
        else:
            _, ds = ev
            key = ("d", id(ds))
            sem = ds.h
            val = ds.total
        if self.seen[eng].get(key, 0) >= val:
            return
        self.seen[eng][key] = val
        self.log[eng].append(("wait", key, val))
        self.engs[eng].wait_ge(sem, val)
        self.ninst[eng] += 1

    def _deps(self, eng, reads, writes):
        evs = []
        for b in reads:
            if b.lw is not None:
                evs.append(b.lw)
        for b in writes:
            if b.lw is not None:
                evs.append(b.lw)
            for ev in b.rd:
                if ev[0] == "e" and ev[1] == eng:
                    continue
                evs.append(ev)
        for ev in evs:
            if ev[0] == "e" and ev[1] == eng and eng == "pe":
                continue
            self._wait(eng, ev)

    def _record(self, ev, reads, writes):
        for b in reads:
            b.rd.append(ev)
            if len(b.rd) > 48:
                b.rd = b.rd[-48:]
        for b in writes:
            b.lw = ev
            b.rd = []

    def op(self, eng, reads, writes, fn, n_inst=1):
        self._deps(eng, reads, writes)
        if self.ecnt[eng] >= EPOCH:
            self._new_epoch(eng)
        ins = fn(self.engs[eng])
        self.ecnt[eng] += 1
        ep = len(self.esem[eng]) - 1
        ins.then_inc(self.esem[eng][ep], 1)
        self.log[eng].append(("inc", ("e", eng, ep), 1, False))
        ev = ("e", eng, ep, self.ecnt[eng])
        self.ninst[eng] += n_inst
        self._record(ev, reads, writes)
        return ev

    def dma(self, out_buf, out_ap, in_buf, in_ap, q="sp", sem_buf=None, **kw):
        if sem_buf is None:
            sem_buf = out_buf if out_buf.kind != "dr" else in_buf
        ds = self.dsem_for(sem_buf)
        self._deps(q, [in_buf], [out_buf])
        ins = self.engs[q].dma_start(out=out_ap, in_=in_ap, **kw)
        ds.total += 16
        ins.then_inc(ds.h, 16)
        self.log[q].append(("inc", ("d", id(ds)), 16, True))
        ev = ("d", ds)
        self.ninst[q] += 1
        self._record(ev, [in_buf], [out_buf])
        if out_buf.kind == "dr":
            self.out_events.append(ev)
        return ev

    def finish(self):
        for ev in self.out_events:
            self._wait("sp", ev)
        self.es.close()


def run_prog(P, in_maps):
    res = run_bass_kernel_spmd(P.nc, in_maps, core_ids=list(range(NCORES)))
    return res.results


def make_ident(P, name="ident", dt=F32):
    src = P.dram(name + "_in", [128, 128], dt, kind="ExternalInput")
    t = P.sbuf(name, [128, 128], dt)
    P.dma(t, t[:], src, src[:])
    return t


def layer_norm_tile(P, X, xap, gB, bB, tmp_stats, epst):
    st, mv, rstd = tmp_stats
    for h in range(2):
        P.op("dve", [X], [st], lambda e, h=h: e.bn_stats(out=st[:, h * 6:(h + 1) * 6],
                                                         in_=xap[:, h * 512:(h + 1) * 512]))
    P.op("dve", [st], [mv], lambda e: e.bn_aggr(out=mv[:], in_=st[:]))
    P.op("act", [mv, epst], [rstd], lambda e: e.activation(out=rstd[:, 0:1], in_=mv[:, 1:2], func=AF.Sqrt,
                                                           bias=epst[:, 0:1], scale=1.0))
    P.op("dve", [rstd], [rstd], lambda e: e.reciprocal(out=rstd[:, 1:2], in_=rstd[:, 0:1]))
    P.op("dve", [X, mv, rstd], [X], lambda e: e.tensor_scalar(out=xap, in0=xap, scalar1=mv[:, 0:1],
                                                              scalar2=rstd[:, 1:2], op0=ALU.subtract,
                                                              op1=ALU.mult))
    P.op("dve", [X, gB], [X], lambda e: e.tensor_tensor(out=xap, in0=xap, in1=gB[:], op=ALU.mult))
    P.op("dve", [X, bB], [X], lambda e: e.tensor_tensor(out=xap, in0=xap, in1=bB[:], op=ALU.add))


PREP_CH = 4096


def build_prep(ncols):
    P = Prog()
    w = P.dram("wflat", [128, ncols], F32, kind="ExternalInput")
    wo = P.dram("wbf", [128, ncols], BF16, kind="ExternalOutput")
    cT = P.dram("cT", [128, 8, 3], F32, kind="ExternalInput")
    mw = P.dram("modw", [DEPTH, D, 768], F32, kind="ExternalInput")
    mb = P.dram("modb", [DEPTH, 768], F32, kind="ExternalInput")
    mo = P.dram("modv", [DEPTH, 3, 768], F32, kind="ExternalOutput")
    nch = ncols // PREP_CH
    stg = [P.sbuf(f"stg{i}", [128, PREP_CH], F32) for i in range(3)]
    obf = [P.sbuf(f"obf{i}", [128, PREP_CH], BF16) for i in range(3)]
    cs = P.sbuf("cs", [128, 8, 3], F32)
    P.dma(cs, cs[:], cT, cT[:])
    ss = P.sbuf("ss", [128, 8, 3], F32)
    P.op("act", [cs], [ss], lambda e: e.activation(out=ss[:], in_=cs[:], func=AF.Silu))
    mws = [P.sbuf(f"mws{i}", [128, 8, 768], F32) for i in range(2)]
    mbs = P.sbuf("mbs", [3, DEPTH, 768], F32)
    for i in range(DEPTH):
        P.dma(mbs, mbs[:, i, :], mb, mb[i:i + 1, :].partition_broadcast(3))
    pm = [P.psum(f"pm{i}", [3, 512], F32) for i in range(4)]
    mres = P.sbuf("mres", [3, DEPTH, 768], F32)
    for i in range(DEPTH):
        ws = mws[i % 2]
        P.dma(ws, ws[:], mw, mw[i].rearrange("(kc p) n -> p kc n", p=128))
        for h in range(2):
            pp = pm[(2 * i + h) % 4]

            def mm(e, ws=ws, pp=pp, h=h):
                for kc in range(8):
                    ins = e.matmul(pp[:, 0:384], lhsT=ss[:, kc, :], rhs=ws[:, kc, h * 384:(h + 1) * 384],
                                   start=(kc == 0), stop=(kc == 7))
                return ins
            P.op("pe", [ss, ws], [pp], mm, n_inst=8)
            P.op("dve", [pp, mbs], [mres], lambda e, pp=pp, h=h, i=i: e.tensor_tensor(
                out=mres[:, i, h * 384:(h + 1) * 384], in0=pp[:, 0:384],
                in1=mbs[:, i, h * 384:(h + 1) * 384], op=ALU.add))
    P.dma(mo, mo[:].rearrange("l r n -> r l n"), mres, mres[:])
    engs = ["dve", "act"]

    def load(c):
        P.dma(stg[c % 3], stg[c % 3][:], w, w[:, c * PREP_CH:(c + 1) * PREP_CH])
    for c in range(min(2, nch)):
        load(c)
    for c in range(nch):
        s = stg[c % 3]
        o = obf[c % 3]
        if c + 2 < nch:
            load(c + 2)
        if engs[c % 2] == "act":
            P.op("act", [s], [o], lambda e, s=s, o=o: e.activation(out=o[:], in_=s[:], func=AF.Copy))
        else:
            P.op("dve", [s], [o], lambda e, s=s, o=o: e.tensor_copy(out=o[:], in_=s[:]))
        P.dma(wo, wo[:, c * PREP_CH:(c + 1) * PREP_CH], o, o[:])
    P.finish()
    return P


CAST_NAMES = ["moe_w1", "moe_w3", "moe_w2", "s5_w_gate", "s5_w_val", "swa_w_qkv", "swa_w_o",
              "gqa_w_qkv", "gqa_w_o", "router_w"]


def run_prep(inputs):
    flats = [np.ascontiguousarray(inputs[n], dtype=np.float32).reshape(-1) for n in CAST_NAMES]
    sizes = [f.size for f in flats]
    tot = sum(sizes)
    unit = NCORES * 128 * PREP_CH
    padded = ((tot + unit - 1) // unit) * unit
    flat = np.zeros(padded, np.float32)
    flat[:tot] = np.concatenate(flats)
    ncols = padded // (NCORES * 128)
    per = flat.reshape(NCORES, 128, ncols)
    c3 = np.concatenate([inputs["c"], inputs["c_ctx"][None, :]], 0).astype(np.float32)
    cT = np.ascontiguousarray(c3.reshape(3, 8, 128).transpose(2, 1, 0))
    P = build_prep(ncols)
    in_maps = []
    for c in range(NCORES):
        in_maps.append({
            "wflat": per[c], "cT": cT,
            "modw": np.ascontiguousarray(inputs["mod_w"][:, :, c * 768:(c + 1) * 768]),
            "modb": np.ascontiguousarray(inputs["mod_b"][:, c * 768:(c + 1) * 768]),
        })
    res = run_prog(P, in_maps)
    wbf = np.concatenate([np.asarray(r["wbf"]).reshape(-1) for r in res])[:tot]
    out = {}
    off = 0
    for n, sz in zip(CAST_NAMES, sizes):
        out[n] = wbf[off:off + sz].reshape(inputs[n].shape)
        off += sz
    modv = np.concatenate([np.asarray(r["modv"]) for r in res], axis=2)
    return out, modv


TG = 1024
NLAT = SEQ // 4


def build_tail(kind, ctx_out):
    P = Prog()
    ntok = NLAT + (NCTX if ctx_out else 0)
    groups = [(g * TG, TG, 0) for g in range(NLAT // TG)]
    if ctx_out:
        groups.append((NLAT, NCTX, 1))
    aT = P.dram("aT", [D, ntok], BF16, kind="ExternalInput")
    wp0 = P.dram("wp0", [D, D], BF16, kind="ExternalInput")
    wp1 = P.dram("wp1", [D, D], BF16, kind="ExternalInput") if kind == "s5" else None
    xin = P.dram("xin", [ntok, D], F32, kind="ExternalInput")
    modrows = P.dram("modrows", [2, 6 * D], F32, kind="ExternalInput")
    modcols = P.dram("modcols", [128, 2 * 6 * 8], F32, kind="ExternalInput")
    lng = P.dram("lng", [2, D], F32, kind="ExternalInput")
    lnb = P.dram("lnb", [2, D], F32, kind="ExternalInput")
    rw = P.dram("rw", [D, NE], BF16, kind="ExternalInput")
    rb = P.dram("rb", [1, NE], F32, kind="ExternalInput")
    w1 = P.dram("w1", [NE, D, DEXP], BF16, kind="ExternalInput")
    w3 = P.dram("w3", [NE, D, DEXP], BF16, kind="ExternalInput")
    w2 = P.dram("w2", [NE, DEXP, D], BF16, kind="ExternalInput")
    xout = P.dram("xout", [ntok, D], F32, kind="ExternalOutput")
    x1scr = P.dram("x1scr", [ntok, D], F32, kind="ExternalOutput" if DEBUG else "Internal")
    combo = P.dram("combo", [ntok, NE], F32, kind="ExternalOutput") if DEBUG else None

    ident = make_ident(P)
    epst = P.sbuf("epst", [128, 1], F32)
    P.op("dve", [], [epst], lambda e: e.memset(epst[:], LN_EPS))
    aTs = P.sbuf("aTs", [128, 8, TG], BF16)
    h2T = P.sbuf("h2T", [128, 8, TG], BF16)
    wps = [P.sbuf("wps0", [128, 8, D], BF16)]
    P.dma(wps[0], wps[0][:], wp0, wp0[:].rearrange("(kc p) n -> p kc n", p=128))
    if kind == "s5":
        wps.append(P.sbuf("wps1", [128, 8, D], BF16))
        P.dma(wps[1], wps[1][:], wp1, wp1[:].rearrange("(kc p) n -> p kc n", p=128))
    xb = [P.sbuf(f"xb{i}", [128, D], F32) for i in range(TG // 128)]
    w13b = [P.sbuf(f"w13b{i}", [128, 2, 8, DEXP], BF16) for i in range(2)]
    w2b = [P.sbuf(f"w2b{i}", [128, 4, D], BF16) for i in range(2)]
    gTb = [P.sbuf(f"gTb{i}", [128, 4, 512], BF16) for i in range(2)]
    silu_t = [P.sbuf(f"silu{i}", [128, 512], F32) for i in range(2)]
    tmpXs = [P.sbuf(f"tmpX{i}", [128, D], F32) for i in range(2)]
    tmpA = [P.sbuf(f"tmpA{i}", [128, 512], F32) for i in range(2)]
    sgt = [P.sbuf(f"sgt{i}", [128, 512], F32) for i in range(2)]
    g1B = P.sbuf("g1B", [128, D], F32)
    g2B = P.sbuf("g2B", [128, D], F32)
    lnGB = [P.sbuf(f"lnGB{i}", [128, D], F32) for i in range(2)]
    lnBB = [P.sbuf(f"lnBB{i}", [128, D], F32) for i in range(2)]
    mcols = P.sbuf("mcols", [128, 2, 6, 8], F32)
    P.dma(mcols, mcols[:].rearrange("p a b c -> p (a b c)"), modcols, modcols[:])
    P.op("dve", [mcols], [mcols], lambda e: e.tensor_scalar(out=mcols[:, :, 4, :], in0=mcols[:, :, 4, :],
                                                            scalar1=1.0, scalar2=None, op0=ALU.add))
    rws = P.sbuf("rws", [128, 8, NE], BF16)
    P.dma(rws, rws[:], rw, rw[:].rearrange("(kc p) n -> p kc n", p=128))
    rbB = P.sbuf("rbB", [128, NE], F32)
    P.dma(rbB, rbB[:], rb, rb[0:1, :].partition_broadcast(128))
    for i in range(2):
        P.dma(lnGB[i], lnGB[i][:], lng, lng[i:i + 1, :].partition_broadcast(128))
        P.dma(lnBB[i], lnBB[i][:], lnb, lnb[i:i + 1, :].partition_broadcast(128))
    comb = P.sbuf("comb", [128, TG // 128, NE], F32)
    rt = {n: P.sbuf("rt_" + n, [128, NE], F32) for n in
          ["aff", "sel", "eq1", "sel2", "masked", "e1", "masked2", "e2", "gate"]}
    rs = {n: P.sbuf("rs_" + n, [128, 4], F32) for n in ["m1", "m2", "gs", "gmask", "pen"]}
    r1 = {n: P.sbuf("r1_" + n, [128, 1], F32) for n in ["gm", "t1", "t2", "den", "rden"]}
    stats = (P.sbuf("st", [128, 12], F32), P.sbuf("mv", [128, 2], F32), P.sbuf("rstd", [128, 2], F32))
    pb = [P.psum(f"pb{i}", [128, 512], F32) for i in range(8)]

    jobs = [(gi, e) for gi in range(len(groups)) for e in range(NE)]
    wstate = {"next": 0}

    def prefetch_weights():
        j = wstate["next"]
        if j >= len(jobs):
            return
        _, e = jobs[j]
        b13, b2 = w13b[j % 2], w2b[j % 2]
        P.dma(b13, b13[:, 0], w1, w1[e].rearrange("(kc p) n -> p kc n", p=128))
        P.dma(b13, b13[:, 1], w3, w3[e].rearrange("(kc p) n -> p kc n", p=128))
        P.dma(b2, b2[:], w2, w2[e].rearrange("(mc p) n -> p mc n", p=128))
        wstate["next"] = j + 1

    prefetch_weights()
    prefetch_weights()
    cur_row = [-1]

    def load_row(r):
        if cur_row[0] == r:
            return
        cur_row[0] = r
        P.dma(g1B, g1B[:], modrows, modrows[r:r + 1, 2 * D:3 * D].partition_broadcast(128))
        P.dma(g2B, g2B[:], modrows, modrows[r:r + 1, 5 * D:6 * D].partition_broadcast(128))

    def topk(t, lp):
        A = rt
        P.op("act", [lp], [A["aff"]], lambda e: e.activation(out=A["aff"][:], in_=lp[:, 0:NE], func=AF.Sigmoid))
        P.op("dve", [A["aff"], rbB], [A["sel"]], lambda e: e.tensor_tensor(
            out=A["sel"][:], in0=A["aff"][:], in1=rbB[:], op=ALU.add))
        sel3 = A["sel"][:].rearrange("p (g k) -> p g k", k=4)
        P.op("dve", [A["sel"]], [rs["m1"]], lambda e: e.tensor_reduce(out=rs["m1"][:], in_=sel3, axis=AX.X, op=ALU.max))
        eq3 = A["eq1"][:].rearrange("p (g k) -> p g k", k=4)
        P.op("dve", [A["sel"], rs["m1"]], [A["eq1"]], lambda e: e.tensor_tensor(
            out=eq3, in0=sel3, in1=rs["m1"][:].unsqueeze(2).to_broadcast([128, 4, 4]), op=ALU.is_equal))
        P.op("dve", [A["eq1"], A["sel"]], [A["sel2"]], lambda e: e.scalar_tensor_tensor(
            out=A["sel2"][:], in0=A["eq1"][:], scalar=-BIG, in1=A["sel"][:], op0=ALU.mult, op1=ALU.add))
        P.op("dve", [A["sel2"]], [rs["m2"]], lambda e: e.tensor_reduce(
            out=rs["m2"][:], in_=A["sel2"][:].rearrange("p (g k) -> p g k", k=4), axis=AX.X, op=ALU.max))
        P.op("dve", [rs["m1"], rs["m2"]], [rs["gs"]], lambda e: e.tensor_tensor(
            out=rs["gs"][:], in0=rs["m1"][:], in1=rs["m2"][:], op=ALU.add))
        P.op("dve", [rs["gs"]], [r1["gm"]], lambda e: e.tensor_reduce(out=r1["gm"][:], in_=rs["gs"][:], axis=AX.X, op=ALU.max))
        P.op("dve", [rs["gs"], r1["gm"]], [rs["pen"]], lambda e: e.tensor_scalar(
            out=rs["pen"][:], in0=rs["gs"][:], scalar1=r1["gm"][:, 0:1], scalar2=-BIG, op0=ALU.is_lt, op1=ALU.mult))
        P.op("dve", [A["sel"], rs["pen"]], [A["masked"]], lambda e: e.tensor_tensor(
            out=A["masked"][:].rearrange("p (g k) -> p g k", k=4), in0=sel3,
            in1=rs["pen"][:].unsqueeze(2).to_broadcast([128, 4, 4]), op=ALU.add))
        P.op("dve", [A["masked"]], [r1["t1"]], lambda e: e.tensor_reduce(out=r1["t1"][:], in_=A["masked"][:], axis=AX.X, op=ALU.max))
        P.op("dve", [A["masked"], r1["t1"]], [A["e1"]], lambda e: e.tensor_scalar(
            out=A["e1"][:], in0=A["masked"][:], scalar1=r1["t1"][:, 0:1], scalar2=None, op0=ALU.is_equal))
        P.op("dve", [A["e1"], A["masked"]], [A["masked2"]], lambda e: e.scalar_tensor_tensor(
            out=A["masked2"][:], in0=A["e1"][:], scalar=-BIG, in1=A["masked"][:], op0=ALU.mult, op1=ALU.add))
        P.op("dve", [A["masked2"]], [r1["t2"]], lambda e: e.tensor_reduce(out=r1["t2"][:], in_=A["masked2"][:], axis=AX.X, op=ALU.max))
        P.op("dve", [A["masked2"], r1["t2"]], [A["e2"]], lambda e: e.tensor_scalar(
            out=A["e2"][:], in0=A["masked2"][:], scalar1=r1["t2"][:, 0:1], scalar2=None, op0=ALU.is_equal))
        P.op("dve", [A["e1"], A["e2"]], [A["e1"]], lambda e: e.tensor_tensor(
            out=A["e1"][:], in0=A["e1"][:], in1=A["e2"][:], op=ALU.add))
        P.op("dve", [A["e1"], A["aff"]], [A["gate"]], lambda e: e.tensor_tensor(
            out=A["gate"][:], in0=A["e1"][:], in1=A["aff"][:], op=ALU.mult))
        P.op("dve", [A["gate"]], [r1["den"]], lambda e: e.tensor_reduce(out=r1["den"][:], in_=A["gate"][:], axis=AX.X, op=ALU.add))
        P.op("dve", [r1["den"]], [r1["rden"]], lambda e: e.reciprocal(out=r1["rden"][:], in_=r1["den"][:]))
        P.op("dve", [A["gate"], r1["rden"]], [comb], lambda e: e.tensor_scalar(
            out=comb[:, t, :], in0=A["gate"][:], scalar1=r1["rden"][:, 0:1], scalar2=None, op0=ALU.mult))

    cnt = {"pbB": 0, "tmp": 0}

    for gi, (g0, tg, row) in enumerate(groups):
        nt = tg // 128
        load_row(row)
        P.dma(aTs, aTs[:, :, 0:tg], aT, aT[:, g0:g0 + tg].rearrange("(kc p) n -> p kc n", p=128))
        for t in range(nt):
            P.dma(xb[t], xb[t][:], xin, xin[g0 + t * 128:g0 + (t + 1) * 128, :])

        def phaseBC(t):
            xbuf = xb[t]
            xa = xbuf[:]
            for h in range(2):
                base = (cnt["pbB"] % 2) * 4
                cnt["pbB"] += 1
                tA = tmpA[cnt["tmp"] % 2]
                sg = sgt[cnt["tmp"] % 2]
                cnt["tmp"] += 1
                pv = pb[base + 0]

                def mm(e, pp, wsb):
                    for kc in range(8):
                        ins = e.matmul(pp[:], lhsT=aTs[:, kc, t * 128:(t + 1) * 128],
                                       rhs=wsb[:, kc, h * 512:(h + 1) * 512], start=(kc == 0), stop=(kc == 7))
                    return ins
                P.op("pe", [aTs, wps[0]], [pv], lambda e: mm(e, pv, wps[0]), n_inst=8)
                if kind == "s5":
                    pg = pb[base + 1]
                    P.op("pe", [aTs, wps[1]], [pg], lambda e: mm(e, pg, wps[1]), n_inst=8)
                    P.op("act", [pg], [sg], lambda e: e.activation(out=sg[:], in_=pg[:], func=AF.Sigmoid))
                    P.op("dve", [pv, sg], [tA], lambda e: e.tensor_tensor(out=tA[:], in0=pv[:], in1=sg[:], op=ALU.mult))
                    P.op("dve", [tA, g1B], [tA], lambda e: e.tensor_tensor(
                        out=tA[:], in0=tA[:], in1=g1B[:, h * 512:(h + 1) * 512], op=ALU.mult))
                else:
                    P.op("dve", [pv, g1B], [tA], lambda e: e.tensor_tensor(
                        out=tA[:], in0=pv[:], in1=g1B[:, h * 512:(h + 1) * 512], op=ALU.mult))
                P.op("dve", [xbuf, tA], [xbuf], lambda e: e.scalar_tensor_tensor(
                    out=xa[:, h * 512:(h + 1) * 512], in0=xa[:, h * 512:(h + 1) * 512], scalar=ALPHA,
                    in1=tA[:], op0=ALU.mult, op1=ALU.add))
            layer_norm_tile(P, xbuf, xa, lnGB[0], lnBB[0], stats, epst)
            P.dma(x1scr, x1scr[g0 + t * 128:g0 + (t + 1) * 128, :], xbuf, xa)

        def phaseDE(t):
            xbuf = xb[t]
            xa = xbuf[:]
            for half in range(2):
                pt = pb[2 + half] if True else None

                def tr(e, pt=pt, half=half):
                    for j in range(4):
                        kc = half * 4 + j
                        ins = e.transpose(pt[:, j * 128:(j + 1) * 128], xa[:, kc * 128:(kc + 1) * 128], ident[:])
                    return ins
                P.op("pe", [xbuf, ident], [pt], tr, n_inst=4)
                for j in range(4):
                    kc = half * 4 + j
                    P.op("act", [pt, mcols], [h2T], lambda e, kc=kc, j=j, pt=pt: e.activation(
                        out=h2T[:, kc, t * 128:(t + 1) * 128], in_=pt[:, j * 128:(j + 1) * 128],
                        func=AF.Identity, scale=mcols[:, row, 4, kc:kc + 1], bias=mcols[:, row, 3, kc:kc + 1]))
            lp = pb[6]

            def rmm(e):
                for kc in range(8):
                    ins = e.matmul(lp[:, 0:NE], lhsT=h2T[:, kc, t * 128:(t + 1) * 128], rhs=rws[:, kc, :],
                                   start=(kc == 0), stop=(kc == 7))
                return ins
            P.op("pe", [h2T, rws], [lp], rmm, n_inst=8)
            topk(t, lp)

        for t in range(nt + 1):
            if t < nt:
                phaseBC(t)
            if t >= 1:
                phaseDE(t - 1)

        if DEBUG:
            for t in range(nt):
                P.dma(combo, combo[g0 + t * 128:g0 + (t + 1) * 128, :], comb, comb[:, t, :])
        blocks = [(b0, min(512, tg - b0)) for b0 in range(0, tg, 512)]
        items = [(e, bi) for e in range(NE) for bi in range(len(blocks))]
        l1cnt = [0]

        def L1(idx):
            e, bi = items[idx]
            b0, bn = blocks[bi]
            j = gi * NE + e
            b13 = w13b[j % 2]
            gT = gTb[idx % 2]
            for m in range(4):
                k2 = l1cnt[0] % 2
                l1cnt[0] += 1
                p1, p3 = pb[k2 * 2], pb[k2 * 2 + 1]

                def mm(e_, pp, which):
                    for kc in range(8):
                        ins = e_.matmul(pp[:, 0:bn], lhsT=b13[:, which, kc, m * 128:(m + 1) * 128],
                                        rhs=h2T[:, kc, b0:b0 + bn], start=(kc == 0), stop=(kc == 7))
                    return ins
                P.op("pe", [b13, h2T], [p1], lambda e_: mm(e_, p1, 0), n_inst=8)
                P.op("pe", [b13, h2T], [p3], lambda e_: mm(e_, p3, 1), n_inst=8)
                sl = silu_t[k2]
                P.op("act", [p1], [sl], lambda e_: e_.activation(out=sl[:, 0:bn], in_=p1[:, 0:bn], func=AF.Silu))
                P.op("dve", [sl, p3], [gT], lambda e_: e_.tensor_tensor(
                    out=gT[:, m, 0:bn], in0=sl[:, 0:bn], in1=p3[:, 0:bn], op=ALU.mult))

        l2cnt = [0]

        def L2(idx):
            e, bi = items[idx]
            b0, bn = blocks[bi]
            j = gi * NE + e
            b2 = w2b[j % 2]
            gT = gTb[idx % 2]
            for s in range(bn // 128):
                t = (b0 // 128) + s
                for h in range(2):
                    po = pb[4 + (l2cnt[0] % 4)]
                    l2cnt[0] += 1

                    def mm(e_, po=po):
                        for m in range(4):
                            ins = e_.matmul(po[:], lhsT=gT[:, m, s * 128:(s + 1) * 128],
                                            rhs=b2[:, m, h * 512:(h + 1) * 512], start=(m == 0), stop=(m == 3))
                        return ins
                    P.op("pe", [gT, b2], [po], mm, n_inst=4)
                    xbuf = xb[t]
                    ya = xbuf[:, h * 512:(h + 1) * 512]
                    if e == 0:
                        P.op("dve", [po, comb], [xbuf], lambda e_, po=po, ya=ya, t=t: e_.tensor_scalar(
                            out=ya, in0=po[:], scalar1=comb[:, t, e:e + 1], scalar2=None, op0=ALU.mult))
                    else:
                        P.op("dve", [po, comb, xbuf], [xbuf], lambda e_, po=po, ya=ya, t=t: e_.scalar_tensor_tensor(
                            out=ya, in0=po[:], scalar=comb[:, t, e:e + 1], in1=ya, op0=ALU.mult, op1=ALU.add))

        L1(0)
        for idx in range(len(items)):
            if idx + 1 < len(items):
                L1(idx + 1)
            L2(idx)
            if items[idx][1] == len(blocks) - 1:
                prefetch_weights()

        def ld(t):
            P.dma(tmpXs[t % 2], tmpXs[t % 2][:], x1scr, x1scr[g0 + t * 128:g0 + (t + 1) * 128, :])
        ld(0)
        for t in range(nt):
            if t + 1 < nt:
                ld(t + 1)
            xbuf = xb[t]
            xa = xbuf[:]
            tmpX = tmpXs[t % 2]
            P.op("dve", [xbuf, g2B], [xbuf], lambda e: e.tensor_tensor(out=xa, in0=xa, in1=g2B[:], op=ALU.mult))
            P.op("dve", [tmpX, xbuf], [xbuf], lambda e: e.scalar_tensor_tensor(
                out=xa, in0=tmpX[:], scalar=ALPHA, in1=xa, op0=ALU.mult, op1=ALU.add))
            layer_norm_tile(P, xbuf, xa, lnGB[1], lnBB[1], stats, epst)
            P.dma(xout, xout[g0 + t * 128:g0 + (t + 1) * 128, :], xbuf, xa)
    P.finish()
    return P


RMS_EPS = 1e-6


def build_qkv(hd, H, KV, qknorm, ctx_rows):
    P = Prog()
    ntok = NLAT + ctx_rows
    nqkv = (H + 2 * KV) * hd
    xin = P.dram("xin", [ntok, D], F32, kind="ExternalInput")
    modcols = P.dram("modcols", [128, 2 * 6 * 8], F32, kind="ExternalInput")
    wq = P.dram("wqkv", [D, nqkv], BF16, kind="ExternalInput")
    cosd = P.dram("cos", [NLAT, hd // 2], F32, kind="ExternalInput")
    sind = P.dram("sin", [NLAT, hd // 2], F32, kind="ExternalInput")
    qTo = P.dram("qT", [hd, H, ntok], BF16, kind="ExternalOutput")
    kTo = P.dram("kT", [hd, KV, ntok], BF16, kind="ExternalOutput")
    vo = P.dram("v", [ntok, KV * hd], BF16, kind="ExternalOutput")
    if qknorm:
        qg = P.dram("qg", [1, hd], F32, kind="ExternalInput")
        kg = P.dram("kg", [1, hd], F32, kind="ExternalInput")
    ident = make_ident(P)
    mcols = P.sbuf("mcols", [128, 2, 6, 8], F32)
    P.dma(mcols, mcols[:].rearrange("p a b c -> p (a b c)"), modcols, modcols[:])
    P.op("dve", [mcols], [mcols], lambda e: e.tensor_scalar(out=mcols[:, :, 1, :], in0=mcols[:, :, 1, :],
                                                            scalar1=1.0, scalar2=None, op0=ALU.add))
    ws = P.sbuf("ws", [128, 8, nqkv], BF16)
    P.dma(ws, ws[:], wq, wq[:].rearrange("(kc p) n -> p kc n", p=128))
    epst = P.sbuf("epst", [128, 1], F32)
    P.op("dve", [], [epst], lambda e: e.memset(epst[:], RMS_EPS))
    if qknorm:
        qgB = P.sbuf("qgB", [128, hd], F32)
        kgB = P.sbuf("kgB", [128, hd], F32)
        P.dma(qgB, qgB[:], qg, qg[0:1, :].partition_broadcast(128))
        P.dma(kgB, kgB[:], kg, kg[0:1, :].partition_broadcast(128))
    xs = [P.sbuf(f"xs{i}", [128, D], F32) for i in range(2)]
    hT = [P.sbuf(f"hT{i}", [128, 8, 128], BF16) for i in range(2)]
    qkv = [P.sbuf(f"qkv{i}", [128, nqkv], F32) for i in range(2)]
    cs = [P.sbuf(f"cs{i}", [128, 2, hd // 2], F32) for i in range(2)]
    NH = H + KV
    ro = [P.sbuf(f"ro{i}", [128, NH * hd], F32) for i in range(2)]
    t1 = P.sbuf("t1", [128, NH * hd // 2], F32)
    t2 = P.sbuf("t2", [128, NH * hd // 2], F32)
    sq = P.sbuf("sq", [128, NH * hd], F32)
    ms = P.sbuf("ms", [128, NH], F32)
    rs = P.sbuf("rs", [128, NH], F32)
    vb = [P.sbuf(f"vb{i}", [128, KV * hd], BF16) for i in range(2)]
    qTs = [P.sbuf(f"qTs{i}", [hd, NH, 128], BF16) for i in range(2)]
    pb = [P.psum(f"pb{i}", [128, 512], F32) for i in range(8)]
    nt = ntok // 128
    pc = [0]
    for t in range(nt):
        row = 0 if t < NLAT // 128 else 1
        x_ = xs[t % 2]
        h_ = hT[t % 2]
        q_ = qkv[t % 2]
        P.dma(x_, x_[:], xin, xin[t * 128:(t + 1) * 128, :])
        if row == 0:
            c_ = cs[t % 2]
            P.dma(c_, c_[:, 0, :], cosd, cosd[t * 128:(t + 1) * 128, :])
            P.dma(c_, c_[:, 1, :], sind, sind[t * 128:(t + 1) * 128, :])
        for half in range(2):
            pt = pb[half]

            def tr(e, pt=pt, half=half):
                for j in range(4):
                    kc = half * 4 + j
                    ins = e.transpose(pt[:, j * 128:(j + 1) * 128], x_[:, kc * 128:(kc + 1) * 128], ident[:])
                return ins
            P.op("pe", [x_, ident], [pt], tr, n_inst=4)
            for j in range(4):
                kc = half * 4 + j
                P.op("act", [pt, mcols], [h_], lambda e, kc=kc, j=j, pt=pt: e.activation(
                    out=h_[:, kc, :], in_=pt[:, j * 128:(j + 1) * 128], func=AF.Identity,
                    scale=mcols[:, row, 1, kc:kc + 1], bias=mcols[:, row, 0, kc:kc + 1]))
        for c0 in range(0, nqkv, 512):
            cn = min(512, nqkv - c0)
            pp = pb[2 + (pc[0] % 2)]
            pc[0] += 1

            def mm(e, pp=pp, c0=c0, cn=cn):
                for kc in range(8):
                    ins = e.matmul(pp[:, 0:cn], lhsT=h_[:, kc, :], rhs=ws[:, kc, c0:c0 + cn],
                                   start=(kc == 0), stop=(kc == 7))
                return ins
            P.op("pe", [h_, ws], [pp], mm, n_inst=8)
            P.op("act", [pp], [q_], lambda e, pp=pp, c0=c0, cn=cn: e.activation(
                out=q_[:, c0:c0 + cn], in_=pp[:, 0:cn], func=AF.Copy))
        v_ = vb[t % 2]
        P.op("dve", [q_], [v_], lambda e: e.tensor_copy(out=v_[:], in_=q_[:, NH * hd:nqkv]))
        P.dma(vo, vo[t * 128:(t + 1) * 128, :], v_, v_[:])
        qk = q_[:, 0:NH * hd]
        if qknorm:
            P.op("dve", [q_], [sq], lambda e: e.tensor_tensor(out=sq[:], in0=qk, in1=qk, op=ALU.mult))
            P.op("dve", [sq], [ms], lambda e: e.tensor_reduce(
                out=ms[:], in_=sq[:].rearrange("p (h d) -> p h d", d=hd), axis=AX.X, op=ALU.add))
            P.op("act", [ms, epst], [rs], lambda e: e.activation(out=rs[:], in_=ms[:], func=AF.Sqrt,
                                                                 bias=epst[:, 0:1], scale=1.0 / hd))
            P.op("dve", [rs], [rs], lambda e: e.reciprocal(out=rs[:], in_=rs[:]))
            q3 = qk.rearrange("p (h d) -> p h d", d=hd)
            P.op("dve", [q_, rs], [q_], lambda e: e.tensor_tensor(
                out=q3, in0=q3, in1=rs[:].unsqueeze(2).to_broadcast([128, NH, hd]), op=ALU.mult))
            P.op("dve", [q_, qgB], [q_], lambda e: e.tensor_tensor(
                out=q3[:, 0:H, :], in0=q3[:, 0:H, :], in1=qgB[:].unsqueeze(1).to_broadcast([128, H, hd]), op=ALU.mult))
            P.op("dve", [q_, kgB], [q_], lambda e: e.tensor_tensor(
                out=q3[:, H:NH, :], in0=q3[:, H:NH, :], in1=kgB[:].unsqueeze(1).to_broadcast([128, KV, hd]), op=ALU.mult))
        r_ = ro[t % 2]
        if row == 0:
            qd = hd // 4
            x5 = qk.rearrange("p (h a two f) -> p h a two f", a=2, two=2, f=qd)
            o5 = r_[:].rearrange("p (h a two f) -> p h a two f", a=2, two=2, f=qd)
            x1, x2 = x5[:, :, :, 0, :], x5[:, :, :, 1, :]
            cB = c_[:, 0, :].rearrange("p (a f) -> p a f", f=qd).unsqueeze(1).to_broadcast([128, NH, 2, qd])
            sB = c_[:, 1, :].rearrange("p (a f) -> p a f", f=qd).unsqueeze(1).to_broadcast([128, NH, 2, qd])
            t1v = t1[:].rearrange("p (h a f) -> p h a f", a=2, f=qd)
            t2v = t2[:].rearrange("p (h a f) -> p h a f", a=2, f=qd)
            P.op("dve", [q_, c_], [t1], lambda e: e.tensor_tensor(out=t1v, in0=x1, in1=cB, op=ALU.mult))
            P.op("dve", [q_, c_], [t2], lambda e: e.tensor_tensor(out=t2v, in0=x2, in1=sB, op=ALU.mult))
            P.op("dve", [t1, t2], [r_], lambda e: e.tensor_tensor(out=o5[:, :, :, 0, :], in0=t1v, in1=t2v, op=ALU.subtract))
            P.op("dve", [q_, c_], [t1], lambda e: e.tensor_tensor(out=t1v, in0=x2, in1=cB, op=ALU.mult))
            P.op("dve", [q_, c_], [t2], lambda e: e.tensor_tensor(out=t2v, in0=x1, in1=sB, op=ALU.mult))
            P.op("dve", [t1, t2], [r_], lambda e: e.tensor_tensor(out=o5[:, :, :, 1, :], in0=t1v, in1=t2v, op=ALU.add))
            src, SRC = r_[:], r_
        else:
            src, SRC = qk, q_
        qt_ = qTs[t % 2]
        per = 512 // 128 if hd <= 128 else 1
        for h0 in range(0, NH, 4):
            hn = min(4, NH - h0)
            pt = pb[4 + ((h0 // 4) % 4)]

            def trq(e, pt=pt, h0=h0, hn=hn):
                for j in range(hn):
                    ins = e.transpose(pt[0:hd, j * 128:(j + 1) * 128], src[:, (h0 + j) * hd:(h0 + j + 1) * hd], ident[:])
                return ins
            P.op("pe", [SRC, ident], [pt], trq, n_inst=hn)
            P.op("act", [pt], [qt_], lambda e, pt=pt, h0=h0, hn=hn: e.activation(
                out=qt_[:, h0:h0 + hn, :], in_=pt[0:hd, 0:hn * 128].rearrange("p (h n) -> p h n", n=128), func=AF.Copy))
        P.dma(qTo, qTo[:, :, t * 128:(t + 1) * 128], qt_, qt_[:, 0:H, :])
        P.dma(kTo, kTo[:, :, t * 128:(t + 1) * 128], qt_, qt_[:, H:NH, :])
    P.finish()
    return P


def build_attn(hd, H, KV, NK, NQtot, qblocks, use_sink, nmask, maskw, hg=1):
    P = Prog()
    R = H // KV
    assert R % hg == 0
    scale = hd ** -0.5
    nkb = NK // 128
    qTd = P.dram("qT", [hd, H, NQtot], BF16, kind="ExternalInput")
    kTd = P.dram("kT", [hd, KV, NK], BF16, kind="ExternalInput")
    vd = P.dram("v", [NK, KV * hd], BF16, kind="ExternalInput")
    oTd = P.dram("oT", [H * hd, NQtot], BF16, kind="ExternalOutput")
    onesd = P.dram("ones_in", [128, 128], BF16, kind="ExternalInput")
    if nmask:
        maskd = P.dram("masks", [128, nmask, maskw], BF16, kind="ExternalInput")
        msk = P.sbuf("msk", [128, nmask, maskw], BF16)
        P.dma(msk, msk[:], maskd, maskd[:])
    if use_sink:
        sinkd = P.dram("sink", [1, H], F32, kind="ExternalInput")
        sinkB = P.sbuf("sinkB", [128, H], F32)
        P.dma(sinkB, sinkB[:], sinkd, sinkd[0:1, :].partition_broadcast(128))
        esink = P.sbuf("esink", [128, H], F32)
    ones = P.sbuf("ones", [128, 128], BF16)
    P.dma(ones, ones[:], onesd, onesd[:])
    onesf = P.sbuf("onesf", [128, 128], F32)
    P.op("dve", [], [onesf], lambda e: e.memset(onesf[:], 1.0))
    kT = P.sbuf("kT_s", [hd, KV, NK], BF16)
    for g in range(KV):
        P.dma(kT, kT[:, g, :], kTd, kTd[:, g, :])
    vs = P.sbuf("v_s", [128, nkb, KV * hd], BF16)
    P.dma(vs, vs[:], vd, vd[:].rearrange("(b p) n -> p b n", p=128))
    NQ = max(q[1] for q in qblocks)
    NC_ = hg * NQ
    assert NC_ <= 512
    qTs = [P.sbuf(f"qTs{i}", [hd, H, NQ], BF16) for i in range(2)]
    sqb = P.sbuf("sqb", [hd, 512], BF16)
    pTs = [P.sbuf(f"pT{i}", [128, NC_], BF16) for i in range(4)]
    accD = [P.sbuf(f"accD{i}", [128, NC_], F32) for i in range(2)]
    rden = [P.sbuf(f"rden{i}", [hd, NC_], F32) for i in range(2)]
    osb = [P.sbuf(f"osb{i}", [hd, NC_], BF16) for i in range(2)]
    kmax = P.sbuf("kmax", [1, 2], F32)
    qmax = P.sbuf("qmax", [1, 2], F32)
    cur = P.sbuf("curmx", [1, 1], F32)
    nshift = P.sbuf("nshift", [128, 1], F32)
    pS = [P.psum(f"pS{i}", [128, 512], F32) for i in range(4)]
    pO = [P.psum(f"pO{i}", [128, 512], F32) for i in range(2)]
    pD = [P.psum(f"pD{i}", [128, 512], F32) for i in range(2)]
    pX = pD[0]

    def max_sq_norm(SRC, chunks, dst):
        first = True
        for ap in chunks:
            cn = ap.shape[-1] if len(ap.shape) == 2 else None
            cn = int(np.prod(ap.shape[1:]))
            P.op("dve", [SRC], [sqb], lambda e, ap=ap, cn=cn: e.tensor_tensor(out=sqb[:, 0:cn], in0=ap, in1=ap, op=ALU.mult))
            P.op("pe", [sqb, ones], [pX], lambda e, cn=cn: e.matmul(pX[0:1, 0:cn], lhsT=ones[0:hd, 0:1], rhs=sqb[:, 0:cn],
                                                                    start=True, stop=True))
            if first:
                P.op("dve", [pX], [dst], lambda e, cn=cn: e.tensor_reduce(out=dst[:, 0:1], in_=pX[0:1, 0:cn], axis=AX.X, op=ALU.max))
                first = False
            else:
                P.op("dve", [pX], [cur], lambda e, cn=cn: e.tensor_reduce(out=cur[:, 0:1], in_=pX[0:1, 0:cn], axis=AX.X, op=ALU.max))
                P.op("dve", [cur, dst], [dst], lambda e: e.tensor_tensor(out=dst[:, 0:1], in0=dst[:, 0:1], in1=cur[:, 0:1], op=ALU.max))

    kflat = kT[:].rearrange("p g n -> p (g n)")
    max_sq_norm(kT, [kflat[:, c0:min(c0 + 512, KV * NK)] for c0 in range(0, KV * NK, 512)], kmax)

    cnt = {"s": 0, "p": 0, "o": 0}
    for bi, (q0, nq, klist) in enumerate(qblocks):
        qt = qTs[bi % 2]
        P.dma(qt, qt[:, :, 0:nq], qTd, qTd[:, :, q0:q0 + nq])
        ncol = hg * nq
        if nq == NQ:
            qflat = qt[:].rearrange("p h n -> p (h n)")
            chunks = [qflat[:, c0:min(c0 + 512, H * NQ)] for c0 in range(0, H * NQ, 512)]
        else:
            chunks = [qt[:, h, 0:nq] for h in range(H)]
        max_sq_norm(qt, chunks, qmax)
        P.op("dve", [qmax, kmax], [qmax], lambda e: e.tensor_tensor(out=qmax[:, 1:2], in0=qmax[:, 0:1], in1=kmax[:, 0:1], op=ALU.mult))
        P.op("act", [qmax], [qmax], lambda e: e.activation(out=qmax[:, 1:2], in_=qmax[:, 1:2], func=AF.Sqrt))
        P.op("dve", [qmax], [qmax], lambda e: e.tensor_scalar(out=qmax[:, 1:2], in0=qmax[:, 1:2], scalar1=-scale, scalar2=None, op0=ALU.mult))
        P.op("pe", [onesf, qmax], [pX], lambda e: e.matmul(pX[:, 0:1], lhsT=onesf[0:1, :], rhs=qmax[0:1, 1:2], start=True, stop=True))
        P.op("dve", [pX], [nshift], lambda e: e.tensor_copy(out=nshift[:], in_=pX[:, 0:1]))
        if use_sink:
            P.op("act", [sinkB, nshift], [esink], lambda e: e.activation(out=esink[:], in_=sinkB[:], func=AF.Exp,
                                                                        bias=nshift[:, 0:1], scale=1.0))
        for v in range(H // hg):
            h0 = v * hg
            g = h0 // R
            po = pO[cnt["o"] % 2]
            pd = pD[cnt["o"] % 2]
            rd = rden[cnt["o"] % 2]
            ob = osb[cnt["o"] % 2]
            acc = accD[cnt["o"] % 2]
            cnt["o"] += 1
            nk = len(klist)
            ps_of = {}
            if hg == 1:
                qrhs = qt[:, h0, 0:nq]
            else:
                qrhs = qt[:, h0:h0 + hg, :].rearrange("p h n -> p (h n)")

            def S(i):
                kb, _ = klist[i]
                ps = pS[cnt["s"] % 4]
                cnt["s"] += 1
                ps_of[i] = ps
                P.op("pe", [kT, qt], [ps], lambda e: e.matmul(ps[:, 0:ncol], lhsT=kT[:, g, kb * 128:(kb + 1) * 128],
                                                              rhs=qrhs, start=True, stop=True))

            def PV(i):
                kb, mid = klist[i]
                ps = ps_of.pop(i)
                pt = pTs[cnt["p"] % 4]
                cnt["p"] += 1
                P.op("act", [ps, nshift], [pt], lambda e: e.activation(out=pt[:, 0:ncol], in_=ps[:, 0:ncol], func=AF.Exp,
                                                                       bias=nshift[:, 0:1], scale=scale))
                if mid is not None:
                    P.op("dve", [pt, msk], [pt], lambda e: e.tensor_tensor(out=pt[:, 0:ncol], in0=pt[:, 0:ncol],
                                                                           in1=msk[:, mid, 0:ncol], op=ALU.mult))
                P.op("pe", [vs, pt], [po], lambda e: e.matmul(po[0:hd, 0:ncol], lhsT=vs[:, kb, g * hd:(g + 1) * hd],
                                                              rhs=pt[:, 0:ncol], start=(i == 0), stop=(i == nk - 1)))
                if i % 2 == 0:
                    P.op("pe", [ones, pt], [pd], lambda e: e.matmul(pd[0:hd, 0:ncol], lhsT=ones[:, 0:hd], rhs=pt[:, 0:ncol],
                                                                    start=(i == 0), stop=(nk == 1)))
                elif i == 1:
                    P.op("dve", [pt], [acc], lambda e: e.tensor_copy(out=acc[:, 0:ncol], in_=pt[:, 0:ncol]))
                else:
                    P.op("dve", [pt, acc], [acc], lambda e: e.tensor_tensor(out=acc[:, 0:ncol], in0=acc[:, 0:ncol],
                                                                            in1=pt[:, 0:ncol], op=ALU.add))
            for i in range(min(2, nk)):
                S(i)
            for i in range(nk):
                if i + 2 < nk:
                    S(i + 2)
                PV(i)
            if nk > 1:
                P.op("pe", [onesf, acc], [pd], lambda e: e.matmul(pd[0:hd, 0:ncol], lhsT=onesf[:, 0:hd], rhs=acc[:, 0:ncol],
                                                                  start=False, stop=True))
            if use_sink:
                for j in range(hg):
                    P.op("dve", [pd, esink], [rd], lambda e, j=j: e.tensor_scalar(
                        out=rd[:, j * nq:(j + 1) * nq], in0=pd[0:hd, j * nq:(j + 1) * nq],
                        scalar1=esink[0:hd, h0 + j:h0 + j + 1], scalar2=None, op0=ALU.add))
                P.op("dve", [rd], [rd], lambda e: e.reciprocal(out=rd[:, 0:ncol], in_=rd[:, 0:ncol]))
            else:
                P.op("dve", [pd], [rd], lambda e: e.reciprocal(out=rd[:, 0:ncol], in_=pd[0:hd, 0:ncol]))
            P.op("dve", [po, rd], [ob], lambda e: e.tensor_tensor(out=ob[:, 0:ncol], in0=po[0:hd, 0:ncol], in1=rd[:, 0:ncol], op=ALU.mult))
            for j in range(hg):
                P.dma(oTd, oTd[(h0 + j) * hd:(h0 + j + 1) * hd, q0:q0 + nq], ob, ob[:, j * nq:(j + 1) * nq])
    P.finish()
    return P


def swa_blocks(ctx_out):
    qb = []
    nb = NLAT // 128
    for i in range(nb):
        left = (i, 2 if i == 0 else 0)
        right = (i + 2, 3 if i == nb - 1 else 1)
        qb.append((i * 128, 128, [left, (i + 1, None), right, (34, None), (35, None)]))
    if ctx_out:
        for j in range(2):
            qb.append((NLAT + j * 128, 128, [(34, None), (35, None)]))
    return qb


def gqa_blocks(ctx_out):
    allk = [(kb, None) for kb in range(130)]
    qb = [(i * 512, 512, allk) for i in range(NLAT // 512)]
    if ctx_out:
        qb.append((NLAT, 256, [(128, None), (129, None)]))
    return qb


GRID_W = 64
ROPE_THETA = 10000.0
_PROGS = {}


def _prog(key, fn):
    if key not in _PROGS:
        _PROGS[key] = fn()
    return _PROGS[key]


def rope_tables(hd):
    quarter = hd // 4
    inv_freq = (ROPE_THETA ** (-np.arange(quarter, dtype=np.float32) / quarter)).astype(np.float32)
    n_rows = SEQ // GRID_W
    rows = np.repeat(np.arange(n_rows, dtype=np.float32), GRID_W)
    cols = np.tile(np.arange(GRID_W, dtype=np.float32), n_rows)
    ang = np.stack([rows[:, None] * inv_freq, cols[:, None] * inv_freq], axis=1)
    return (np.cos(ang).astype(np.float32).reshape(SEQ, hd // 2),
            np.sin(ang).astype(np.float32).reshape(SEQ, hd // 2))


def core_bq(c):
    return c // 4, c % 4


def modcols_for(modv, i, b):
    rows = np.stack([modv[i, b], modv[i, 2]])
    cols = np.ascontiguousarray(rows.reshape(2, 6, 8, 128).transpose(3, 0, 1, 2)).reshape(128, 96)
    return np.ascontiguousarray(rows), cols


IDENT = np.eye(128, dtype=np.float32)
ONES_BF = np.ones((128, 128), dtype=NPBF)


SWA_HG = 4


def tri_masks(c):
    b, q = core_bq(c)
    kl = np.arange(128)[:, None]
    ql = np.arange(128)[None, :]
    L = (kl >= ql).astype(np.float32)
    Rm = (kl <= ql).astype(np.float32)
    Le = L * (0.0 if q == 0 else 1.0)
    Re = Rm * (0.0 if q == 3 else 1.0)
    m = np.stack([L, Rm, Le, Re], axis=1)
    return np.ascontiguousarray(np.tile(m, (1, 1, SWA_HG))).astype(NPBF)


def run_tail(kind, ctx_out, i, aT_list, x, ctx, wp, wb, modv, inputs):
    P = _prog(("tail", kind, ctx_out), lambda: build_tail(kind, ctx_out))
    in_maps = []
    for c in range(NCORES):
        b, q = core_bq(c)
        rows, cols = modcols_for(modv, i, b)
        xin = x[b, q * NLAT:(q + 1) * NLAT]
        if ctx_out:
            xin = np.concatenate([xin, ctx[b]], 0)
        m = {"ident_in": IDENT, "aT": aT_list[c], "wp0": wp[0], "xin": np.ascontiguousarray(xin),
             "modrows": rows, "modcols": cols, "lng": inputs["ln_g"][i], "lnb": inputs["ln_b"][i],
             "rw": wb["router_w"], "rb": inputs["router_b"][None, :].astype(np.float32),
             "w1": wb["moe_w1"][i], "w3": wb["moe_w3"][i], "w2": wb["moe_w2"][i]}
        if kind == "s5":
            m["wp1"] = wp[1]
        in_maps.append(m)
    res = run_prog(P, in_maps)
    xn = np.empty_like(x)
    cn = np.array(ctx, copy=True)
    for c in range(NCORES):
        b, q = core_bq(c)
        o = np.asarray(res[c]["xout"])
        xn[b, q * NLAT:(q + 1) * NLAT] = o[:NLAT]
        if ctx_out and q == 0:
            cn[b] = o[NLAT:]
    return xn, cn


def run_attn_layer(i, which, ctx_out, x, ctx, wb, modv, inputs):
    if which == "swa":
        hd, H, KV = 64, 16, 2
        wqkv, wo = wb["swa_w_qkv"][0], wb["swa_w_o"][0]
    else:
        hd, H, KV = 128, 8, 2
        wqkv, wo = wb["gqa_w_qkv"][0], wb["gqa_w_o"][0]
    cos, sin = rope_tables(hd)
    Pq = _prog(("qkv", which), lambda: build_qkv(hd, H, KV, which == "gqa", NCTX))
    in_maps = []
    for c in range(NCORES):
        b, q = core_bq(c)
        _, cols = modcols_for(modv, i, b)
        m = {"ident_in": IDENT, "xin": np.ascontiguousarray(np.concatenate([x[b, q * NLAT:(q + 1) * NLAT], ctx[b]], 0)),
             "modcols": cols, "wqkv": wqkv, "cos": np.ascontiguousarray(cos[q * NLAT:(q + 1) * NLAT]),
             "sin": np.ascontiguousarray(sin[q * NLAT:(q + 1) * NLAT])}
        if which == "gqa":
            m["qg"] = inputs["gqa_q_norm"].astype(np.float32).reshape(1, hd)
            m["kg"] = inputs["gqa_k_norm"].astype(np.float32).reshape(1, hd)
        in_maps.append(m)
    rq = run_prog(Pq, in_maps)
    qT = [np.asarray(r["qT"]) for r in rq]
    kT = [np.asarray(r["kT"]) for r in rq]
    vv = [np.asarray(r["v"]) for r in rq]
    nq_tot = NLAT + (NCTX if ctx_out else 0)
    in_maps = []
    if which == "swa":
        Pa = _prog(("attn", which, ctx_out), lambda: build_attn(hd, H, KV, 4608, nq_tot, swa_blocks(ctx_out), True, 4, 128 * SWA_HG, hg=SWA_HG))
        for c in range(NCORES):
            b, q = core_bq(c)
            zk = np.zeros((hd, KV, 128), NPBF)
            zv = np.zeros((128, KV * hd), NPBF)
            kl = kT[c - 1][:, :, NLAT - 128:NLAT] if q > 0 else zk
            kr = kT[c + 1][:, :, 0:128] if q < 3 else zk
            vl = vv[c - 1][NLAT - 128:NLAT] if q > 0 else zv
            vr = vv[c + 1][0:128] if q < 3 else zv
            kk = np.concatenate([kl, kT[c][:, :, :NLAT], kr, kT[c][:, :, NLAT:]], axis=2)
            vk = np.concatenate([vl, vv[c][:NLAT], vr, vv[c][NLAT:]], axis=0)
            in_maps.append({"qT": np.ascontiguousarray(qT[c][:, :, :nq_tot]), "kT": np.ascontiguousarray(kk),
                            "v": np.ascontiguousarray(vk), "ones_in": ONES_BF, "masks": tri_masks(c),
                            "sink": inputs["swa_sink"].astype(np.float32).reshape(1, H)})
    else:
        Pa = _prog(("attn", which, ctx_out), lambda: build_attn(hd, H, KV, 16640, nq_tot, gqa_blocks(ctx_out), False, 0, 0))
        for c in range(NCORES):
            b, q = core_bq(c)
            cs = [4 * b + j for j in range(4)]
            kk = np.concatenate([kT[j][:, :, :NLAT] for j in cs] + [kT[c][:, :, NLAT:]], axis=2)
            vk = np.concatenate([vv[j][:NLAT] for j in cs] + [vv[c][NLAT:]], axis=0)
            in_maps.append({"qT": np.ascontiguousarray(qT[c][:, :, :nq_tot]), "kT": np.ascontiguousarray(kk),
                            "v": np.ascontiguousarray(vk), "ones_in": ONES_BF})
    ra = run_prog(Pa, in_maps)
    oT = [np.asarray(r["oT"]) for r in ra]
    return run_tail("attn", ctx_out, i, oT, x, ctx, [wo], wb, modv, inputs), (qT, kT, vv, oT)


NCH = 130
PI = float(np.pi)


def build_s5():
    P = Prog()
    ntk = NCH * 128
    xtok = P.dram("xtok", [2, ntk, 128], F32, kind="ExternalInput")
    xT = P.dram("xT", [128, 2 * ntk], F32, kind="ExternalInput")
    mrow = P.dram("mrow", [3, 2, 128], F32, kind="ExternalInput")
    mcol = P.dram("mcol", [128, 6], F32, kind="ExternalInput")
    ared = P.dram("are", [64, 16], F32, kind="ExternalInput")
    aimd = P.dram("aim", [64, 16], F32, kind="ExternalInput")
    ldtd = P.dram("ldt", [1, 16], F32, kind="ExternalInput")
    bred = P.dram("bre", [64, 256], F32, kind="ExternalInput")
    bimd = P.dram("bim", [64, 256], F32, kind="ExternalInput")
    cred = P.dram("cre", [64, 256], F32, kind="ExternalInput")
    cimd = P.dram("cim", [64, 256], F32, kind="ExternalInput")
    dskd = P.dram("dsk", [128, 1], F32, kind="ExternalInput")
    antid = P.dram("anti_in", [128, 128], BF16, kind="ExternalInput")
    gTo = P.dram("gT", [128, 2 * ntk], BF16, kind="ExternalOutput")
    hk = P.dram("hk", [8, 256, 256], BF16, kind="Internal")
    ident = make_ident(P)
    anti = P.sbuf("anti", [128, 128], BF16)
    P.dma(anti, anti[:], antid, antid[:])

    def sb(name, shape, dt=F32):
        return P.sbuf(name, shape, dt)

    def ld(name, src, shape, ap=None):
        t = sb(name, shape)
        P.dma(t, t[:], src, src[:] if ap is None else ap)
        return t

    def tt(out_b, out_ap, a_b, a_ap, b_b, b_ap, op):
        P.op("dve", [a_b, b_b], [out_b], lambda e: e.tensor_tensor(out=out_ap, in0=a_ap, in1=b_ap, op=op))

    are = ld("are_s", ared, [64, 16])
    aim = ld("aim_s", aimd, [64, 16])
    dt = sb("dt_s", [64, 16])
    P.dma(dt, dt[:], ldtd, ldtd[0:1, :].partition_broadcast(64))
    bre = ld("bre_s", bred, [64, 256])
    bim = ld("bim_s", bimd, [64, 256])
    cre = ld("cre_s", cred, [64, 256])
    cim = ld("cim_s", cimd, [64, 256])
    dsk = ld("dsk_s", dskd, [128, 1])
    mc = ld("mc_s", mcol, [128, 6])
    P.op("act", [dt], [dt], lambda e: e.activation(out=dt[:], in_=dt[:], func=AF.Exp))
    adr = sb("adr", [64, 16])
    adi = sb("adi", [64, 16])
    tt(adr, adr[:], are, are[:], dt, dt[:], ALU.mult)
    tt(adi, adi[:], aim, aim[:], dt, dt[:], ALU.mult)
    mag = sb("mag", [64, 16])
    P.op("act", [adr], [mag], lambda e: e.activation(out=mag[:], in_=adr[:], func=AF.Exp))
    rr = sb("rr", [64, 32])
    rm = sb("rm", [64, 32])
    P.op("dve", [adi], [rr], lambda e: e.tensor_copy(out=rr[:, 0:16], in_=adi[:]))
    P.op("dve", [adi], [rr], lambda e: e.tensor_scalar(out=rr[:, 16:32], in0=adi[:], scalar1=PI / 2, scalar2=None, op0=ALU.add))
    for _ in range(5):
        P.op("dve", [rr], [rm], lambda e: e.tensor_scalar(out=rm[:], in0=rr[:], scalar1=PI, scalar2=2 * PI,
                                                          op0=ALU.is_gt, op1=ALU.mult))
        tt(rr, rr[:], rr, rr[:], rm, rm[:], ALU.subtract)
    sc = sb("sincos", [64, 32])
    P.op("act", [rr], [sc], lambda e: e.activation(out=sc[:], in_=rr[:], func=AF.Sin))
    lre = sb("lre", [64, 16])
    lim = sb("lim", [64, 16])
    tt(lre, lre[:], mag, mag[:], sc, sc[:, 16:32], ALU.mult)
    tt(lim, lim[:], mag, mag[:], sc, sc[:, 0:16], ALU.mult)
    den = sb("den", [64, 16])
    tmp = sb("tmp16", [64, 16])
    tmp2 = sb("tmp16b", [64, 16])
    tt(den, den[:], are, are[:], are, are[:], ALU.mult)
    tt(tmp, tmp[:], aim, aim[:], aim, aim[:], ALU.mult)
    tt(den, den[:], den, den[:], tmp, tmp[:], ALU.add)
    P.op("dve", [den], [den], lambda e: e.reciprocal(out=den[:], in_=den[:]))
    nre = sb("nre", [64, 16])
    P.op("dve", [lre], [nre], lambda e: e.tensor_scalar(out=nre[:], in0=lre[:], scalar1=-1.0, scalar2=None, op0=ALU.add))
    fre = sb("fre", [64, 16])
    fim = sb("fim", [64, 16])
    tt(fre, fre[:], nre, nre[:], are, are[:], ALU.mult)
    tt(tmp, tmp[:], lim, lim[:], aim, aim[:], ALU.mult)
    tt(fre, fre[:], fre, fre[:], tmp, tmp[:], ALU.add)
    tt(fre, fre[:], fre, fre[:], den, den[:], ALU.mult)
    tt(fim, fim[:], lim, lim[:], are, are[:], ALU.mult)
    tt(tmp, tmp[:], nre, nre[:], aim, aim[:], ALU.mult)
    tt(fim, fim[:], fim, fim[:], tmp, tmp[:], ALU.subtract)
    tt(fim, fim[:], fim, fim[:], den, den[:], ALU.mult)

    def v3(t):
        return t[:].rearrange("p (a c) -> p a c", c=16)

    def bc3(t):
        return t[:].unsqueeze(2).to_broadcast([64, 16, 16])
    bbre = sb("bbre", [64, 256])
    bbim = sb("bbim", [64, 256])
    t256 = sb("t256", [64, 256])
    tt(bbre, v3(bbre), bre, v3(bre), fre, bc3(fre), ALU.mult)
    tt(t256, v3(t256), bim, v3(bim), fim, bc3(fim), ALU.mult)
    tt(bbre, bbre[:], bbre, bbre[:], t256, t256[:], ALU.subtract)
    tt(bbim, v3(bbim), bim, v3(bim), fre, bc3(fre), ALU.mult)
    tt(t256, v3(t256), bre, v3(bre), fim, bc3(fim), ALU.mult)
    tt(bbim, bbim[:], bbim, bbim[:], t256, t256[:], ALU.add)
    clre = sb("clre", [64, 256])
    clim = sb("clim", [64, 256])
    tt(clre, v3(clre), cre, v3(cre), lre, bc3(lre), ALU.mult)
    tt(t256, v3(t256), cim, v3(cim), lim, bc3(lim), ALU.mult)
    tt(clre, clre[:], clre, clre[:], t256, t256[:], ALU.subtract)
    tt(clim, v3(clim), cre, v3(cre), lim, bc3(lim), ALU.mult)
    tt(t256, v3(t256), cim, v3(cim), lre, bc3(lre), ALU.mult)
    tt(clim, clim[:], clim, clim[:], t256, t256[:], ALU.add)
    pw = sb("pw", [64, 8, 2, 16])
    P.op("dve", [lre], [pw], lambda e: e.tensor_copy(out=pw[:, 0, 0, :], in_=lre[:]))
    P.op("dve", [lim], [pw], lambda e: e.tensor_copy(out=pw[:, 0, 1, :], in_=lim[:]))
    for i in range(7):
        tt(tmp, tmp[:], pw, pw[:, i, 0, :], pw, pw[:, i, 0, :], ALU.mult)
        tt(tmp2, tmp2[:], pw, pw[:, i, 1, :], pw, pw[:, i, 1, :], ALU.mult)
        tt(pw, pw[:, i + 1, 0, :], tmp, tmp[:], tmp2, tmp2[:], ALU.subtract)
        tt(tmp, tmp[:], pw, pw[:, i, 0, :], pw, pw[:, i, 1, :], ALU.mult)
        P.op("dve", [tmp], [pw], lambda e, i=i: e.tensor_scalar(out=pw[:, i + 1, 1, :], in0=tmp[:], scalar1=2.0,
                                                                scalar2=None, op0=ALU.mult))
    Rm = sb("Rm", [64, 16])
    upw = sb("upw", [64, 8, 2, 16])
    tt(tmp, tmp[:], pw, pw[:, 7, 0, :], pw, pw[:, 7, 0, :], ALU.mult)
    tt(tmp2, tmp2[:], pw, pw[:, 7, 1, :], pw, pw[:, 7, 1, :], ALU.mult)
    tt(tmp, tmp[:], tmp, tmp[:], tmp2, tmp2[:], ALU.add)
    P.op("act", [tmp], [Rm], lambda e: e.activation(out=Rm[:], in_=tmp[:], func=AF.Sqrt))
    P.op("dve", [Rm], [tmp], lambda e: e.reciprocal(out=tmp[:], in_=Rm[:]))
    tt(upw, upw[:, 0, 0, :], pw, pw[:, 7, 0, :], tmp, tmp[:], ALU.mult)
    tt(upw, upw[:, 0, 1, :], pw, pw[:, 7, 1, :], tmp, tmp[:], ALU.mult)
    for i in range(7):
        tt(tmp, tmp[:], upw, upw[:, i, 0, :], upw, upw[:, i, 0, :], ALU.mult)
        tt(tmp2, tmp2[:], upw, upw[:, i, 1, :], upw, upw[:, i, 1, :], ALU.mult)
        tt(upw, upw[:, i + 1, 0, :], tmp, tmp[:], tmp2, tmp2[:], ALU.subtract)
        tt(tmp, tmp[:], upw, upw[:, i, 0, :], upw, upw[:, i, 1, :], ALU.mult)
        P.op("dve", [tmp], [upw], lambda e, i=i: e.tensor_scalar(out=upw[:, i + 1, 1, :], in0=tmp[:], scalar1=2.0,
                                                                 scalar2=None, op0=ALU.mult))
    tre = sb("tre", [64, 16, 128])
    tim = sb("tim", [64, 16, 128])
    ta = sb("ta", [64, 8, 64])
    tb = sb("tb", [64, 8, 64])
    P.op("dve", [], [tre], lambda e: e.memset(tre[:], 1.0))
    P.op("dve", [], [tim], lambda e: e.memset(tim[:], 0.0))
    for i in range(7):
        m = 1 << i
        for d in range(2):
            gs = slice(d * 8, d * 8 + 8)
            if d == 0:
                src, dst = slice(128 - m, 128), slice(128 - 2 * m, 128 - m)
            else:
                src, dst = slice(0, m), slice(m, 2 * m)
            pr = pw[:, i, 0, gs].unsqueeze(2).to_broadcast([64, 8, m])
            pi_ = pw[:, i, 1, gs].unsqueeze(2).to_broadcast([64, 8, m])
            tt(ta, ta[:, :, 0:m], tre, tre[:, gs, src], pw, pr, ALU.mult)
            tt(tb, tb[:, :, 0:m], tim, tim[:, gs, src], pw, pi_, ALU.mult)
            tt(tre, tre[:, gs, dst], ta, ta[:, :, 0:m], tb, tb[:, :, 0:m], ALU.subtract)
            tt(ta, ta[:, :, 0:m], tre, tre[:, gs, src], pw, pi_, ALU.mult)
            tt(tb, tb[:, :, 0:m], tim, tim[:, gs, src], pw, pr, ALU.mult)
            tt(tim, tim[:, gs, dst], ta, ta[:, :, 0:m], tb, tb[:, :, 0:m], ALU.add)
    pb = [P.psum(f"pb{i}", [128, 512], F32) for i in range(8)]
    cbre = sb("cbre", [64, 256])
    cbim = sb("cbim", [64, 256])
    cbt1 = sb("cbt1", [64, 256])
    cbt2 = sb("cbt2", [64, 256])
    hks = [sb(f"hks{i}", [128, 256], BF16) for i in range(2)]
    hcnt = [0]
    for g in range(8):
        pss = {}
        for d in range(2):
            dg = d * 8 + g
            cB = lambda t: t[:, dg * 16:(dg + 1) * 16].unsqueeze(2).to_broadcast([64, 16, 16])
            bB = lambda t: t[:, dg * 16:(dg + 1) * 16].unsqueeze(1).to_broadcast([64, 16, 16])
            o1 = cbt1[:].rearrange("p (c k) -> p c k", k=16)
            o2 = cbt2[:].rearrange("p (c k) -> p c k", k=16)
            tt(cbt1, o1, cre, cB(cre), bbre, bB(bbre), ALU.mult)
            tt(cbt2, o2, cim, cB(cim), bbim, bB(bbim), ALU.mult)
            tt(cbre, cbre[:], cbt1, cbt1[:], cbt2, cbt2[:], ALU.subtract)
            tt(cbt1, o1, cre, cB(cre), bbim, bB(bbim), ALU.mult)
            tt(cbt2, o2, cim, cB(cim), bbre, bB(bbre), ALU.mult)
            P.op("dve", [cbt1, cbt2], [cbim], lambda e: e.scalar_tensor_tensor(
                out=cbim[:], in0=cbt1[:], scalar=-1.0, in1=cbt2[:], op0=ALU.mult, op1=ALU.subtract))
            for half in range(2):
                ps = pb[d * 2 + half]

                def mm(e, ps=ps, half=half, dg=dg):
                    e.matmul(ps[:, 0:128], lhsT=cbre[:, half * 128:(half + 1) * 128], rhs=tre[:, dg, :], start=True, stop=False)
                    return e.matmul(ps[:, 0:128], lhsT=cbim[:, half * 128:(half + 1) * 128], rhs=tim[:, dg, :], start=False, stop=True)
                P.op("pe", [cbre, cbim, tre, tim], [ps], mm, n_inst=2)
                pss[(d, half)] = ps
        for half in range(2):
            hs = hks[hcnt[0] % 2]
            hcnt[0] += 1
            pa, pbb = pss[(0, half)], pss[(1, half)]
            P.op("act", [pa], [hs], lambda e: e.activation(out=hs[:, 0:127], in_=pa[:, 0:127], func=AF.Copy))
            P.op("act", [pbb], [hs], lambda e: e.activation(out=hs[:, 128:255], in_=pbb[:, 1:128], func=AF.Copy))
            P.op("act", [pa], [cbt1], lambda e: e.activation(out=cbt1[0:64, 0:1], in_=pa[0:64, 127:128], func=AF.Copy))
            t128 = sb(f"t128_{g}_{half}", [128, 2])
            P.op("act", [pa], [t128], lambda e: e.activation(out=t128[:, 0:1], in_=pa[:, 127:128], func=AF.Copy))
            P.op("dve", [pbb, t128], [hs], lambda e: e.tensor_tensor(out=hs[:, 127:128], in0=pbb[:, 0:1], in1=t128[:, 0:1], op=ALU.add))
            P.op("dve", [], [hs], lambda e: e.memset(hs[:, 255:256], 0.0))
            P.dma(hk, hk[g, half * 128:(half + 1) * 128, :], hs, hs[:])
    U = sb("U", [128, NCH, 128], BF16)
    Ysb = sb("Ysb", [128, NCH, 128], BF16)
    xl = [sb(f"xl{i}", [128, 4, 128]) for i in range(2)]
    scB = sb("scB", [128, 2, 128])
    Sst = sb("Sst", [64, 16, 2, NCH])
    Hin = sb("Hin", [64, 16, 2, NCH], BF16)
    Wre = sb("Wre", [64, 8, NCH])
    Wim = sb("Wim", [64, 8, NCH])
    UPre = sb("UPre", [64, 8, 136])
    UPim = sb("UPim", [64, 8, 136])
    scr = [sb(f"scr{i}", [64, 1040]) for i in range(3)]
    dg3v = [t[:, 0:1024].rearrange("p (c q) -> p c q", q=64) for t in scr]
    WinS = [sb(f"WinS{i}", [128, 16, 2, 64], BF16) for i in range(2)]
    Wo = sb("Wo", [64, 2, 2, 16, 128], BF16)
    HK = [sb(f"HK{i}", [128, 16, 128], BF16) for i in range(2)]
    xTb = [sb(f"xTb{i}", [128, 512]) for i in range(2)]
    zv = [sb(f"zv{i}", [128, 512]) for i in range(2)]
    gv = [sb(f"gv{i}", [128, 512], BF16) for i in range(2)]
    AB = sb("AB", [128, 3, 2])
    for r in range(3):
        P.op("dve", [mc, dsk], [AB], lambda e, r=r: e.tensor_scalar(out=AB[:, r, 0:1], in0=mc[:, 2 * r + 1:2 * r + 2], scalar1=1.0,
                                                                    scalar2=dsk[:, 0:1], op0=ALU.add, op1=ALU.mult))
        P.op("dve", [mc, dsk], [AB], lambda e, r=r: e.tensor_scalar(out=AB[:, r, 1:2], in0=mc[:, 2 * r:2 * r + 1], scalar1=dsk[:, 0:1],
                                                                    scalar2=None, op0=ALU.mult))
    ident64 = ident[0:64, 0:64]
    pcnt = [0]

    def nextpb():
        pcnt[0] += 1
        return pb[pcnt[0] % 8]

    for b in range(2):
        def load_rowmod(r):
            P.dma(scB, scB[:, 0, :], mrow, mrow[r:r + 1, 0, :].partition_broadcast(128))
            P.dma(scB, scB[:, 1, :], mrow, mrow[r:r + 1, 1, :].partition_broadcast(128))
            P.op("dve", [scB], [scB], lambda e: e.tensor_scalar(out=scB[:, 1, :], in0=scB[:, 1, :], scalar1=1.0, scalar2=None, op0=ALU.add))
        li = [0]

        def do_chunks(k0, kn):
            x_ = xl[li[0] % 2]
            li[0] += 1
            P.dma(x_, x_[:, 0:kn, :], xtok, xtok[b, k0 * 128:(k0 + kn) * 128, :].rearrange("(k p) c -> p k c", p=128))
            tt(x_, x_[:, 0:kn, :], x_, x_[:, 0:kn, :], scB, scB[:, 1, :].unsqueeze(1).to_broadcast([128, kn, 128]), ALU.mult)
            tt(U, U[:, k0:k0 + kn, :], x_, x_[:, 0:kn, :], scB, scB[:, 0, :].unsqueeze(1).to_broadcast([128, kn, 128]), ALU.add)
        load_rowmod(2)
        do_chunks(0, 2)
        load_rowmod(b)
        for k0 in range(2, NCH, 4):
            do_chunks(k0, 4)
        for dg in range(16):
            g = dg % 8
            W = WinS[dg % 2]
            idb = ident64.unsqueeze(1).to_broadcast([64, 16, 64])
            bsl = lambda t: t[:, dg * 16:(dg + 1) * 16].unsqueeze(2).to_broadcast([64, 16, 64])
            tt(scr[0], dg3v[0], ident, idb, bbre, bsl(bbre), ALU.mult)
            tt(scr[1], dg3v[1], ident, idb, bbim, bsl(bbim), ALU.mult)
            P.op("dve", [scr[1]], [scr[2]], lambda e: e.tensor_scalar(out=scr[2][:, 0:1024], in0=scr[1][:, 0:1024], scalar1=-1.0, scalar2=None, op0=ALU.mult))
            for hc in range(2):
                for ri in range(2):
                    ps = nextpb()
                    r1 = scr[0] if ri == 0 else scr[1]
                    r2 = scr[2] if ri == 0 else scr[0]

                    def mm(e, ps=ps, r1=r1, r2=r2, hc=hc, dg=dg):
                        e.matmul(ps[:, 0:512], lhsT=tre[:, dg, :], rhs=r1[:, hc * 512:(hc + 1) * 512],
                                 start=True, stop=False)
                        return e.matmul(ps[:, 0:512], lhsT=tim[:, dg, :], rhs=r2[:, hc * 512:(hc + 1) * 512],
                                        start=False, stop=True)
                    P.op("pe", [tre, tim, r1, r2], [ps], mm, n_inst=2)
                    P.op("act", [ps], [W], lambda e, ps=ps, hc=hc, ri=ri, W=W: e.activation(
                        out=W[:, hc * 8:(hc + 1) * 8, ri, :], in_=ps[:, 0:512].rearrange("p (c q) -> p c q", q=64), func=AF.Copy))
            for ri in range(2):
                ps = nextpb()

                def mm2(e, ps=ps, ri=ri, W=W, g=g):
                    for c_ in range(16):
                        ins = e.matmul(ps[0:64, 0:NCH], lhsT=W[:, c_, ri, :], rhs=U[:, :, g * 16 + c_],
                                       start=(c_ == 0), stop=(c_ == 15))
                    return ins
                P.op("pe", [W, U], [ps], mm2, n_inst=16)
                P.op("act", [ps], [Sst], lambda e, ps=ps, ri=ri, dg=dg: e.activation(out=Sst[:, dg, ri, :], in_=ps[0:64, 0:NCH], func=AF.Copy))
        def rev(ap3, lo, hi):
            v = ap3[:, :, lo:hi]
            return bass.AP(v.tensor, v.offset + (hi - lo - 1), [list(v.ap[0]), list(v.ap[1]), [-1, hi - lo]])

        def crot(o_re_b, o_re, o_im_b, o_im, c_ap, s_ap, x_b, xr, xi, n, conj, Wt):
            t1 = scr[2][:, 0:8 * n].rearrange("p (g n) -> p g n", n=n)
            t2 = Wt[:, 0:8 * n].rearrange("p (g n) -> p g n", n=n) if len(Wt[:].shape) == 2 else Wt[:, :, 0:n]
            x2_b = Zim_t if x_b is Zre_t else x_b
            tt(scr[2], t1, UPre, c_ap, x_b, xr, ALU.mult)
            tt(Wt, t2, UPim, s_ap, x2_b, xi, ALU.mult)
            tt(o_re_b, o_re, scr[2], t1, Wt, t2, ALU.add if conj else ALU.subtract)
            tt(scr[2], t1, UPre, c_ap, x2_b, xi, ALU.mult)
            tt(Wt, t2, UPim, s_ap, x_b, xr, ALU.mult)
            tt(o_im_b, o_im, scr[2], t1, Wt, t2, ALU.subtract if conj else ALU.add)

        Zre_t, Zim_t = scr[0], scr[1]
        Zre = Zre_t[:, 0:8 * NCH].rearrange("p (g n) -> p g n", n=NCH)
        Zim = Zim_t[:, 0:8 * NCH].rearrange("p (g n) -> p g n", n=NCH)
        for d in range(2):
            gs = slice(d * 8, d * 8 + 8)
            P.op("dve", [], [UPre], lambda e: e.memset(UPre[:], 1.0))
            P.op("dve", [], [UPim], lambda e: e.memset(UPim[:], 0.0))
            for i in range(8):
                m = 1 << i
                ln = min(m, 136 - m)
                pr = upw[:, i, 0, gs].unsqueeze(2).to_broadcast([64, 8, ln])
                pi_ = upw[:, i, 1, gs].unsqueeze(2).to_broadcast([64, 8, ln])
                a1 = scr[0][:, 0:8 * ln].rearrange("p (g n) -> p g n", n=ln)
                a2 = scr[1][:, 0:8 * ln].rearrange("p (g n) -> p g n", n=ln)
                tt(scr[0], a1, UPre, UPre[:, :, 0:ln], upw, pr, ALU.mult)
                tt(scr[1], a2, UPim, UPim[:, :, 0:ln], upw, pi_, ALU.mult)
                tt(scr[2], scr[2][:, 0:8 * ln].rearrange("p (g n) -> p g n", n=ln), scr[0], a1, scr[1], a2, ALU.subtract)
                tt(scr[0], a1, UPre, UPre[:, :, 0:ln], upw, pi_, ALU.mult)
                tt(scr[1], a2, UPim, UPim[:, :, 0:ln], upw, pr, ALU.mult)
                tt(UPim, UPim[:, :, m:m + ln], scr[0], a1, scr[1], a2, ALU.add)
                P.op("dve", [scr[2]], [UPre], lambda e, m=m, ln=ln: e.tensor_copy(
                    out=UPre[:, :, m:m + ln], in_=scr[2][:, 0:8 * ln].rearrange("p (g n) -> p g n", n=ln)))
            Sre3, Sim3 = Sst[:, gs, 0, :], Sst[:, gs, 1, :]
            if d == 0:
                pieces = [(0, NCH, Sre3[:, :, 0:NCH], Sim3[:, :, 0:NCH])]
            else:
                pieces = [(0, 2, rev(Sre3, 0, 2), rev(Sim3, 0, 2)), (2, NCH, rev(Sre3, 2, NCH), rev(Sim3, 2, NCH))]
            for (j0, j1, sr, si) in pieces:
                n = j1 - j0
                crot(Wre, Wre[:, :, j0:j1], Wim, Wim[:, :, j0:j1], UPre[:, :, j0 + 1:j1 + 1], UPim[:, :, j0 + 1:j1 + 1],
                     Sst, sr, si, n, True, scr[0])
            for g in range(8):
                dg = d * 8 + g
                P.op("dve", [Rm, Wre], [Zre_t], lambda e, g=g, dg=dg: e.tensor_tensor_scan(
                    out=Zre[:, g, :], data0=Rm[:, dg:dg + 1].to_broadcast([64, NCH]), data1=Wre[:, g, :],
                    initial=0.0, op0=ALU.mult, op1=ALU.add))
                P.op("dve", [Rm, Wim], [Zim_t], lambda e, g=g, dg=dg: e.tensor_tensor_scan(
                    out=Zim[:, g, :], data0=Rm[:, dg:dg + 1].to_broadcast([64, NCH]), data1=Wim[:, g, :],
                    initial=0.0, op0=ALU.mult, op1=ALU.add))
            Hre3, Him3 = Hin[:, gs, 0, :], Hin[:, gs, 1, :]
            if d == 0:
                P.op("dve", [], [Hin], lambda e: e.memset(Hin[:, gs, :, 0:1], 0.0))
                crot(Hin, Hre3[:, :, 1:NCH], Hin, Him3[:, :, 1:NCH], UPre[:, :, 1:NCH], UPim[:, :, 1:NCH],
                     Zre_t, Zre[:, :, 0:NCH - 1], Zim[:, :, 0:NCH - 1], NCH - 1, False, Wre)
            else:
                P.op("dve", [], [Hin], lambda e: e.memset(Hin[:, gs, :, 1:2], 0.0))
                crot(Hin, Hre3[:, :, 0:1], Hin, Him3[:, :, 0:1], UPre[:, :, 1:2], UPim[:, :, 1:2],
                     Zre_t, Zre[:, :, 0:1], Zim[:, :, 0:1], 1, False, Wre)
                crot(Hin, Hre3[:, :, 2:NCH], Hin, Him3[:, :, 2:NCH], rev(UPre, 2, NCH), rev(UPim, 2, NCH),
                     Zre_t, rev(Zre, 1, NCH - 1), rev(Zim, 1, NCH - 1), NCH - 2, False, Wre)
        hkc = [0]
        for g in range(8):
            for d in range(2):
                dg = d * 8 + g
                w1v = scr[0][:, 0:1024].rearrange("p (c t) -> p c t", t=128)
                w2v = scr[1][:, 0:1024].rearrange("p (c t) -> p c t", t=128)
                for hw in range(2):
                    cB = lambda t: t[:, dg * 16 + hw * 8:dg * 16 + hw * 8 + 8].unsqueeze(2).to_broadcast([64, 8, 128])
                    tB = lambda t: t[:, dg, :].unsqueeze(1).to_broadcast([64, 8, 128])
                    cs_ = slice(hw * 8, hw * 8 + 8)
                    tt(scr[0], w1v, clre, cB(clre), tre, tB(tre), ALU.mult)
                    tt(scr[1], w2v, clim, cB(clim), tim, tB(tim), ALU.mult)
                    tt(Wo, Wo[:, d, 0, cs_, :], scr[0], w1v, scr[1], w2v, ALU.subtract)
                    tt(scr[0], w1v, clre, cB(clre), tim, tB(tim), ALU.mult)
                    tt(scr[1], w2v, clim, cB(clim), tre, tB(tre), ALU.mult)
                    P.op("dve", [scr[0], scr[1]], [Wo], lambda e, d=d, cs_=cs_: e.scalar_tensor_tensor(
                        out=Wo[:, d, 1, cs_, :], in0=w1v, scalar=-1.0, in1=w2v, op0=ALU.mult, op1=ALU.subtract))
            for c_ in range(16):
                Hk_ = HK[hkc[0] % 2]
                hkc[0] += 1
                base = hk[g, c_ * 16, 0]
                src = bass.AP(hk.h.tensor, (g * 256 + c_ * 16) * 256, [[1, 128], [256, 16], [1, 128]])
                P.dma(Hk_, Hk_[:], hk, src)
                ps = nextpb()

                def mm3(e, ps=ps, Hk_=Hk_, g=g, c_=c_):
                    for k_ in range(16):
                        e.matmul(ps[:, 0:NCH], lhsT=Hk_[:, k_, :], rhs=U[:, :, g * 16 + k_], start=(k_ == 0), stop=False)
                    for d in range(2):
                        dg = d * 8 + g
                        e.matmul(ps[:, 0:NCH], lhsT=Wo[:, d, 0, c_, :], rhs=Hin[:, dg, 0, :], start=False, stop=False)
                        ins = e.matmul(ps[:, 0:NCH], lhsT=Wo[:, d, 1, c_, :], rhs=Hin[:, dg, 1, :], start=False, stop=(d == 1))
                    return ins
                P.op("pe", [Hk_, U, Wo, Hin], [ps], mm3, n_inst=20)
                P.op("act", [ps], [Ysb], lambda e, ps=ps, g=g, c_=c_: e.activation(out=Ysb[:, :, g * 16 + c_], in_=ps[:, 0:NCH], func=AF.Copy))
        blocks = [(0, 2, 2)] + [(k0, 4, b) for k0 in range(2, NCH, 4)]
        for bi, (k0, kn, r) in enumerate(blocks):
            nt_ = kn * 128
            col0 = b * ntk + k0 * 128
            xt_ = xTb[bi % 2]
            P.dma(xt_, xt_[:, 0:nt_], xT, xT[:, col0:col0 + nt_])
            ps = nextpb()

            def trj(e, ps=ps, k0=k0, kn=kn):
                for j in range(kn):
                    ins = e.matmul(ps[:, j * 128:(j + 1) * 128], lhsT=Ysb[:, k0 + j, :], rhs=anti[:], start=True, stop=True)
                return ins
            P.op("pe", [Ysb, anti], [ps], trj, n_inst=kn)
            z_, g_ = zv[bi % 2], gv[bi % 2]
            y_ = xt_
            P.op("act", [xt_, AB], [xt_], lambda e, r=r: e.activation(out=xt_[:, 0:nt_], in_=xt_[:, 0:nt_], func=AF.Identity,
                                                                      scale=AB[:, r, 0:1], bias=AB[:, r, 1:2]))
            tt(y_, y_[:, 0:nt_], ps, ps[:, 0:nt_], xt_, xt_[:, 0:nt_], ALU.add)
            tt(z_, z_[:, 0:nt_], y_, y_[:, 0:nt_], y_, y_[:, 0:nt_], ALU.mult)
            P.op("dve", [z_], [z_], lambda e: e.tensor_scalar(out=z_[:, 0:nt_], in0=z_[:, 0:nt_], scalar1=0.044715, scalar2=1.0,
                                                              op0=ALU.mult, op1=ALU.add))
            tt(z_, z_[:, 0:nt_], z_, z_[:, 0:nt_], y_, y_[:, 0:nt_], ALU.mult)
            P.op("act", [z_], [z_], lambda e: e.activation(out=z_[:, 0:nt_], in_=z_[:, 0:nt_], func=AF.Sigmoid, scale=1.5957691216057308))
            tt(g_, g_[:, 0:nt_], y_, y_[:, 0:nt_], z_, z_[:, 0:nt_], ALU.mult)
            P.dma(gTo, gTo[:, col0:col0 + nt_], g_, g_[:, 0:nt_])
    P.finish()
    return P


ANTI_BF = np.ascontiguousarray(np.eye(128, dtype=np.float32)[::-1]).astype(NPBF)


def run_s5_layer(i, j, ctx_out, x, ctx, wb, modv, inputs):
    P = _prog(("s5",), build_s5)
    in_maps = []
    for c in range(NCORES):
        ch = slice(128 * c, 128 * c + 128)
        gsl = slice(8 * c, 8 * c + 8)
        xtok = np.ascontiguousarray(np.concatenate([ctx[:, :, ch], x[:, :, ch]], axis=1))
        xT = np.ascontiguousarray(xtok.transpose(2, 0, 1).reshape(128, -1))
        mrow = np.ascontiguousarray(np.stack([modv[i, :, 0 * D:1 * D][:, ch], modv[i, :, 1 * D:2 * D][:, ch]], axis=1))
        mcol = np.ascontiguousarray(mrow.transpose(2, 0, 1).reshape(128, 6))

        def pg(a):
            return np.ascontiguousarray(a[:, gsl, :].transpose(2, 0, 1).reshape(64, 16)).astype(np.float32)
        bre = np.ascontiguousarray(inputs["s5_b_re"][j][:, gsl].transpose(2, 0, 1, 3).reshape(64, 256)).astype(np.float32)
        bim = np.ascontiguousarray(inputs["s5_b_im"][j][:, gsl].transpose(2, 0, 1, 3).reshape(64, 256)).astype(np.float32)
        cre = np.ascontiguousarray(inputs["s5_c_re"][j][:, gsl].transpose(3, 0, 1, 2).reshape(64, 256)).astype(np.float32)
        cim = np.ascontiguousarray(inputs["s5_c_im"][j][:, gsl].transpose(3, 0, 1, 2).reshape(64, 256)).astype(np.float32)
        in_maps.append({"ident_in": IDENT, "anti_in": ANTI_BF, "xtok": xtok, "xT": xT, "mrow": mrow, "mcol": mcol,
                        "are": pg(inputs["s5_a_re"][j]), "aim": pg(inputs["s5_a_im"][j]),
                        "ldt": np.ascontiguousarray(inputs["s5_log_dt"][j][:, gsl].reshape(1, 16)).astype(np.float32),
                        "bre": bre, "bim": bim, "cre": cre, "cim": cim,
                        "dsk": np.ascontiguousarray(inputs["s5_d"][j][ch].reshape(128, 1)).astype(np.float32)})
    res = run_prog(P, in_maps)
    gT = np.concatenate([np.asarray(r["gT"]) for r in res], axis=0)
    ntk = NCH * 128
    aT = []
    for c in range(NCORES):
        b, q = core_bq(c)
        lat = gT[:, b * ntk + NCTX + q * NLAT: b * ntk + NCTX + (q + 1) * NLAT]
        if ctx_out:
            lat = np.concatenate([lat, gT[:, b * ntk: b * ntk + NCTX]], axis=1)
        aT.append(np.ascontiguousarray(lat))
    return run_tail("s5", ctx_out, i, aT, x, ctx, [wb["s5_w_val"][j], wb["s5_w_gate"][j]], wb, modv, inputs), gT


def kernel(**inputs):
    inputs = {k: np.asarray(v) for k, v in inputs.items()}
    wb, modv = run_prep(inputs)
    x = np.ascontiguousarray(inputs["x"], dtype=np.float32)
    ctx = np.ascontiguousarray(inputs["ctx"], dtype=np.float32)
    (x, ctx), _ = run_s5_layer(0, 0, True, x, ctx, wb, modv, inputs)
    (x, ctx), _ = run_attn_layer(1, "swa", True, x, ctx, wb, modv, inputs)
    (x, ctx), _ = run_attn_layer(2, "gqa", True, x, ctx, wb, modv, inputs)
    (x, ctx), _ = run_s5_layer(3, 1, False, x, ctx, wb, modv, inputs)
    return np.ascontiguousarray(x, dtype=np.float32)
```
